# Optimizing a Trainium2 kernel written in Bass

```python
import jax, jax.numpy as jnp
from jax import lax
import numpy as np

D_MODEL = 1024
BATCH = 8
SEQ = 2048
DEPTH = 2

MIX_WIDTH = D_MODEL // 2
RET_HEADS = 4
RET_V_DIM = MIX_WIDTH // RET_HEADS
RET_QK_DIM = RET_V_DIM // 2
RET_CHUNK = 128
SC_WIDTH = MIX_WIDTH
SC_KERNEL = 3
CF_WIDTH = MIX_WIDTH
CF_KERNEL = 31
N_BRANCH = 3
D_FF = int(round(8 * D_MODEL / 3 / 256)) * 256
FFN_KERNEL = 3
ROPE_BASE = 10000.0
EPS = 1e-6

IN_SIZES = [RET_HEADS * RET_QK_DIM, RET_HEADS * RET_QK_DIM, MIX_WIDTH, MIX_WIDTH,
            SC_WIDTH, SC_WIDTH, SC_WIDTH, CF_WIDTH, CF_WIDTH, N_BRANCH * D_MODEL]
IN_WIDTH = sum(IN_SIZES)

kernel_name = "hybrid_retention_shortconv_conformer_gated_block"


def rmsnorm(x, g):
    xf = x.astype(jnp.float32)
    y = xf * lax.rsqrt(jnp.mean(xf * xf, axis=-1, keepdims=True) + EPS)
    return (y * g.astype(jnp.float32)).astype(x.dtype)


def layernorm(x, g, b):
    xf = x.astype(jnp.float32)
    mu = jnp.mean(xf, axis=-1, keepdims=True)
    xc = xf - mu
    y = xc * lax.rsqrt(jnp.mean(xc * xc, axis=-1, keepdims=True) + EPS)
    return (y * g.astype(jnp.float32) + b.astype(jnp.float32)).astype(x.dtype)


def causal_dwconv(x, w, b=None):
    K, C = w.shape
    xp = jnp.pad(x, ((0, 0), (K - 1, 0), (0, 0)))
    y = lax.conv_general_dilated(xp, w.astype(x.dtype)[:, None, :], window_strides=(1,),
                                 padding='VALID', dimension_numbers=('NWC', 'WIO', 'NWC'),
                                 feature_group_count=C)
    if b is not None:
        y = y + b.astype(x.dtype)
    return y


def rotary(x, pos):
    half = x.shape[-1] // 2
    inv = ROPE_BASE ** (-jnp.arange(half, dtype=jnp.float32) / half)
    ang = pos.astype(jnp.float32)[:, None] * inv[None, :]
    cos = jnp.cos(ang)[None, :, None, :]
    sin = jnp.sin(ang)[None, :, None, :]
    x1, x2 = x[..., :half], x[..., half:]
    return jnp.concatenate([x1 * cos - x2 * sin, x2 * cos + x1 * sin], axis=-1)


def retention_chunkwise(q, k, v):
    B, S, H, dk = q.shape
    dv = v.shape[-1]
    C = RET_CHUNK
    N = S // C
    gamma = 1.0 - jnp.exp2(-5.0 - jnp.arange(H, dtype=jnp.float32))
    log_g = jnp.log(gamma)
    i = jnp.arange(C, dtype=jnp.float32)
    diff = i[:, None] - i[None, :]
    decay = jnp.where(diff[None] >= 0,
                      jnp.exp(jnp.maximum(diff, 0.0)[None] * log_g[:, None, None]), 0.0)
    zeta = jnp.exp((C - 1.0 - i)[None, :] * log_g[:, None])
    xi = jnp.exp((i + 1.0)[None, :] * log_g[:, None])
    chunk_decay = jnp.exp(C * log_g)

    def chunks(t):
        return t.reshape(B, N, C, H, t.shape[-1]).transpose(0, 3, 1, 2, 4)

    qc, kc, vc = chunks(q), chunks(k), chunks(v)
    scores = jnp.einsum('bhncd,bhnmd->bhncm', qc, kc) * decay[None, :, None]
    o_inner = jnp.einsum('bhncm,bhnme->bhnce', scores, vc)
    kv = jnp.einsum('bhnmd,bhnme->bhnde', kc * zeta[None, :, None, :, None], vc)

    def step(R, kv_n):
        return chunk_decay[None, :, None, None] * R + kv_n, R

    _, R_prev = lax.scan(step, jnp.zeros((B, H, dk, dv), jnp.float32), jnp.moveaxis(kv, 2, 0))
    R_prev = jnp.moveaxis(R_prev, 0, 2)
    o_cross = jnp.einsum('bhncd,bhnde->bhnce', qc, R_prev) * xi[None, :, None, :, None]
    return (o_inner + o_cross).transpose(0, 2, 3, 1, 4).reshape(B, S, H, dv)


def head_groupnorm(o):
    mu = jnp.mean(o, axis=-1, keepdims=True)
    oc = o - mu
    return oc * lax.rsqrt(jnp.mean(oc * oc, axis=-1, keepdims=True) + EPS)


def setup_inputs(seed: int = 0) -> dict:
    key = jax.random.key(seed)
    ks = jax.random.split(key, 20)
    f32 = jnp.float32

    def w(k, shape, fan_in):
        return jax.random.normal(k, shape, f32) * (fan_in ** -0.5)

    def gain(k, shape):
        return 1.0 + 0.02 * jax.random.normal(k, shape, f32)

    def small(k, shape):
        return 0.01 * jax.random.normal(k, shape, f32)

    L = DEPTH
    return {
        "x": jax.random.normal(ks[0], (BATCH, SEQ, D_MODEL), f32),
        "norm_mix_pre": gain(ks[1], (L, D_MODEL)),
        "norm_mix_post": gain(ks[2], (L, D_MODEL)),
        "norm_ffn_pre": gain(ks[3], (L, D_MODEL)),
        "norm_ffn_post": gain(ks[4], (L, D_MODEL)),
        "w_in": w(ks[5], (L, D_MODEL, IN_WIDTH), D_MODEL),
        "w_ret_out": w(ks[6], (L, MIX_WIDTH, D_MODEL), MIX_WIDTH),
        "sc_conv_w": w(ks[7], (L, SC_KERNEL, SC_WIDTH), SC_KERNEL),
        "w_sc_out": w(ks[8], (L, SC_WIDTH, D_MODEL), SC_WIDTH),
        "cf_conv_w": w(ks[9], (L, CF_KERNEL, CF_WIDTH), CF_KERNEL),
        "cf_conv_b": small(ks[10], (L, CF_WIDTH)),
        "cf_ln_g": gain(ks[11], (L, CF_WIDTH)),
        "cf_ln_b": small(ks[12], (L, CF_WIDTH)),
        "w_cf_out": w(ks[13], (L, CF_WIDTH, D_MODEL), CF_WIDTH),
        "w_o": w(ks[14], (L, D_MODEL, D_MODEL), D_MODEL),
        "w_up": w(ks[15], (L, D_MODEL, 2 * D_FF), D_MODEL),
        "ffn_conv_w": w(ks[16], (L, FFN_KERNEL, 2 * D_FF), FFN_KERNEL),
        "w_down": w(ks[17], (L, D_FF, D_MODEL), D_FF),
    }


def reference(x, norm_mix_pre, norm_mix_post, norm_ffn_pre, norm_ffn_post, w_in, w_ret_out,
              sc_conv_w, w_sc_out, cf_conv_w, cf_conv_b, cf_ln_g, cf_ln_b, w_cf_out, w_o,
              w_up, ffn_conv_w, w_down):
    B, S, D = x.shape
    dt = x.dtype
    pos = jnp.arange(S)
    split_idx = [int(c) for c in np.cumsum(IN_SIZES)[:-1]]
    for l in range(DEPTH):
        h = rmsnorm(x, norm_mix_pre[l])
        proj = h @ w_in[l].astype(dt)
        (q, k, v, g_ret, sc_b, sc_c, sc_x, cf_a, cf_b, gate_logits) = jnp.split(proj, split_idx, axis=-1)

        qh = rotary(q.astype(jnp.float32).reshape(B, S, RET_HEADS, RET_QK_DIM), pos)
        kh = rotary(k.astype(jnp.float32).reshape(B, S, RET_HEADS, RET_QK_DIM), pos) * (RET_QK_DIM ** -0.5)
        vh = v.astype(jnp.float32).reshape(B, S, RET_HEADS, RET_V_DIM)
        o = head_groupnorm(retention_chunkwise(qh, kh, vh)).reshape(B, S, MIX_WIDTH).astype(dt)
        y_ret = (jax.nn.silu(g_ret) * o) @ w_ret_out[l].astype(dt)

        u = causal_dwconv(sc_c * sc_x, sc_conv_w[l])
        y_sc = (sc_b * u) @ w_sc_out[l].astype(dt)

        glu = cf_a * jax.nn.sigmoid(cf_b)
        c = causal_dwconv(glu, cf_conv_w[l], cf_conv_b[l])
        c = jax.nn.silu(layernorm(c, cf_ln_g[l], cf_ln_b[l]))
        y_cf = c @ w_cf_out[l].astype(dt)

        gates = jax.nn.sigmoid(gate_logits).reshape(B, S, N_BRANCH, D)
        merged = gates[:, :, 0] * y_ret + gates[:, :, 1] * y_sc + gates[:, :, 2] * y_cf
        x = x + rmsnorm(merged @ w_o[l].astype(dt), norm_mix_post[l])

        h = rmsnorm(x, norm_ffn_pre[l])
        up = causal_dwconv(h @ w_up[l].astype(dt), ffn_conv_w[l])
        gate, val = jnp.split(up, 2, axis=-1)
        f = (jax.nn.silu(gate) * val) @ w_down[l].astype(dt)
        x = x + rmsnorm(f, norm_ffn_post[l])
    return x
```

```python
import contextlib
import numpy as np
import concourse.bass as bass
import concourse.mybir as mybir
from concourse.bass_utils import run_bass_kernel_spmd

F32 = mybir.dt.float32
BF16 = mybir.dt.bfloat16
AF = mybir.ActivationFunctionType
ALU = mybir.AluOpType

D = 1024
SEQ = 2048
NL = 2
NFF = 22
EPS = 1e-6
NSLOT = 3
SLOTC = 4608
ENGS = ("pe", "act", "dve", "pool", "sp")

PV_MIXPRE, PV_FFNPRE, PV_SCW, PV_CFW, PV_CFB, PV_LNG, PV_LNB, PV_FGW, PV_FVW = 0, 8, 16, 28, 152, 156, 160, 164, 230
NPV = 296
C_ID, C_DM, C_XI, C_ZS, C_GC = 0, 128, 640, 896, 1152
NCST = 1154


class Sched:
    def __init__(self, nc, self_sync=("act", "dve", "pool")):
        self.nc = nc
        self.prog = {e: [] for e in ENGS}
        self.cnt = {e: 0 for e in ENGS}
        self.waited = {}
        self.lastw = {}
        self.readers = {}
        self.self_sync = set(self_sync)
        self.dma_sems = {}
        self.n_instr = 0

    def _need(self, eng, reads, writes, skip=None):
        need = {}

        def add(dep):
            if dep is None:
                return
            d, v = dep
            if need.get(d, 0) < v:
                need[d] = v

        for r in reads:
            add(self.lastw.get(r))
            if r.startswith("pf") or r.startswith("pb"):
                for d, v in self.readers.get(r, {}).items():
                    if d != eng:
                        add((d, v))
        for w in writes:
            add(self.lastw.get(w))
            for d, v in self.readers.get(w, {}).items():
                add((d, v))
        out = []
        for d, v in need.items():
            if d == skip:
                continue
            if d == eng and eng not in self.self_sync:
                continue
            key = (eng, d)
            if self.waited.get(key, 0) >= v:
                continue
            self.waited[key] = v
            out.append((d, v))
        return out

    def _mark(self, who, idx, reads, writes):
        for r in reads:
            self.readers.setdefault(r, {})[who] = idx
        for w in writes:
            self.lastw[w] = (who, idx)
            self.readers[w] = {}

    def op(self, eng, fn, reads=(), writes=()):
        waits = self._need(eng, reads, writes)
        self.cnt[eng] += 1
        self.prog[eng].append((waits, fn, ("eng", eng, 1)))
        self._mark(eng, self.cnt[eng], reads, writes)
        self.n_instr += 1

    def pe(self, fns, reads=(), writes=()):
        waits = self._need("pe", reads, writes)
        self.cnt["pe"] += 1
        n = len(fns)
        for i, fn in enumerate(fns):
            self.prog["pe"].append((waits if i == 0 else [], fn, ("eng", "pe", 1) if i == n - 1 else None))
        self._mark("pe", self.cnt["pe"], reads, writes)
        self.n_instr += n

    def dma(self, queue, fn, semname, reads=(), writes=()):
        d = "dma:" + semname
        waits = self._need(queue, reads, writes, skip=d)
        c = self.dma_sems.setdefault(semname, [0])
        c[0] += 16
        self.prog[queue].append((waits, fn, ("dma", semname, 16)))
        self._mark(d, c[0], reads, writes)
        self.n_instr += 1

    def barrier(self):
        snap = dict(self.cnt)
        dsnap = {"dma:" + k: v[0] for k, v in self.dma_sems.items()}
        for e in ENGS:
            if e == "pe":
                continue
            waits = []
            for d, v in list(snap.items()) + list(dsnap.items()):
                if d != e and v > 0 and self.waited.get((e, d), 0) < v:
                    self.waited[(e, d)] = v
                    waits.append((d, v))
            if waits:
                self.prog[e].append((waits, None, None))

    def emit(self):
        nc = self.nc
        with nc.cleanup_on_exit():
            sems = {}
            for e in ENGS:
                sems[e] = nc.alloc_semaphore(name="s_" + e)
            for k in self.dma_sems:
                sems["dma:" + k] = nc.alloc_semaphore(name="d_" + k)
            for s_ in sems.values():
                nc.gpsimd.sem_clear(s_)
            nc.all_engine_barrier()
            with nc.Block() as block:

                def run(eng_name):
                    def body(eng):
                        for waits, fn, inc in self.prog[eng_name]:
                            for d, v in waits:
                                eng.wait_ge(sems[d], v)
                            if fn is None:
                                continue
                            ins = fn(eng)
                            if inc is not None:
                                kind, name, n = inc
                                ins.then_inc(sems[name] if kind == "eng" else sems["dma:" + name], n)
                    return body

                block.tensor(run("pe"))
                block.scalar(run("act"))
                block.vector(run("dve"))
                block.gpsimd(run("pool"))
                block.sync(run("sp"))


def _blk(w):
    K, n = w.shape
    return np.ascontiguousarray(w.reshape(K // 128, 128, n).transpose(1, 0, 2)).reshape(128, -1)


def weight_blocks():
    blocks = [("q", 8, 512), ("k", 8, 512), ("v", 8, 512), ("g", 8, 512)]
    blocks += [("sc%d" % j, 8, 384) for j in range(4)]
    blocks += [("cf%d" % j, 8, 256) for j in range(4)]
    blocks += [("gb%d" % f, 1, 4608) for f in range(8)]
    blocks += [("wo0", 8, 512), ("wo1", 8, 512)]
    blocks += [("up%d" % j, 8, 256) for j in range(NFF)]
    blocks += [("wd%d" % c, 2, 1024) for c in range(11)]
    offs = {}
    o = 0
    for name, kc, n in blocks:
        offs[name] = (o, kc, n)
        o += kc * n
    return blocks, offs, o


def prep_layer_stream(inp, l):
    w_in = np.asarray(inp["w_in"][l], dtype=np.float32)
    perm = np.array([h * 64 + (i + 32) % 64 for h in range(4) for i in range(64)])
    q = w_in[:, 0:256]
    k = w_in[:, 256:512]
    parts = {}
    parts["q"] = np.concatenate([q, q[:, perm]], axis=1)
    parts["k"] = np.concatenate([k, k[:, perm]], axis=1)
    parts["v"] = w_in[:, 512:1024]
    parts["g"] = w_in[:, 1024:1536]
    for j in range(4):
        s = slice(j * 128, (j + 1) * 128)
        parts["sc%d" % j] = np.concatenate([w_in[:, 1536:2048][:, s], w_in[:, 2048:2560][:, s], w_in[:, 2560:3072][:, s]], axis=1)
        parts["cf%d" % j] = np.concatenate([w_in[:, 3072:3584][:, s], w_in[:, 3584:4096][:, s]], axis=1)
    wro = np.asarray(inp["w_ret_out"][l], np.float32)
    wso = np.asarray(inp["w_sc_out"][l], np.float32)
    wco = np.asarray(inp["w_cf_out"][l], np.float32)
    for f in range(8):
        s = slice(f * 128, (f + 1) * 128)
        gt = np.concatenate([w_in[:, 4096 + b * 1024: 4096 + (b + 1) * 1024][:, s] for b in range(3)], axis=1)
        bo = np.concatenate([wro[:, s], wso[:, s], wco[:, s]], axis=1)
        parts["gb%d" % f] = np.concatenate([_blk(gt), _blk(bo)], axis=1)
    wo = np.asarray(inp["w_o"][l], np.float32)
    parts["wo0"] = wo[:, 0:512]
    parts["wo1"] = wo[:, 512:1024]
    wup = np.asarray(inp["w_up"][l], np.float32)
    for j in range(NFF):
        s = slice(j * 128, (j + 1) * 128)
        parts["up%d" % j] = np.concatenate([wup[:, :2816][:, s], wup[:, 2816:][:, s]], axis=1)
    wd = np.asarray(inp["w_down"][l], np.float32)
    for c in range(11):
        parts["wd%d" % c] = wd[c * 256:(c + 1) * 256, :]
    blocks, offs, total = weight_blocks()
    out = np.empty((128, total), np.float32)
    for name, kc, n in blocks:
        o = offs[name][0]
        out[:, o:o + kc * n] = parts[name] if name.startswith("gb") else _blk(parts[name])
    return out


def prep_pv(inp, l):
    pv = np.zeros((128, NPV), np.float32)

    def fm(vec, nchunk):
        return np.asarray(vec, np.float32).reshape(nchunk, 128).T

    pv[:, PV_MIXPRE:PV_MIXPRE + 8] = fm(inp["norm_mix_pre"][l], 8)
    pv[:, PV_FFNPRE:PV_FFNPRE + 8] = fm(inp["norm_ffn_pre"][l], 8)
    scw = np.asarray(inp["sc_conv_w"][l], np.float32)
    pv[:, PV_SCW:PV_SCW + 12] = scw.reshape(3, 4, 128).transpose(2, 1, 0).reshape(128, 12)
    cfw = np.asarray(inp["cf_conv_w"][l], np.float32)
    pv[:, PV_CFW:PV_CFW + 124] = cfw.reshape(31, 4, 128).transpose(2, 1, 0).reshape(128, 124)
    pv[:, PV_CFB:PV_CFB + 4] = fm(inp["cf_conv_b"][l], 4)
    pv[:, PV_LNG:PV_LNG + 4] = fm(inp["cf_ln_g"][l], 4)
    pv[:, PV_LNB:PV_LNB + 4] = fm(inp["cf_ln_b"][l], 4)
    fw = np.asarray(inp["ffn_conv_w"][l], np.float32)
    pv[:, PV_FGW:PV_FGW + 66] = fw[:, :2816].reshape(3, NFF, 128).transpose(2, 1, 0).reshape(128, 66)
    pv[:, PV_FVW:PV_FVW + 66] = fw[:, 2816:].reshape(3, NFF, 128).transpose(2, 1, 0).reshape(128, 66)
    return pv


def make_consts():
    cst = np.zeros((128, NCST), np.float64)
    cst[:, C_ID:C_ID + 128] = np.eye(128)
    gam = 1.0 - np.exp2(-5.0 - np.arange(4))
    m = np.arange(128)[:, None]
    c = np.arange(128)[None, :]
    for h in range(4):
        dm = np.where(c >= m, gam[h] ** np.maximum(c - m, 0), 0.0) * 0.125
        sl_ = [0, 2, 1, 3][h]
        cst[:, C_DM + sl_ * 128:C_DM + (sl_ + 1) * 128] = dm
        cst[:, C_ZS + h * 64:C_ZS + (h + 1) * 64] = (0.125 * gam[h] ** (127 - np.arange(128)))[:, None]
    p = np.arange(128)
    for ch in range(2):
        hh = ch * 2 + p // 64
        xi = gam[hh][:, None] ** (np.arange(128)[None, :] + 1.0)
        cst[:, C_XI + ch * 128:C_XI + (ch + 1) * 128] = xi
        cst[:, C_GC + ch] = gam[hh] ** 128
    inv = 10000.0 ** (-np.arange(32) / 32.0)
    t = np.arange(SEQ)[None, :]
    ang = t * inv[p % 32][:, None]
    sign = np.where((p % 64) < 32, -1.0, 1.0)[:, None]
    rope = np.stack([np.cos(ang), sign * np.sin(ang)], axis=0)
    return cst.astype(np.float32), rope.astype(np.float32)


class Arena:
    def __init__(self, nc, S, nbytes):
        self.t = nc.alloc_sbuf_tensor("arena", [128, nbytes // 2], BF16)
        self.top = 0
        self.cap = nbytes
        self.stack = []
        self.S = S
        self.peak = 0

    def push(self):
        self.stack.append(self.top)

    def pop(self):
        self.S.barrier()
        self.top = self.stack.pop()

    def alloc(self, shape, dtype):
        n = int(np.prod(shape))
        esz = 4 if dtype == F32 else 2
        nb = (n * esz + 63) // 64 * 64
        off = self.top
        self.top += nb
        self.peak = max(self.peak, self.top)
        assert self.top <= self.cap, ("arena overflow", self.top, self.cap)
        ap = self.t[:, off // 2: off // 2 + (n * esz) // 2]
        if dtype == F32:
            ap = ap.bitcast(F32)
        if len(shape) == 2:
            names = "a b"
        else:
            names = "a b c"
        if len(shape) == 1:
            return ap
        kw = {"a": shape[0]} if len(shape) == 2 else {"a": shape[0], "b": shape[1]}
        return ap.rearrange("p (%s) -> p %s" % (names, names), **kw)


def build(n_layers=NL, dbg=()):
    nc = bass.Bass("TRN2", target_bir_lowering=False)
    blocks, offs, WCOLS = weight_blocks()
    x_d = nc.dram_tensor("x", [SEQ, D], F32, kind="ExternalInput").ap()
    wst_d = nc.dram_tensor("wst", [n_layers, 128, WCOLS], F32, kind="ExternalInput").ap()
    pv_d = nc.dram_tensor("pv", [n_layers, 128, NPV], F32, kind="ExternalInput").ap()
    gbc_d = nc.dram_tensor("gbc", [n_layers, 2, 128, D], F32, kind="ExternalInput").ap()
    cst_d = nc.dram_tensor("cst", [128, NCST], F32, kind="ExternalInput").ap()
    rope_d = nc.dram_tensor("rope", [2, 128, SEQ], F32, kind="ExternalInput").ap()
    out_d = nc.dram_tensor("out", [SEQ, D], F32, kind="ExternalOutput").ap()
    dbg_d = {}

    S = Sched(nc)

    X = nc.alloc_sbuf_tensor("X", [128, 16, D], F32)
    HT = nc.alloc_sbuf_tensor("HT", [128, 8, 1024], BF16)
    RING = nc.alloc_sbuf_tensor("RING", [128, NSLOT, SLOTC], BF16)
    CST = nc.alloc_sbuf_tensor("CST", [128, NCST], F32)
    PV = nc.alloc_sbuf_tensor("PVt", [128, NPV], F32)
    GBC = nc.alloc_sbuf_tensor("GBC", [128, D], F32)
    IDB = nc.alloc_sbuf_tensor("IDB", [128, 128], BF16)
    ONEC = nc.alloc_sbuf_tensor("ONEC", [128, 2], BF16)
    ONER = nc.alloc_sbuf_tensor("ONER", [1, 128], F32)
    RS = nc.alloc_sbuf_tensor("RS", [128, 2, 128], F32)
    RB = nc.alloc_sbuf_tensor("RB", [128, 2, 128], BF16)
    PH = nc.alloc_sbuf_tensor("PH", [128, 4, 2], BF16)
    GH = nc.alloc_sbuf_tensor("GH", [128, 4, 30], BF16)
    FH = nc.alloc_sbuf_tensor("FH", [128, NFF, 4], BF16)
    SM_ = nc.alloc_sbuf_tensor("SMALL", [128, 64], F32)
    PST = nc.alloc_sbuf_tensor("PST", [128, 32], F32)
    JUNK = nc.alloc_sbuf_tensor("JUNK", [128, 1024], BF16)
    PSF = nc.alloc_psum_tensor("PSF", [128, 6, 512], F32)
    PSB = nc.alloc_psum_tensor("PSB", [128, 2, 1024], BF16)
    AR = Arena(nc, S, 85 * 1024)

    ident_f = CST[:, C_ID:C_ID + 128]
    dmask = CST[:, C_DM:C_DM + 512]
    zsfull = CST[:, C_ZS:C_ZS + 256]

    st = {"pf": 0, "pb": 0, "blk": 0, "issued": 0}
    stream = []
    for l in range(n_layers):
        for hf in range(2):
            for name, kc, n in blocks:
                if not (name.startswith("up") or name.startswith("wd")):
                    stream.append((l, name))
        for qt in range(4):
            for j in range(NFF):
                stream.append((l, "up%d" % j))

    def pf(fixed=None):
        if fixed is not None:
            return PSF[:, fixed, :], "pf%d" % fixed
        b = st["pf"] % 6
        st["pf"] += 1
        return PSF[:, b, :], "pf%d" % b

    def pb():
        b = st["pb"] % 2
        st["pb"] += 1
        return PSB[:, b, 0:512], "pb%d" % b

    def issue_block(i):
        l, name = stream[i]
        o, kc, n = offs[name]
        s = i % NSLOT
        cols = kc * n
        S.dma("pool", lambda e: e.dma_start(
            out=RING[:, s, 0:cols].rearrange("p (a b) -> p a b", b=512),
            in_=wst_d[l, :, o:o + cols].rearrange("p (a b) -> p a b", b=512)),
            "ring%d" % s, writes=["ring%d" % s])

    def wblock(l, name):
        i = st["blk"]
        assert stream[i] == (l, name), (stream[i], l, name)
        st["blk"] += 1
        while st["issued"] <= min(i + 1, len(stream) - 1):
            issue_block(st["issued"])
            st["issued"] += 1
        o, kc, n = offs[name]
        s = i % NSLOT
        return RING[:, s, 0:kc * n].rearrange("p (k n) -> p k n", n=n), "ring%d" % s

    class Rot:
        def __init__(self, name, shape, dtype, n=2):
            self.bufs = [AR.alloc(shape, dtype) for _ in range(n)]
            self.name = name
            self.i = 0

        def get(self):
            k = self.i % len(self.bufs)
            self.i += 1
            return self.bufs[k], "%s#%d" % (self.name, k)

    def ACT(out, in_, func, r, w, **kw):
        S.op("act", lambda e: e.activation(out=out, in_=in_, func=func, **kw), r, w)

    def TT(out, in0, in1, op, r, w):
        S.op("dve", lambda e: e.tensor_tensor(out=out, in0=in0, in1=in1, op=op), r, w)

    def TS(out, in0, s1, s2, op0, op1, r, w):
        if op1 is None:
            S.op("dve", lambda e: e.tensor_scalar(out=out, in0=in0, scalar1=s1, scalar2=None, op0=op0), r, w)
        else:
            S.op("dve", lambda e: e.tensor_scalar(out=out, in0=in0, scalar1=s1, scalar2=s2, op0=op0, op1=op1), r, w)

    def STT(out, in0, sc, in1, op0, op1, r, w):
        S.op("dve", lambda e: e.scalar_tensor_tensor(out=out, in0=in0, scalar=sc, in1=in1, op0=op0, op1=op1), r, w)

    def CP(out, in_, r, w):
        S.op("dve", lambda e: e.tensor_copy(out=out, in_=in_), r, w)

    def MM(specs, r, w):
        S.pe([(lambda e, o=o, a=a, b=b, s0=s0, s1=s1: e.matmul(o, lhsT=a, rhs=b, start=s0, stop=s1))
              for (o, a, b, s0, s1) in specs], r, w)

    def TR(specs, r, w):
        S.pe([(lambda e, o=o, a=a: e.transpose(out=o, in_=a, identity=IDB[:])) for (o, a) in specs], r, w)

    def POW(out, in0, r, w):
        tag = w[0] + "~sq"
        S.op("act", lambda e: e.activation(out=out, in_=in0, func=AF.Sqrt), r, [tag])
        S.op("dve", lambda e: e.reciprocal(out=out, in_=out), [tag], w)

    def MEMSET(ap, val, w):
        S.op("dve", lambda e: e.memset(ap, val), [], w)

    def DBG(name, ap, reads):
        if name not in dbg:
            return
        shape = list(ap.shape)
        d = nc.dram_tensor("dbg_" + name, shape, ap.dtype, kind="ExternalOutput").ap()
        dbg_d[name] = d
        S.dma("sp", lambda e: e.dma_start(out=d, in_=ap), "dbg_" + name, reads=reads)

    S.dma("sp", lambda e: e.dma_start(out=CST[:], in_=cst_d), "cst", writes=["cst"])
    for q4 in range(4):
        S.dma("sp", lambda e, q4=q4: e.dma_start(
            out=X[:, q4 * 4:(q4 + 1) * 4, :],
            in_=x_d[q4 * 512:(q4 + 1) * 512, :].rearrange("(a p) d -> p a d", p=128)),
            "x%d" % q4, writes=["x%d" % (q4 * 4 + a) for a in range(4)])
    CP(IDB[:], ident_f, ["cst"], ["idb"])
    MEMSET(ONEC[:], 1.0 / 512.0, ["onec"])
    MEMSET(ONER[:], 1.0, ["oner"])

    def prenorm(l, tiles, gcol, HTv, hn_alloc, hn_res):
        nt = len(tiles)
        for g4 in range(nt // 4):
            so = g4 * 4
            tg = "g%d" % g4
            for i in range(so, so + 4):
                tt = tiles[i]
                ACT(hn_alloc[:, i, :], X[:, tt, :], AF.Square, ["x%d" % tt], hn_res(i) + ["ss" + tg], accum_out=SM_[:, i:i + 1])
            TS(SM_[:, 8 + so:12 + so], SM_[:, so:so + 4], 1.0 / D, EPS, ALU.mult, ALU.add, ["ss" + tg], ["ms" + tg])
            POW(SM_[:, 16 + so:20 + so], SM_[:, 8 + so:12 + so], ["ms" + tg], ["rstd" + tg])
            for i in range(so, so + 4):
                tt = tiles[i]
                TS(hn_alloc[:, i, :], X[:, tt, :], SM_[:, 16 + i:17 + i], None, ALU.mult, None,
                   ["x%d" % tt, "rstd" + tg], hn_res(i))
            for kc in range(8):
                p, pr = pb()
                TR([(p[:, a * 128:(a + 1) * 128], hn_alloc[:, g4 * 4 + a, kc * 128:(kc + 1) * 128]) for a in range(4)],
                   [x for a in range(4) for x in hn_res(g4 * 4 + a)] + ["idb"], [pr])
                dst = HTv[:, kc, g4 * 512:(g4 + 1) * 512]
                res = ["ht%d_%d" % (kc, g4)]
                if kc % 2 == 0:
                    ACT(dst, p, AF.Copy, [pr, "pv"], res, scale=PV[:, gcol + kc:gcol + kc + 1])
                else:
                    TS(dst, p, PV[:, gcol + kc:gcol + kc + 1], None, ALU.mult, None, [pr, "pv"], res)

    def postnorm(tt, Y, Yr, tmpR):
        junk = tmpR["junk"]
        for h in range(2):
            ACT(junk, Y[h], AF.Square, [Yr[h]], ["junk", "ss2_%d" % h], accum_out=SM_[:, 32 + h:33 + h])
        TT(SM_[:, 34:35], SM_[:, 32:33], SM_[:, 33:34], ALU.add, ["ss2_0", "ss2_1"], ["ms2"])
        TS(SM_[:, 35:36], SM_[:, 34:35], 1.0 / D, EPS, ALU.mult, ALU.add, ["ms2"], ["ms2b"])
        POW(SM_[:, 36:37], SM_[:, 35:36], ["ms2b"], ["rstd2"])
        for h in range(2):
            t, tr = tmpR["t"].get()
            STT(t, Y[h], SM_[:, 36:37], GBC[:, h * 512:(h + 1) * 512], ALU.mult, ALU.mult, [Yr[h], "rstd2", "gbc"], [tr])
            TT(X[:, tt, h * 512:(h + 1) * 512], X[:, tt, h * 512:(h + 1) * 512], t, ALU.add, ["x%d" % tt, tr], ["x%d" % tt])

    for l in range(n_layers):
        S.dma("sp", lambda e, l=l: e.dma_start(out=PV[:], in_=pv_d[l]), "pv", writes=["pv"])
        for hf in range(2):
            T0 = hf * 1024
            AR.push()
            ART = AR.alloc([4, 1024], BF16)
            AST = AR.alloc([4, 1024], BF16)
            ACFT = AR.alloc([4, 1024], BF16)

            AR.push()
            HN = AR.alloc([8, 1024], BF16)
            prenorm(l, [hf * 8 + i for i in range(8)], PV_MIXPRE, HT, HN, lambda i: ["hn%d" % i])
            AR.pop()
            htall = ["ht%d_%d" % (kc, g) for kc in range(8) for g in range(2)]
            if l == 0 and hf == 0:
                DBG("HT", HT[:], htall)

            AR.push()
            QT = AR.alloc([2, 1024], BF16)
            KT = AR.alloc([2, 1024], BF16)
            QXT = AR.alloc([2, 1024], BF16)
            ropeR = Rot("rope", [2, 512], F32)
            t1R = Rot("t1", [512], F32)
            t2R = Rot("t2", [512], F32)
            for which in ("q", "k"):
                W, wr = wblock(l, which)
                dstT = QT if which == "q" else KT
                for pt in range(2):
                    rp, rr = ropeR.get()
                    S.dma("sp", lambda e, rp=rp, src_=rope_d[:, :, T0 + pt * 512:T0 + (pt + 1) * 512].rearrange("t p n -> p t n"): e.dma_start(
                        out=rp, in_=src_),
                        "rope" + rr[-1], writes=[rr])
                    for c in range(2):
                        pa, par = pf()
                        MM([(pa, W[:, kc, c * 128:(c + 1) * 128], HT[:, kc, pt * 512:(pt + 1) * 512], kc == 0, kc == 7)
                            for kc in range(8)], [wr] + ["ht%d_%d" % (kc, pt) for kc in range(8)], [par])
                        pb_, pbr = pf()
                        MM([(pb_, W[:, kc, 256 + c * 128:256 + (c + 1) * 128], HT[:, kc, pt * 512:(pt + 1) * 512], kc == 0, kc == 7)
                            for kc in range(8)], [wr] + ["ht%d_%d" % (kc, pt) for kc in range(8)], [pbr])
                        t1, t1r = t1R.get()
                        t2, t2r = t2R.get()
                        TT(t1, pa, rp[:, 0, :], ALU.mult, [par, rr], [t1r])
                        TT(t2, pb_, rp[:, 1, :], ALU.mult, [pbr, rr], [t2r])
                        dres = "%sT%d_%d" % (which, c, pt)
                        TT(dstT[:, c, pt * 512:(pt + 1) * 512], t1, t2, ALU.add, [t1r, t2r], [dres])
                        if which == "q":
                            for r4 in range(4):
                                cs = slice(pt * 512 + r4 * 128, pt * 512 + (r4 + 1) * 128)
                                TT(QXT[:, c, cs], QT[:, c, cs], CST[:, C_XI + c * 128:C_XI + (c + 1) * 128], ALU.mult,
                                   [dres, "cst"], ["qxT%d_%d" % (c, pt)])
            if l == 0 and hf == 0:
                DBG("QT", QT, ["qT%d_%d" % (c, pt) for c in range(2) for pt in range(2)])
                DBG("KT", KT, ["kT%d_%d" % (c, pt) for c in range(2) for pt in range(2)])

            Wv, wvr = wblock(l, "v")
            Wg, wgr = wblock(l, "g")
            VbR = Rot("vb", [512], BF16)
            SGR = Rot("sg", [512], BF16)
            KZR = Rot("kz", [256], BF16)
            SMR = Rot("sm", [512], BF16)
            ONR = Rot("on", [512], F32)
            AAR = Rot("aa", [512], BF16)
            if hf == 0:
                MEMSET(RS[:], 0.0, ["rs"])
                MEMSET(RB[:], 0.0, ["rb"])
            def ret_a(i):
                pt = i // 4
                tok = slice(i * 128, (i + 1) * 128)
                htr = ["ht%d_%d" % (kc, pt) for kc in range(8)]
                pv_, pvr = pf(0)
                MM([(pv_, HT[:, kc, tok], Wv[:, kc, :], kc == 0, kc == 7) for kc in range(8)], htr + [wvr], [pvr])
                vb, vbr = VbR.get()
                ACT(vb, pv_, AF.Copy, [pvr], [vbr])
                pg, pgr = pf(1)
                MM([(pg, HT[:, kc, tok], Wg[:, kc, :], kc == 0, kc == 7) for kc in range(8)], htr + [wgr], [pgr])
                sg, sgr = SGR.get()
                ACT(sg, pg, AF.Silu, [pgr], [sgr])
                pk, pkr = pb()
                TR([(pk[:, c * 128:(c + 1) * 128], KT[:, c, tok]) for c in range(2)],
                   ["kT%d_%d" % (c, pt) for c in range(2)] + ["idb"], [pkr])
                kz, kzr = KZR.get()
                TT(kz, pk[:, 0:256], zsfull, ALU.mult, [pkr, "cst"], [kzr])
                return (vb, vbr, sg, sgr, kz, kzr)

            def ret_front(i, actx, mid_cb=None, after_rs=None):
                n = hf * 8 + i
                pt = i // 4
                tok = slice(i * 128, (i + 1) * 128)
                vb, vbr, sg, sgr, kz, kzr = actx
                psA, psAr = pf(2)
                psB, psBr = pf(3)
                HS = [0, 2, 1, 3]
                SL = [0, 2, 1, 3]
                sc_specs = []
                for h in range(4):
                    s_ = SL[h]
                    bank = psA if s_ < 2 else psB
                    sc_specs.append((bank[:, (s_ % 2) * 128:(s_ % 2) * 128 + 128],
                                     KT[(h % 2) * 64:(h % 2) * 64 + 64, h // 2, tok],
                                     QT[(h % 2) * 64:(h % 2) * 64 + 64, h // 2, tok], True, True))
                MM(sc_specs, ["kT%d_%d" % (c, pt) for c in range(2)] + ["qT%d_%d" % (c, pt) for c in range(2)], [psAr, psBr])
                sm, smr = SMR.get()
                TT(sm[:, 0:256], psA[:, 0:256], dmask[:, 0:256], ALU.mult, [psAr, "cst"], [smr + "a"])
                TT(sm[:, 256:512], psB[:, 0:256], dmask[:, 256:512], ALU.mult, [psBr, "cst"], [smr + "b"])
                if mid_cb is not None:
                    mid_cb()
                poA, poAr = pf(4)
                poB, poBr = pf(5)

                def obank(h):
                    s_ = SL[h]
                    return (poA if s_ < 2 else poB)[:, (s_ % 2) * 128:(s_ % 2) * 128 + 128]

                def obr(h):
                    return poAr if SL[h] < 2 else poBr

                specs = []
                for h in range(4):
                    s_ = SL[h]
                    specs.append((obank(h), sm[:, s_ * 128:(s_ + 1) * 128], vb[:, h * 128:(h + 1) * 128], True, n == 0))
                    if n > 0:
                        specs.append((obank(h),
                                      QXT[(h % 2) * 64:(h % 2) * 64 + 64, h // 2, tok],
                                      RB[(h % 2) * 64:(h % 2) * 64 + 64, h // 2, :], False, True))
                MM(specs, [smr + "a", smr + "b", vbr, "rb"] + ["qxT%d_%d" % (c, pt) for c in range(2)], [poAr, poBr])
                if n < 15:
                    pkv, pkvr = pf(2)
                    MM([(pkv[:, p * 256:(p + 1) * 256], kz[:, p * 128:(p + 1) * 128], vb[:, p * 256:(p + 1) * 256], True, True)
                        for p in range(2)], [kzr, vbr], [pkvr])
                    for p in range(2):
                        for j in range(2):
                            rows = slice(j * 64, (j + 1) * 64)
                            STT(RS[rows, p, :], RS[rows, p, :], CST[rows, C_GC + p:C_GC + p + 1],
                                pkv[rows, p * 256 + j * 128:p * 256 + (j + 1) * 128], ALU.mult, ALU.add,
                                ["rs", pkvr, "cst"], ["rs"])
                    CP(RB[:], RS[:], ["rs"], ["rb"])
                if after_rs is not None:
                    after_rs()
                for h in range(4):
                    S.op("dve", lambda e, o_=SM_[:, 40 + h * 6:46 + h * 6], i_=obank(h): e.bn_stats(out=o_, in_=i_),
                         [obr(h)], ["bst%d" % h])
                for h in range(4):
                    S.op("dve", lambda e, o_=SM_[:, 20 + h * 2:22 + h * 2], i_=SM_[:, 40 + h * 6:46 + h * 6]: e.bn_aggr(out=o_, in_=i_),
                         ["bst%d" % h], ["mv%d" % h])
                mv3 = SM_[:, 20:28].rearrange("p (h t) -> p h t", t=2)
                TS(SM_[:, 28:32], mv3[:, :, 1], EPS, None, ALU.add, None, ["mv%d" % h for h in range(4)], ["ve"])
                POW(SM_[:, 4:8], SM_[:, 28:32], ["ve"], ["grstd"])
                STT(SM_[:, 12:16], mv3[:, :, 0], -1.0, SM_[:, 4:8], ALU.mult, ALU.mult, ["mv%d" % h for h in range(4)] + ["grstd"], ["gnmr"])
                on, onr = ONR.get()
                for h in range(4):
                    ACT(on[:, h * 128:(h + 1) * 128], obank(h), AF.Identity, [obr(h), "grstd", "gnmr"], [onr + str(h)],
                        scale=SM_[:, 4 + h:5 + h], bias=SM_[:, 12 + h:13 + h])
                return {"on": on, "onr": onr, "sg": sg, "sgr": sgr, "tok": tok, "pt": pt}

            def ret_aa(ctx):
                aa, aar = AAR.get()
                TT(aa, ctx["on"], ctx["sg"], ALU.mult, [ctx["onr"] + str(h) for h in range(4)] + [ctx["sgr"]], [aar])
                ctx["aa"], ctx["aar"] = aa, aar

            def ret_tail(ctx):
                aa, aar, tok, pt = ctx["aa"], ctx["aar"], ctx["tok"], ctx["pt"]
                pa2, pa2r = pb()
                TR([(pa2[:, k4 * 128:(k4 + 1) * 128], aa[:, k4 * 128:(k4 + 1) * 128]) for k4 in range(4)], [aar, "idb"], [pa2r])
                ACT(ART[:, :, tok], pa2.rearrange("p (k n) -> p k n", n=128), AF.Copy, [pa2r], ["art%d" % pt])

            actx = ret_a(0)
            prev = None
            for i in range(8):
                holder = {}

                def after_rs(i=i, holder=holder):
                    if i + 1 < 8:
                        holder["a"] = ret_a(i + 1)

                mid = (lambda c=prev: ret_aa(c)) if prev is not None else None
                cur = ret_front(i, actx, mid_cb=mid, after_rs=after_rs)
                if prev is not None:
                    ret_tail(prev)
                prev = cur
                actx = holder.get("a")
            ret_aa(prev)
            ret_tail(prev)
            if l == 0 and hf == 0:
                DBG("ART", ART, ["art0", "art1"])

            PjR = Rot("pj", [1026], BF16)
            BjR = Rot("bj", [1024], BF16)
            cxR = Rot("cx", [512], F32)
            DGR = Rot("dg", [3, 128], BF16)
            def sc_front(j):
                W, wr = wblock(l, "sc%d" % j)
                pj, pjr = PjR.get()
                bj, bjr = BjR.get()
                if hf == 0:
                    MEMSET(pj[:, 0:2], 0.0, [pjr + "h"])
                else:
                    CP(pj[:, 0:2], PH[:, j, :], ["ph%d" % j], [pjr + "h"])
                dg, dgr = DGR.get()
                for k in range(3):
                    TS(dg[:, k, :], ident_f, PV[:, PV_SCW + j * 3 + k:PV_SCW + j * 3 + k + 1], None, ALU.mult, None,
                       ["cst", "pv"], [dgr + str(k)])
                for pt in range(2):
                    htr = ["ht%d_%d" % (kc, pt) for kc in range(8)]
                    ps3 = []
                    for b in range(3):
                        p, pr = pf()
                        MM([(p, W[:, kc, b * 128:(b + 1) * 128], HT[:, kc, pt * 512:(pt + 1) * 512], kc == 0, kc == 7)
                            for kc in range(8)], htr + [wr], [pr])
                        ps3.append((p, pr))
                    ACT(bj[:, pt * 512:(pt + 1) * 512], ps3[0][0], AF.Copy, [ps3[0][1]], [bjr + str(pt)])
                    cx, cxr = cxR.get()
                    ACT(cx, ps3[1][0], AF.Copy, [ps3[1][1]], [cxr])
                    TT(pj[:, 2 + pt * 512:2 + (pt + 1) * 512], ps3[2][0], cx, ALU.mult, [ps3[2][1], cxr], [pjr + str(pt)])
                CP(PH[:, j, :], pj[:, 1024:1026], [pjr + "1", pjr + "h"], ["ph%d" % j])
                return (j, pj, pjr, bj, bjr, dg, dgr)

            def sc_tail(ctx):
                j, pj, pjr, bj, bjr, dg, dgr = ctx
                for pt in range(2):
                    p, pr = pf()
                    MM([(p, dg[:, k, :], pj[:, pt * 512 + k:pt * 512 + k + 512], k == 0, k == 2) for k in range(3)],
                       [dgr + "0", dgr + "1", dgr + "2", pjr + "h", pjr + "0", pjr + "1"], [pr])
                    TT(AST[:, j, pt * 512:(pt + 1) * 512], p, bj[:, pt * 512:(pt + 1) * 512], ALU.mult, [pr, bjr + str(pt)], ["ast%d" % pt])

            sctx = sc_front(0)
            for j in range(4):
                nctx = sc_front(j + 1) if j + 1 < 4 else None
                sc_tail(sctx)
                sctx = nctx
            AR.pop()
            if l == 0 and hf == 0:
                DBG("AST", AST, ["ast0", "ast1"])

            AR.push()
            G = AR.alloc([4, 1054], BF16)
            CB = AR.alloc([4, 1024], BF16)
            CSQ = AR.alloc([4, 1024], BF16)
            DG31R = Rot("dg31", [31, 128], BF16)
            sbR = Rot("sb", [512], F32)
            for j in range(4):
                W, wr = wblock(l, "cf%d" % j)
                if hf == 0:
                    MEMSET(G[:, j, 0:30], 0.0, ["g%dh" % j])
                else:
                    CP(G[:, j, 0:30], GH[:, j, :], ["gh%d" % j], ["g%dh" % j])
                for pt in range(2):
                    htr = ["ht%d_%d" % (kc, pt) for kc in range(8)]
                    p2 = []
                    for b in range(2):
                        p, pr = pf()
                        MM([(p, W[:, kc, b * 128:(b + 1) * 128], HT[:, kc, pt * 512:(pt + 1) * 512], kc == 0, kc == 7)
                            for kc in range(8)], htr + [wr], [pr])
                        p2.append((p, pr))
                    sb, sbr = sbR.get()
                    ACT(sb, p2[1][0], AF.Sigmoid, [p2[1][1]], [sbr])
                    TT(G[:, j, 30 + pt * 512:30 + (pt + 1) * 512], p2[0][0], sb, ALU.mult, [p2[0][1], sbr], ["g%d_%d" % (j, pt)])
                CP(GH[:, j, :], G[:, j, 1024:1054], ["g%d_1" % j, "g%dh" % j], ["gh%d" % j])
            def dg31_build(j):
                dg, dgr = DG31R.get()
                for k in range(31):
                    sc_ap = PV[:, PV_CFW + j * 31 + k:PV_CFW + j * 31 + k + 1]
                    TS(dg[:, k, :], ident_f, sc_ap, None, ALU.mult, None, ["cst", "pv"], [dgr + "_%d" % k])
                return dg, dgr

            rowA = AR.alloc([512], F32)
            rowB = AR.alloc([512], F32)
            rowC = AR.alloc([512], F32)
            rowD = AR.alloc([512], F32)
            tR = Rot("lnt", [512], F32)
            t2R_ = Rot("lnt2", [512], F32)
            bc = {}

            def ln_chain(pt):
                sl = slice(pt * 512, (pt + 1) * 512)
                pm, pmr = pf(0)
                MM([(pm[0:1, :], ONEC[:, 0:1], CB[:, j, sl], j == 0, j == 3) for j in range(4)],
                   ["onec"] + ["cb%d_%d" % (j, pt) for j in range(4)], [pmr])
                pe2, pe2r = pf(1)
                MM([(pe2[0:1, :], ONEC[:, 0:1], CSQ[:, j, sl], j == 0, j == 3) for j in range(4)],
                   ["onec"] + ["csq%d_%d" % (j, pt) for j in range(4)], [pe2r])
                ACT(rowA[0:1, :], pm[0:1, :], AF.Copy, [pmr], ["rowA"])
                ACT(rowB[0:1, :], pm[0:1, :], AF.Square, [pmr], ["rowB"])
                STT(rowB[0:1, :], pe2[0:1, :], EPS, rowB[0:1, :], ALU.add, ALU.subtract, [pe2r, "rowB"], ["rowB"])
                POW(rowC[0:1, :], rowB[0:1, :], ["rowB"], ["rowC"])
                STT(rowD[0:1, :], rowA[0:1, :], -1.0, rowC[0:1, :], ALU.mult, ALU.mult, ["rowA", "rowC"], ["rowD"])
                pr_, prr = pf(2 + 2 * pt)
                MM([(pr_, ONER[0:1, :], rowC[0:1, :], True, True)], ["oner", "rowC"], [prr])
                pn_, pnr = pf(3 + 2 * pt)
                MM([(pn_, ONER[0:1, :], rowD[0:1, :], True, True)], ["oner", "rowD"], [pnr])
                bc[pt] = (pr_, prr, pn_, pnr)

            def ln_norm(pt):
                sl = slice(pt * 512, (pt + 1) * 512)
                pr_, prr, pn_, pnr = bc[pt]
                for j in range(4):
                    t, tr = tR.get()
                    TT(t, pr_, CB[:, j, sl], ALU.mult, [prr, "cb%d_%d" % (j, pt)], [tr])
                    t2, t2r = t2R_.get()
                    TT(t2, pn_, t, ALU.add, [pnr, tr], [t2r])
                    ACT(ACFT[:, j, sl], t2, AF.Silu, [t2r, "pv"], ["acft%d" % pt],
                        scale=PV[:, PV_LNG + j:PV_LNG + j + 1], bias=PV[:, PV_LNB + j:PV_LNB + j + 1])

            dgc = dg31_build(0)
            for j in range(4):
                dgn = dg31_build(j + 1) if j + 1 < 4 else None
                dg, dgr = dgc
                for pt in range(2):
                    p, pr = pf(0) if (j == 3 and pt == 1) else pf()
                    MM([(p, dg[:, k, :], G[:, j, pt * 512 + k:pt * 512 + k + 512], k == 0, k == 30) for k in range(31)],
                       [dgr + "_%d" % k for k in range(31)] + ["g%dh" % j, "g%d_0" % j, "g%d_1" % j], [pr])
                    bias = PV[:, PV_CFB + j:PV_CFB + j + 1]
                    ACT(CB[:, j, pt * 512:(pt + 1) * 512], p, AF.Identity, [pr, "pv"], ["cb%d_%d" % (j, pt)], bias=bias)
                    ACT(CSQ[:, j, pt * 512:(pt + 1) * 512], p, AF.Square, [pr, "pv"], ["csq%d_%d" % (j, pt)], bias=bias)
                    if j == 3 and pt == 0:
                        ln_chain(0)
                dgc = dgn
            ln_chain(1)
            ln_norm(0)
            ln_norm(1)
            AR.pop()
            if l == 0 and hf == 0:
                DBG("ACFT", ACFT, ["acft0", "acft1"])

            AR.push()
            MT = AR.alloc([8, 1024], BF16)
            sgR = Rot("sgm", [512], F32, 3)
            mR = Rot("mm", [512], F32, 2)
            tmR = Rot("tm", [512], F32, 2)
            AB = [ART, AST, ACFT]
            ABn = ["art", "ast", "acft"]
            for f in range(8):
                Wgb, wgr_ = wblock(l, "gb%d" % f)
                wbr_ = wgr_
                Wg_ = Wgb[:, 0, 0:3072].rearrange("p (k n) -> p k n", n=384)
                Wb_ = Wgb[:, 0, 3072:4608].rearrange("p (k n) -> p k n", n=384)
                for pt in range(2):
                    sl = slice(pt * 512, (pt + 1) * 512)
                    htr = ["ht%d_%d" % (kc, pt) for kc in range(8)]
                    m, mr = mR.get()
                    for b in range(3):
                        pg_, pgr_ = pf()
                        MM([(pg_, Wg_[:, kc, b * 128:(b + 1) * 128], HT[:, kc, sl], kc == 0, kc == 7) for kc in range(8)],
                           htr + [wgr_], [pgr_])
                        py_, pyr_ = pf()
                        MM([(py_, Wb_[:, k4, b * 128:(b + 1) * 128], AB[b][:, k4, sl], k4 == 0, k4 == 3) for k4 in range(4)],
                           [ABn[b] + str(pt), wbr_], [pyr_])
                        sgm, sgmr = sgR.get()
                        ACT(sgm, pg_, AF.Sigmoid, [pgr_], [sgmr])
                        if b == 0:
                            TT(m, py_, sgm, ALU.mult, [pyr_, sgmr], [mr])
                        else:
                            tm, tmr = tmR.get()
                            TT(tm, py_, sgm, ALU.mult, [pyr_, sgmr], [tmr])
                            if b == 1:
                                TT(m, m, tm, ALU.add, [mr, tmr], [mr])
                            else:
                                TT(MT[:, f, sl], m, tm, ALU.add, [mr, tmr], ["mt%d" % pt])
            if l == 0 and hf == 0:
                DBG("MT", MT, ["mt0", "mt1"])

            S.dma("sp", lambda e, l=l: e.dma_start(out=GBC[:], in_=gbc_d[l, 0]), "gbc", writes=["gbc"])
            Wo0, wo0r = wblock(l, "wo0")
            Wo1, wo1r = wblock(l, "wo1")
            tmpR = {"junk": AR.alloc([512], BF16), "t": Rot("pnt", [512], F32, 2)}
            for i in range(8):
                tt = hf * 8 + i
                pt = i // 4
                tok = slice(i * 128, (i + 1) * 128)
                Y, Yr = [], []
                for h, (Wo, wor) in enumerate(((Wo0, wo0r), (Wo1, wo1r))):
                    p, pr = pf()
                    MM([(p, MT[:, kc, tok], Wo[:, kc, :], kc == 0, kc == 7) for kc in range(8)], ["mt%d" % pt, wor], [pr])
                    Y.append(p)
                    Yr.append(pr)
                postnorm(tt, Y, Yr, tmpR)
            AR.pop()
            AR.pop()
            if l == 0 and hf == 0:
                DBG("X1", X[:, 0:8, :], ["x%d" % t for t in range(8)])

        AR.push()
        WD = AR.alloc([NFF, 1024], BF16)
        ACTT = AR.alloc([NFF, 512], BF16)
        HN2 = AR.alloc([2, 1024], BF16)
        UGR = Rot("ug", [514], BF16)
        UVR = Rot("uv", [514], BF16)
        DGF = Rot("dgf", [6, 128], BF16)
        slR = Rot("sl", [512], BF16)
        tmpR = {"junk": AR.alloc([512], BF16), "t": Rot("pnt", [512], F32, 2)}
        S.dma("sp", lambda e, l=l: e.dma_start(out=GBC[:], in_=gbc_d[l, 1]), "gbc", writes=["gbc"])
        wd_issued = 0

        def ffn_stats(q):
            sl_ = (q % 2) * 16
            for a in range(4):
                tt = q * 4 + a
                ACT(JUNK[:], X[:, tt, :], AF.Square, ["x%d" % tt], ["junkp", "fss%d" % (q % 2)],
                    accum_out=PST[:, sl_ + a:sl_ + a + 1])
            TS(PST[:, sl_ + 4:sl_ + 8], PST[:, sl_:sl_ + 4], 1.0 / D, EPS, ALU.mult, ALU.add, ["fss%d" % (q % 2)], ["fms%d" % (q % 2)])
            POW(PST[:, sl_ + 8:sl_ + 12], PST[:, sl_ + 4:sl_ + 8], ["fms%d" % (q % 2)], ["frs%d" % (q % 2)])

        def ffn_apply(q):
            sl_ = (q % 2) * 16
            g = q % 2
            for pair in range(2):
                for a2 in range(2):
                    a = pair * 2 + a2
                    tt = q * 4 + a
                    TS(HN2[:, a2, :], X[:, tt, :], PST[:, sl_ + 8 + a:sl_ + 9 + a], None, ALU.mult, None,
                       ["x%d" % tt, "frs%d" % (q % 2)], ["hn2_%d" % a2])
                for kc in range(8):
                    p, pr = pb()
                    TR([(p[:, a2 * 128:(a2 + 1) * 128], HN2[:, a2, kc * 128:(kc + 1) * 128]) for a2 in range(2)],
                       ["hn2_0", "hn2_1", "idb"], [pr])
                    dst = HT[:, kc, g * 512 + pair * 256:g * 512 + (pair + 1) * 256]
                    res = ["ht%d_%d_%d" % (kc, g, pair)]
                    if kc % 2 == 0:
                        ACT(dst, p[:, 0:256], AF.Copy, [pr, "pv"], res, scale=PV[:, PV_FFNPRE + kc:PV_FFNPRE + kc + 1])
                    else:
                        TS(dst, p[:, 0:256], PV[:, PV_FFNPRE + kc:PV_FFNPRE + kc + 1], None, ALU.mult, None, [pr, "pv"], res)

        ffn_stats(0)
        ffn_apply(0)
        for qt in range(4):
            hg = qt % 2
            HTq = HT[:, :, hg * 512:(hg + 1) * 512]
            htr = ["ht%d_%d_%d" % (kc, hg, pr_) for kc in range(8) for pr_ in range(2)]
            def up_front(j):
                W, wr = wblock(l, "up%d" % j)
                ug, ugr = UGR.get()
                uv, uvr = UVR.get()
                if qt == 0:
                    MEMSET(ug[:, 0:2], 0.0, [ugr + "h"])
                    MEMSET(uv[:, 0:2], 0.0, [uvr + "h"])
                else:
                    CP(ug[:, 0:2], FH[:, j, 0:2], ["fh%d" % j], [ugr + "h"])
                    CP(uv[:, 0:2], FH[:, j, 2:4], ["fh%d" % j], [uvr + "h"])
                dg, dgr = DGF.get()
                for k in range(3):
                    TS(dg[:, k, :], ident_f, PV[:, PV_FGW + j * 3 + k:PV_FGW + j * 3 + k + 1], None, ALU.mult, None, ["cst", "pv"], [dgr + str(k)])
                    TS(dg[:, 3 + k, :], ident_f, PV[:, PV_FVW + j * 3 + k:PV_FVW + j * 3 + k + 1], None, ALU.mult, None, ["cst", "pv"], [dgr + str(3 + k)])
                pg_, pgr_ = pf()
                MM([(pg_, W[:, kc, 0:128], HTq[:, kc, :], kc == 0, kc == 7) for kc in range(8)], htr + [wr], [pgr_])
                pv2, pv2r = pf()
                MM([(pv2, W[:, kc, 128:256], HTq[:, kc, :], kc == 0, kc == 7) for kc in range(8)], htr + [wr], [pv2r])
                ACT(ug[:, 2:514], pg_, AF.Copy, [pgr_], [ugr])
                ACT(uv[:, 2:514], pv2, AF.Copy, [pv2r], [uvr])
                CP(FH[:, j, 0:2], ug[:, 512:514], [ugr, ugr + "h"], ["fh%d" % j])
                CP(FH[:, j, 2:4], uv[:, 512:514], [uvr, uvr + "h"], ["fh%d" % j])
                return (j, ug, ugr, uv, uvr, dg, dgr)

            def up_tail(ctx):
                j, ug, ugr, uv, uvr, dg, dgr = ctx
                pcg, pcgr = pf()
                MM([(pcg, dg[:, k, :], ug[:, k:k + 512], k == 0, k == 2) for k in range(3)],
                   [dgr + "0", dgr + "1", dgr + "2", ugr, ugr + "h"], [pcgr])
                pcv, pcvr = pf()
                MM([(pcv, dg[:, 3 + k, :], uv[:, k:k + 512], k == 0, k == 2) for k in range(3)],
                   [dgr + "3", dgr + "4", dgr + "5", uvr, uvr + "h"], [pcvr])
                sl_, slr = slR.get()
                ACT(sl_, pcg, AF.Silu, [pcgr], [slr])
                TT(ACTT[:, j, :], pcv, sl_, ALU.mult, [pcvr, slr], ["actt%d" % j])

            def wd_maybe(j):
                nonlocal wd_issued
                if qt == 0 and j % 2 == 1 and wd_issued < 11:
                    c = wd_issued
                    o, kc_, n_ = offs["wd%d" % c]
                    S.dma("pool", lambda e, o_=WD[:, 2 * c:2 * c + 2, :].rearrange("p k (a b) -> p (k a) b", b=512),
                          i_=wst_d[l, :, o:o + 2048].rearrange("p (a b) -> p a b", b=512): e.dma_start(out=o_, in_=i_),
                          "wd", writes=["wd"])
                    wd_issued += 1

            uctx = up_front(0)
            for j in range(NFF):
                wd_maybe(j)
                if qt < 3 and j == 6:
                    ffn_stats(qt + 1)
                if qt < 3 and j == 13:
                    ffn_apply(qt + 1)
                nctx = up_front(j + 1) if j + 1 < NFF else None
                up_tail(uctx)
                uctx = nctx
            if l == 0 and qt == 0:
                DBG("ACTT", ACTT, ["actt%d" % j for j in range(NFF)])
            for a in range(4):
                tt = qt * 4 + a
                tok = slice(a * 128, (a + 1) * 128)
                Y, Yr = [], []
                for h in range(2):
                    p, pr = pf()
                    MM([(p, ACTT[:, kc, tok], WD[:, kc, h * 512:(h + 1) * 512], kc == 0, kc == NFF - 1) for kc in range(NFF)],
                       ["actt%d" % kc for kc in range(NFF)] + ["wd"], [pr])
                    Y.append(p)
                    Yr.append(pr)
                postnorm(tt, Y, Yr, tmpR)
            if l == n_layers - 1:
                S.dma("sp", lambda e, qt=qt: e.dma_start(
                    out=out_d[qt * 512:(qt + 1) * 512, :].rearrange("(a p) d -> p a d", p=128),
                    in_=X[:, qt * 4:(qt + 1) * 4, :]), "out%d" % qt, reads=["x%d" % (qt * 4 + a) for a in range(4)])
        AR.pop()
    S.barrier()
    S.emit()
    return nc, dbg_d, S, AR


_CACHE = {}


def kernel(**inputs):
    x = np.asarray(inputs["x"], np.float32)
    B = x.shape[0]
    wst = np.stack([prep_layer_stream(inputs, l) for l in range(NL)], axis=0)
    pv = np.stack([prep_pv(inputs, l) for l in range(NL)], axis=0)
    gbc = np.stack([np.stack([np.broadcast_to(np.asarray(inputs["norm_mix_post"][l], np.float32)[None, :], (128, D)),
                              np.broadcast_to(np.asarray(inputs["norm_ffn_post"][l], np.float32)[None, :], (128, D))], axis=0)
                    for l in range(NL)], axis=0)
    gbc = np.ascontiguousarray(gbc)
    cst, rope = make_consts()
    nc = build()[0]
    in_maps = [{"x": np.ascontiguousarray(x[b]), "wst": wst, "pv": pv, "gbc": gbc, "cst": cst, "rope": rope} for b in range(B)]
    res = run_bass_kernel_spmd(nc, in_maps, core_ids=list(range(B)))
    return np.stack([np.asarray(r["out"], np.float32) for r in res.results], axis=0)
```

```python
import contextlib
import numpy as np
import concourse.bass as bass
import concourse.mybir as mybir
from concourse.bass_utils import run_bass_kernel_spmd

F32 = mybir.dt.float32
BF16 = mybir.dt.bfloat16
AF = mybir.ActivationFunctionType
ALU = mybir.AluOpType

D = 1024
SEQ = 2048
NL = 2
NFF = 22
EPS = 1e-6
NSLOT = 3
SLOTC = 4608
ENGS = ("pe", "act", "dve", "pool", "sp")

PV_MIXPRE, PV_FFNPRE, PV_SCW, PV_CFW, PV_CFB, PV_LNG, PV_LNB, PV_FGW, PV_FVW = 0, 8, 16, 28, 152, 156, 160, 164, 230
NPV = 296
C_ID, C_DM, C_XI, C_ZS, C_GC = 0, 128, 640, 896, 1152
NCST = 1154


class Sched:
    def __init__(self, nc, self_sync=("act", "dve", "pool")):
        self.nc = nc
        self.prog = {e: [] for e in ENGS}
        self.cnt = {e: 0 for e in ENGS}
        self.waited = {}
        self.lastw = {}
        self.readers = {}
        self.self_sync = set(self_sync)
        self.dma_sems = {}
        self.n_instr = 0

    def _need(self, eng, reads, writes, skip=None):
        need = {}

        def add(dep):
            if dep is None:
                return
            d, v = dep
            if need.get(d, 0) < v:
                need[d] = v

        for r in reads:
            add(self.lastw.get(r))
            if r.startswith("pf") or r.startswith("pb"):
                for d, v in self.readers.get(r, {}).items():
                    if d != eng:
                        add((d, v))
        for w in writes:
            add(self.lastw.get(w))
            for d, v in self.readers.get(w, {}).items():
                add((d, v))
        out = []
        for d, v in need.items():
            if d == skip:
                continue
            if d == eng and eng not in self.self_sync:
                continue
            key = (eng, d)
            if self.waited.get(key, 0) >= v:
                continue
            self.waited[key] = v
            out.append((d, v))
        return out

    def _mark(self, who, idx, reads, writes):
        for r in reads:
            self.readers.setdefault(r, {})[who] = idx
        for w in writes:
            self.lastw[w] = (who, idx)
            self.readers[w] = {}

    def op(self, eng, fn, reads=(), writes=()):
        waits = self._need(eng, reads, writes)
        self.cnt[eng] += 1
        self.prog[eng].append((waits, fn, ("eng", eng, 1)))
        self._mark(eng, self.cnt[eng], reads, writes)
        self.n_instr += 1

    def pe(self, fns, reads=(), writes=()):
        waits = self._need("pe", reads, writes)
        self.cnt["pe"] += 1
        n = len(fns)
        for i, fn in enumerate(fns):
            self.prog["pe"].append((waits if i == 0 else [], fn, ("eng", "pe", 1) if i == n - 1 else None))
        self._mark("pe", self.cnt["pe"], reads, writes)
        self.n_instr += n

    def dma(self, queue, fn, semname, reads=(), writes=()):
        d = "dma:" + semname
        waits = self._need(queue, reads, writes, skip=d)
        c = self.dma_sems.setdefault(semname, [0])
        c[0] += 16
        self.prog[queue].append((waits, fn, ("dma", semname, 16)))
        self._mark(d, c[0], reads, writes)
        self.n_instr += 1

    def barrier(self):
        snap = dict(self.cnt)
        dsnap = {"dma:" + k: v[0] for k, v in self.dma_sems.items()}
        for e in ENGS:
            if e == "pe":
                continue
            waits = []
            for d, v in list(snap.items()) + list(dsnap.items()):
                if d != e and v > 0 and self.waited.get((e, d), 0) < v:
                    self.waited[(e, d)] = v
                    waits.append((d, v))
            if waits:
                self.prog[e].append((waits, None, None))

    def emit(self):
        nc = self.nc
        with nc.cleanup_on_exit():
            sems = {}
            for e in ENGS:
                sems[e] = nc.alloc_semaphore(name="s_" + e)
            for k in self.dma_sems:
                sems["dma:" + k] = nc.alloc_semaphore(name="d_" + k)
            for s_ in sems.values():
                nc.gpsimd.sem_clear(s_)
            nc.all_engine_barrier()
            with nc.Block() as block:

                def run(eng_name):
                    def body(eng):
                        for waits, fn, inc in self.prog[eng_name]:
                            for d, v in waits:
                                eng.wait_ge(sems[d], v)
                            if fn is None:
                                continue
                            ins = fn(eng)
                            if inc is not None:
                                kind, name, n = inc
                                ins.then_inc(sems[name] if kind == "eng" else sems["dma:" + name], n)
                    return body

                block.tensor(run("pe"))
                block.scalar(run("act"))
                block.vector(run("dve"))
                block.gpsimd(run("pool"))
                block.sync(run("sp"))


def _blk(w):
    K, n = w.shape
    return np.ascontiguousarray(w.reshape(K // 128, 128, n).transpose(1, 0, 2)).reshape(128, -1)


def weight_blocks():
    blocks = [("q", 8, 512), ("k", 8, 512), ("v", 8, 512), ("g", 8, 512)]
    blocks += [("sc%d" % j, 8, 384) for j in range(4)]
    blocks += [("cf%d" % j, 8, 256) for j in range(4)]
    blocks += [("gb%d" % f, 1, 4608) for f in range(8)]
    blocks += [("wo0", 8, 512), ("wo1", 8, 512)]
    blocks += [("up%d" % j, 8, 256) for j in range(NFF)]
    blocks += [("wd%d" % c, 2, 1024) for c in range(11)]
    offs = {}
    o = 0
    for name, kc, n in blocks:
        offs[name] = (o, kc, n)
        o += kc * n
    return blocks, offs, o


def prep_layer_stream(inp, l):
    w_in = np.asarray(inp["w_in"][l], dtype=np.float32)
    perm = np.array([h * 64 + (i + 32) % 64 for h in range(4) for i in range(64)])
    q = w_in[:, 0:256]
    k = w_in[:, 256:512]
    parts = {}
    parts["q"] = np.concatenate([q, q[:, perm]], axis=1)
    parts["k"] = np.concatenate([k, k[:, perm]], axis=1)
    parts["v"] = w_in[:, 512:1024]
    parts["g"] = w_in[:, 1024:1536]
    for j in range(4):
        s = slice(j * 128, (j + 1) * 128)
        parts["sc%d" % j] = np.concatenate([w_in[:, 1536:2048][:, s], w_in[:, 2048:2560][:, s], w_in[:, 2560:3072][:, s]], axis=1)
        parts["cf%d" % j] = np.concatenate([w_in[:, 3072:3584][:, s], w_in[:, 3584:4096][:, s]], axis=1)
    wro = np.asarray(inp["w_ret_out"][l], np.float32)
    wso = np.asarray(inp["w_sc_out"][l], np.float32)
    wco = np.asarray(inp["w_cf_out"][l], np.float32)
    for f in range(8):
        s = slice(f * 128, (f + 1) * 128)
        gt = np.concatenate([w_in[:, 4096 + b * 1024: 4096 + (b + 1) * 1024][:, s] for b in range(3)], axis=1)
        bo = np.concatenate([wro[:, s], wso[:, s], wco[:, s]], axis=1)
        parts["gb%d" % f] = np.concatenate([_blk(gt), _blk(bo)], axis=1)
    wo = np.asarray(inp["w_o"][l], np.float32)
    parts["wo0"] = wo[:, 0:512]
    parts["wo1"] = wo[:, 512:1024]
    wup = np.asarray(inp["w_up"][l], np.float32)
    for j in range(NFF):
        s = slice(j * 128, (j + 1) * 128)
        parts["up%d" % j] = np.concatenate([wup[:, :2816][:, s], wup[:, 2816:][:, s]], axis=1)
    wd = np.asarray(inp["w_down"][l], np.float32)
    for c in range(11):
        parts["wd%d" % c] = wd[c * 256:(c + 1) * 256, :]
    blocks, offs, total = weight_blocks()
    out = np.empty((128, total), np.float32)
    for name, kc, n in blocks:
        o = offs[name][0]
        out[:, o:o + kc * n] = parts[name] if name.startswith("gb") else _blk(parts[name])
    return out


def prep_pv(inp, l):
    pv = np.zeros((128, NPV), np.float32)

    def fm(vec, nchunk):
        return np.asarray(vec, np.float32).reshape(nchunk, 128).T

    pv[:, PV_MIXPRE:PV_MIXPRE + 8] = fm(inp["norm_mix_pre"][l], 8)
    pv[:, PV_FFNPRE:PV_FFNPRE + 8] = fm(inp["norm_ffn_pre"][l], 8)
    scw = np.asarray(inp["sc_conv_w"][l], np.float32)
    pv[:, PV_SCW:PV_SCW + 12] = scw.reshape(3, 4, 128).transpose(2, 1, 0).reshape(128, 12)
    cfw = np.asarray(inp["cf_conv_w"][l], np.float32)
    pv[:, PV_CFW:PV_CFW + 124] = cfw.reshape(31, 4, 128).transpose(2, 1, 0).reshape(128, 124)
    pv[:, PV_CFB:PV_CFB + 4] = fm(inp["cf_conv_b"][l], 4)
    pv[:, PV_LNG:PV_LNG + 4] = fm(inp["cf_ln_g"][l], 4)
    pv[:, PV_LNB:PV_LNB + 4] = fm(inp["cf_ln_b"][l], 4)
    fw = np.asarray(inp["ffn_conv_w"][l], np.float32)
    pv[:, PV_FGW:PV_FGW + 66] = fw[:, :2816].reshape(3, NFF, 128).transpose(2, 1, 0).reshape(128, 66)
    pv[:, PV_FVW:PV_FVW + 66] = fw[:, 2816:].reshape(3, NFF, 128).transpose(2, 1, 0).reshape(128, 66)
    return pv


def make_consts():
    cst = np.zeros((128, NCST), np.float64)
    cst[:, C_ID:C_ID + 128] = np.eye(128)
    gam = 1.0 - np.exp2(-5.0 - np.arange(4))
    m = np.arange(128)[:, None]
    c = np.arange(128)[None, :]
    for h in range(4):
        dm = np.where(c >= m, gam[h] ** np.maximum(c - m, 0), 0.0) * 0.125
        sl_ = [0, 2, 1, 3][h]
        cst[:, C_DM + sl_ * 128:C_DM + (sl_ + 1) * 128] = dm
        cst[:, C_ZS + h * 64:C_ZS + (h + 1) * 64] = (0.125 * gam[h] ** (127 - np.arange(128)))[:, None]
    p = np.arange(128)
    for ch in range(2):
        hh = ch * 2 + p // 64
        xi = gam[hh][:, None] ** (np.arange(128)[None, :] + 1.0)
        cst[:, C_XI + ch * 128:C_XI + (ch + 1) * 128] = xi
        cst[:, C_GC + ch] = gam[hh] ** 128
    inv = 10000.0 ** (-np.arange(32) / 32.0)
    t = np.arange(SEQ)[None, :]
    ang = t * inv[p % 32][:, None]
    sign = np.where((p % 64) < 32, -1.0, 1.0)[:, None]
    rope = np.stack([np.cos(ang), sign * np.sin(ang)], axis=0)
    return cst.astype(np.float32), rope.astype(np.float32)


class Arena:
    def __init__(self, nc, S, nbytes):
        self.t = nc.alloc_sbuf_tensor("arena", [128, nbytes // 2], BF16)
        self.top = 0
        self.cap = nbytes
        self.stack = []
        self.S = S
        self.peak = 0

    def push(self):
        self.stack.append(self.top)

    def pop(self):
        self.S.barrier()
        self.top = self.stack.pop()

    def alloc(self, shape, dtype):
        n = int(np.prod(shape))
        esz = 4 if dtype == F32 else 2
        nb = (n * esz + 63) // 64 * 64
        off = self.top
        self.top += nb
        self.peak = max(self.peak, self.top)
        assert self.top <= self.cap, ("arena overflow", self.top, self.cap)
        ap = self.t[:, off // 2: off // 2 + (n * esz) // 2]
        if dtype == F32:
            ap = ap.bitcast(F32)
        if len(shape) == 2:
            names = "a b"
        else:
            names = "a b c"
        if len(shape) == 1:
            return ap
        kw = {"a": shape[0]} if len(shape) == 2 else {"a": shape[0], "b": shape[1]}
        return ap.rearrange("p (%s) -> p %s" % (names, names), **kw)


def build(n_layers=NL, dbg=()):
    nc = bass.Bass("TRN2", target_bir_lowering=False)
    blocks, offs, WCOLS = weight_blocks()
    x_d = nc.dram_tensor("x", [SEQ, D], F32, kind="ExternalInput").ap()
    wst_d = nc.dram_tensor("wst", [n_layers, 128, WCOLS], F32, kind="ExternalInput").ap()
    pv_d = nc.dram_tensor("pv", [n_layers, 128, NPV], F32, kind="ExternalInput").ap()
    gbc_d = nc.dram_tensor("gbc", [n_layers, 2, 128, D], F32, kind="ExternalInput").ap()
    cst_d = nc.dram_tensor("cst", [128, NCST], F32, kind="ExternalInput").ap()
    rope_d = nc.dram_tensor("rope", [2, 128, SEQ], F32, kind="ExternalInput").ap()
    out_d = nc.dram_tensor("out", [SEQ, D], F32, kind="ExternalOutput").ap()
    dbg_d = {}

    S = Sched(nc)

    X = nc.alloc_sbuf_tensor("X", [128, 16, D], F32)
    HT = nc.alloc_sbuf_tensor("HT", [128, 8, 1024], BF16)
    RING = nc.alloc_sbuf_tensor("RING", [128, NSLOT, SLOTC], BF16)
    CST = nc.alloc_sbuf_tensor("CST", [128, NCST], F32)
    PV = nc.alloc_sbuf_tensor("PVt", [128, NPV], F32)
    GBC = nc.alloc_sbuf_tensor("GBC", [128, D], F32)
    IDB = nc.alloc_sbuf_tensor("IDB", [128, 128], BF16)
    ONEC = nc.alloc_sbuf_tensor("ONEC", [128, 2], BF16)
    ONER = nc.alloc_sbuf_tensor("ONER", [1, 128], F32)
    RS = nc.alloc_sbuf_tensor("RS", [128, 2, 128], F32)
    RB = nc.alloc_sbuf_tensor("RB", [128, 2, 128], BF16)
    PH = nc.alloc_sbuf_tensor("PH", [128, 4, 2], BF16)
    GH = nc.alloc_sbuf_tensor("GH", [128, 4, 30], BF16)
    FH = nc.alloc_sbuf_tensor("FH", [128, NFF, 4], BF16)
    SM_ = nc.alloc_sbuf_tensor("SMALL", [128, 64], F32)
    PST = nc.alloc_sbuf_tensor("PST", [128, 32], F32)
    MST = nc.alloc_sbuf_tensor("MST", [128, 48], F32)
    JUNK = nc.alloc_sbuf_tensor("JUNK", [128, 1024], BF16)
    PSF = nc.alloc_psum_tensor("PSF", [128, 6, 512], F32)
    PSB = nc.alloc_psum_tensor("PSB", [128, 2, 1024], BF16)
    AR = Arena(nc, S, 85 * 1024)

    ident_f = CST[:, C_ID:C_ID + 128]
    dmask = CST[:, C_DM:C_DM + 512]
    zsfull = CST[:, C_ZS:C_ZS + 256]

    st = {"pf": 0, "pb": 0, "blk": 0, "issued": 0}
    stream = []
    for l in range(n_layers):
        for hf in range(2):
            for name, kc, n in blocks:
                if not (name.startswith("up") or name.startswith("wd")):
                    stream.append((l, name))
        for qt in range(4):
            for j in range(NFF):
                stream.append((l, "up%d" % j))

    def pf(fixed=None):
        if fixed is not None:
            return PSF[:, fixed, :], "pf%d" % fixed
        b = st["pf"] % 6
        st["pf"] += 1
        return PSF[:, b, :], "pf%d" % b

    def pb():
        b = st["pb"] % 2
        st["pb"] += 1
        return PSB[:, b, 0:512], "pb%d" % b

    def issue_block(i):
        l, name = stream[i]
        o, kc, n = offs[name]
        s = i % NSLOT
        cols = kc * n
        S.dma("pool", lambda e: e.dma_start(
            out=RING[:, s, 0:cols].rearrange("p (a b) -> p a b", b=512),
            in_=wst_d[l, :, o:o + cols].rearrange("p (a b) -> p a b", b=512)),
            "ring%d" % s, writes=["ring%d" % s])

    def wblock(l, name):
        i = st["blk"]
        assert stream[i] == (l, name), (stream[i], l, name)
        st["blk"] += 1
        while st["issued"] <= min(i + 1, len(stream) - 1):
            issue_block(st["issued"])
            st["issued"] += 1
        o, kc, n = offs[name]
        s = i % NSLOT
        return RING[:, s, 0:kc * n].rearrange("p (k n) -> p k n", n=n), "ring%d" % s

    class Rot:
        def __init__(self, name, shape, dtype, n=2):
            self.bufs = [AR.alloc(shape, dtype) for _ in range(n)]
            self.name = name
            self.i = 0

        def get(self):
            k = self.i % len(self.bufs)
            self.i += 1
            return self.bufs[k], "%s#%d" % (self.name, k)

    def ACT(out, in_, func, r, w, **kw):
        S.op("act", lambda e: e.activation(out=out, in_=in_, func=func, **kw), r, w)

    def TT(out, in0, in1, op, r, w):
        S.op("dve", lambda e: e.tensor_tensor(out=out, in0=in0, in1=in1, op=op), r, w)

    def TS(out, in0, s1, s2, op0, op1, r, w):
        if op1 is None:
            S.op("dve", lambda e: e.tensor_scalar(out=out, in0=in0, scalar1=s1, scalar2=None, op0=op0), r, w)
        else:
            S.op("dve", lambda e: e.tensor_scalar(out=out, in0=in0, scalar1=s1, scalar2=s2, op0=op0, op1=op1), r, w)

    def STT(out, in0, sc, in1, op0, op1, r, w):
        S.op("dve", lambda e: e.scalar_tensor_tensor(out=out, in0=in0, scalar=sc, in1=in1, op0=op0, op1=op1), r, w)

    def CP(out, in_, r, w):
        S.op("dve", lambda e: e.tensor_copy(out=out, in_=in_), r, w)

    def MM(specs, r, w):
        S.pe([(lambda e, o=o, a=a, b=b, s0=s0, s1=s1: e.matmul(o, lhsT=a, rhs=b, start=s0, stop=s1))
              for (o, a, b, s0, s1) in specs], r, w)

    def TR(specs, r, w):
        S.pe([(lambda e, o=o, a=a: e.transpose(out=o, in_=a, identity=IDB[:])) for (o, a) in specs], r, w)

    def POW(out, in0, r, w):
        tag = w[0] + "~sq"
        S.op("act", lambda e: e.activation(out=out, in_=in0, func=AF.Sqrt), r, [tag])
        S.op("dve", lambda e: e.reciprocal(out=out, in_=out), [tag], w)

    def MEMSET(ap, val, w):
        S.op("dve", lambda e: e.memset(ap, val), [], w)

    def DBG(name, ap, reads):
        if name not in dbg:
            return
        shape = list(ap.shape)
        d = nc.dram_tensor("dbg_" + name, shape, ap.dtype, kind="ExternalOutput").ap()
        dbg_d[name] = d
        S.dma("sp", lambda e: e.dma_start(out=d, in_=ap), "dbg_" + name, reads=reads)

    S.dma("sp", lambda e: e.dma_start(out=CST[:], in_=cst_d), "cst", writes=["cst"])
    for q4 in range(4):
        S.dma("sp", lambda e, q4=q4: e.dma_start(
            out=X[:, q4 * 4:(q4 + 1) * 4, :],
            in_=x_d[q4 * 512:(q4 + 1) * 512, :].rearrange("(a p) d -> p a d", p=128)),
            "x%d" % q4, writes=["x%d" % (q4 * 4 + a) for a in range(4)])
    CP(IDB[:], ident_f, ["cst"], ["idb"])
    MEMSET(ONEC[:], 1.0 / 512.0, ["onec"])
    MEMSET(ONER[:], 1.0, ["oner"])

    def prenorm_stats(tiles, slot):
        for g4 in range(2):
            so = slot * 24 + g4 * 4
            tg = "%d_%d" % (slot, g4)
            for i in range(4):
                tt = tiles[g4 * 4 + i]
                ACT(JUNK[:], X[:, tt, :], AF.Square, ["x%d" % tt], ["junkp", "mss" + tg], accum_out=MST[:, so + i:so + i + 1])
            TS(MST[:, so + 8:so + 12], MST[:, so:so + 4], 1.0 / D, EPS, ALU.mult, ALU.add, ["mss" + tg], ["mms" + tg])
            POW(MST[:, so + 16:so + 20], MST[:, so + 8:so + 12], ["mms" + tg], ["mrs" + tg])

    def prenorm(l, tiles, gcol, HTv, hn_alloc, hn_res, slot):
        for g4 in range(2):
            so = slot * 24 + g4 * 4
            tg = "%d_%d" % (slot, g4)
            for i in range(g4 * 4, g4 * 4 + 4):
                tt = tiles[i]
                TS(hn_alloc[:, i, :], X[:, tt, :], MST[:, so + 16 + (i - g4 * 4):so + 17 + (i - g4 * 4)], None, ALU.mult, None,
                   ["x%d" % tt, "mrs" + tg], hn_res(i))
            for kc in range(8):
                p, pr = pb()
                TR([(p[:, a * 128:(a + 1) * 128], hn_alloc[:, g4 * 4 + a, kc * 128:(kc + 1) * 128]) for a in range(4)],
                   [x for a in range(4) for x in hn_res(g4 * 4 + a)] + ["idb"], [pr])
                dst = HTv[:, kc, g4 * 512:(g4 + 1) * 512]
                res = ["ht%d_%d" % (kc, g4)]
                if kc % 2 == 0:
                    ACT(dst, p, AF.Copy, [pr, "pv"], res, scale=PV[:, gcol + kc:gcol + kc + 1])
                else:
                    TS(dst, p, PV[:, gcol + kc:gcol + kc + 1], None, ALU.mult, None, [pr, "pv"], res)

    def ffn_stats(q):
        sl_ = (q % 2) * 16
        for a in range(4):
            tt = q * 4 + a
            ACT(JUNK[:], X[:, tt, :], AF.Square, ["x%d" % tt], ["junkp", "fss%d" % (q % 2)],
                accum_out=PST[:, sl_ + a:sl_ + a + 1])
        TS(PST[:, sl_ + 4:sl_ + 8], PST[:, sl_:sl_ + 4], 1.0 / D, EPS, ALU.mult, ALU.add, ["fss%d" % (q % 2)], ["fms%d" % (q % 2)])
        POW(PST[:, sl_ + 8:sl_ + 12], PST[:, sl_ + 4:sl_ + 8], ["fms%d" % (q % 2)], ["frs%d" % (q % 2)])

    def postnorm(tt, Y, Yr, tmpR):
        junk = tmpR["junk"]
        for h in range(2):
            ACT(junk, Y[h], AF.Square, [Yr[h]], ["junk", "ss2_%d" % h], accum_out=SM_[:, 32 + h:33 + h])
        TT(SM_[:, 34:35], SM_[:, 32:33], SM_[:, 33:34], ALU.add, ["ss2_0", "ss2_1"], ["ms2"])
        TS(SM_[:, 35:36], SM_[:, 34:35], 1.0 / D, EPS, ALU.mult, ALU.add, ["ms2"], ["ms2b"])
        POW(SM_[:, 36:37], SM_[:, 35:36], ["ms2b"], ["rstd2"])
        for h in range(2):
            t, tr = tmpR["t"].get()
            STT(t, Y[h], SM_[:, 36:37], GBC[:, h * 512:(h + 1) * 512], ALU.mult, ALU.mult, [Yr[h], "rstd2", "gbc"], [tr])
            TT(X[:, tt, h * 512:(h + 1) * 512], X[:, tt, h * 512:(h + 1) * 512], t, ALU.add, ["x%d" % tt, tr], ["x%d" % tt])

    for l in range(n_layers):
        S.dma("sp", lambda e, l=l: e.dma_start(out=PV[:], in_=pv_d[l]), "pv", writes=["pv"])
        for hf in range(2):
            T0 = hf * 1024
            AR.push()
            ART = AR.alloc([4, 1024], BF16)
            AST = AR.alloc([4, 1024], BF16)
            ACFT = AR.alloc([4, 1024], BF16)

            AR.push()
            HN = AR.alloc([8, 1024], BF16)
            if l == 0 and hf == 0:
                prenorm_stats([i for i in range(8)], 0)
            prenorm(l, [hf * 8 + i for i in range(8)], PV_MIXPRE, HT, HN, lambda i: ["hn%d" % i], hf)
            AR.pop()
            htall = ["ht%d_%d" % (kc, g) for kc in range(8) for g in range(2)]
            if l == 0 and hf == 0:
                DBG("HT", HT[:], htall)

            AR.push()
            QT = AR.alloc([2, 1024], BF16)
            KT = AR.alloc([2, 1024], BF16)
            QXT = AR.alloc([2, 1024], BF16)
            ropeR = Rot("rope", [2, 512], F32)
            t1R = Rot("t1", [512], F32)
            t2R = Rot("t2", [512], F32)
            for which in ("q", "k"):
                W, wr = wblock(l, which)
                dstT = QT if which == "q" else KT
                for pt in range(2):
                    rp, rr = ropeR.get()
                    S.dma("sp", lambda e, rp=rp, src_=rope_d[:, :, T0 + pt * 512:T0 + (pt + 1) * 512].rearrange("t p n -> p t n"): e.dma_start(
                        out=rp, in_=src_),
                        "rope" + rr[-1], writes=[rr])
                    for c in range(2):
                        pa, par = pf()
                        MM([(pa, W[:, kc, c * 128:(c + 1) * 128], HT[:, kc, pt * 512:(pt + 1) * 512], kc == 0, kc == 7)
                            for kc in range(8)], [wr] + ["ht%d_%d" % (kc, pt) for kc in range(8)], [par])
                        pb_, pbr = pf()
                        MM([(pb_, W[:, kc, 256 + c * 128:256 + (c + 1) * 128], HT[:, kc, pt * 512:(pt + 1) * 512], kc == 0, kc == 7)
                            for kc in range(8)], [wr] + ["ht%d_%d" % (kc, pt) for kc in range(8)], [pbr])
                        t1, t1r = t1R.get()
                        t2, t2r = t2R.get()
                        TT(t1, pa, rp[:, 0, :], ALU.mult, [par, rr], [t1r])
                        TT(t2, pb_, rp[:, 1, :], ALU.mult, [pbr, rr], [t2r])
                        dres = "%sT%d_%d" % (which, c, pt)
                        TT(dstT[:, c, pt * 512:(pt + 1) * 512], t1, t2, ALU.add, [t1r, t2r], [dres])
                        if which == "q":
                            for r4 in range(4):
                                cs = slice(pt * 512 + r4 * 128, pt * 512 + (r4 + 1) * 128)
                                TT(QXT[:, c, cs], QT[:, c, cs], CST[:, C_XI + c * 128:C_XI + (c + 1) * 128], ALU.mult,
                                   [dres, "cst"], ["qxT%d_%d" % (c, pt)])
            if l == 0 and hf == 0:
                DBG("QT", QT, ["qT%d_%d" % (c, pt) for c in range(2) for pt in range(2)])
                DBG("KT", KT, ["kT%d_%d" % (c, pt) for c in range(2) for pt in range(2)])

            Wv, wvr = wblock(l, "v")
            Wg, wgr = wblock(l, "g")
            VbR = Rot("vb", [512], BF16)
            SGR = Rot("sg", [512], BF16)
            KZR = Rot("kz", [256], BF16)
            SMR = Rot("sm", [512], BF16)
            ONR = Rot("on", [512], F32)
            AAR = Rot("aa", [512], BF16)
            if hf == 0:
                MEMSET(RS[:], 0.0, ["rs"])
                MEMSET(RB[:], 0.0, ["rb"])
            def ret_front(i, mid_cb=None):
                n = hf * 8 + i
                pt = i // 4
                tok = slice(i * 128, (i + 1) * 128)
                htr = ["ht%d_%d" % (kc, pt) for kc in range(8)]
                pv_, pvr = pf(0)
                MM([(pv_, HT[:, kc, tok], Wv[:, kc, :], kc == 0, kc == 7) for kc in range(8)], htr + [wvr], [pvr])
                vb, vbr = VbR.get()
                ACT(vb, pv_, AF.Copy, [pvr], [vbr])
                pg, pgr = pf(1)
                MM([(pg, HT[:, kc, tok], Wg[:, kc, :], kc == 0, kc == 7) for kc in range(8)], htr + [wgr], [pgr])
                sg, sgr = SGR.get()
                ACT(sg, pg, AF.Silu, [pgr], [sgr])
                pk, pkr = pb()
                TR([(pk[:, c * 128:(c + 1) * 128], KT[:, c, tok]) for c in range(2)],
                   ["kT%d_%d" % (c, pt) for c in range(2)] + ["idb"], [pkr])
                kz, kzr = KZR.get()
                TT(kz, pk[:, 0:256], zsfull, ALU.mult, [pkr, "cst"], [kzr])
                psA, psAr = pf(2)
                psB, psBr = pf(3)
                HS = [0, 2, 1, 3]
                SL = [0, 2, 1, 3]
                sc_specs = []
                for h in range(4):
                    s_ = SL[h]
                    bank = psA if s_ < 2 else psB
                    sc_specs.append((bank[:, (s_ % 2) * 128:(s_ % 2) * 128 + 128],
                                     KT[(h % 2) * 64:(h % 2) * 64 + 64, h // 2, tok],
                                     QT[(h % 2) * 64:(h % 2) * 64 + 64, h // 2, tok], True, True))
                MM(sc_specs, ["kT%d_%d" % (c, pt) for c in range(2)] + ["qT%d_%d" % (c, pt) for c in range(2)], [psAr, psBr])
                sm, smr = SMR.get()
                TT(sm[:, 0:256], psA[:, 0:256], dmask[:, 0:256], ALU.mult, [psAr, "cst"], [smr + "a"])
                TT(sm[:, 256:512], psB[:, 0:256], dmask[:, 256:512], ALU.mult, [psBr, "cst"], [smr + "b"])
                if mid_cb is not None:
                    mid_cb()
                poA, poAr = pf(4)
                poB, poBr = pf(5)

                def obank(h):
                    s_ = SL[h]
                    return (poA if s_ < 2 else poB)[:, (s_ % 2) * 128:(s_ % 2) * 128 + 128]

                def obr(h):
                    return poAr if SL[h] < 2 else poBr

                specs = []
                for h in range(4):
                    s_ = SL[h]
                    specs.append((obank(h), sm[:, s_ * 128:(s_ + 1) * 128], vb[:, h * 128:(h + 1) * 128], True, n == 0))
                    if n > 0:
                        specs.append((obank(h),
                                      QXT[(h % 2) * 64:(h % 2) * 64 + 64, h // 2, tok],
                                      RB[(h % 2) * 64:(h % 2) * 64 + 64, h // 2, :], False, True))
                MM(specs, [smr + "a", smr + "b", vbr, "rb"] + ["qxT%d_%d" % (c, pt) for c in range(2)], [poAr, poBr])
                if n < 15:
                    pkv, pkvr = pf(2)
                    MM([(pkv[:, p * 256:(p + 1) * 256], kz[:, p * 128:(p + 1) * 128], vb[:, p * 256:(p + 1) * 256], True, True)
                        for p in range(2)], [kzr, vbr], [pkvr])
                    for p in range(2):
                        for j in range(2):
                            rows = slice(j * 64, (j + 1) * 64)
                            STT(RS[rows, p, :], RS[rows, p, :], CST[rows, C_GC + p:C_GC + p + 1],
                                pkv[rows, p * 256 + j * 128:p * 256 + (j + 1) * 128], ALU.mult, ALU.add,
                                ["rs", pkvr, "cst"], ["rs"])
                    CP(RB[:], RS[:], ["rs"], ["rb"])
                for h in range(4):
                    S.op("dve", lambda e, o_=SM_[:, 40 + h * 6:46 + h * 6], i_=obank(h): e.bn_stats(out=o_, in_=i_),
                         [obr(h)], ["bst%d" % h])
                for h in range(4):
                    S.op("dve", lambda e, o_=SM_[:, 20 + h * 2:22 + h * 2], i_=SM_[:, 40 + h * 6:46 + h * 6]: e.bn_aggr(out=o_, in_=i_),
                         ["bst%d" % h], ["mv%d" % h])
                mv3 = SM_[:, 20:28].rearrange("p (h t) -> p h t", t=2)
                TS(SM_[:, 28:32], mv3[:, :, 1], EPS, None, ALU.add, None, ["mv%d" % h for h in range(4)], ["ve"])
                POW(SM_[:, 4:8], SM_[:, 28:32], ["ve"], ["grstd"])
                STT(SM_[:, 12:16], mv3[:, :, 0], -1.0, SM_[:, 4:8], ALU.mult, ALU.mult, ["mv%d" % h for h in range(4)] + ["grstd"], ["gnmr"])
                on, onr = ONR.get()
                for h in range(4):
                    ACT(on[:, h * 128:(h + 1) * 128], obank(h), AF.Identity, [obr(h), "grstd", "gnmr"], [onr + str(h)],
                        scale=SM_[:, 4 + h:5 + h], bias=SM_[:, 12 + h:13 + h])
                return {"on": on, "onr": onr, "sg": sg, "sgr": sgr, "tok": tok, "pt": pt}

            def ret_aa(ctx):
                aa, aar = AAR.get()
                TT(aa, ctx["on"], ctx["sg"], ALU.mult, [ctx["onr"] + str(h) for h in range(4)] + [ctx["sgr"]], [aar])
                ctx["aa"], ctx["aar"] = aa, aar

            def ret_tail(ctx):
                aa, aar, tok, pt = ctx["aa"], ctx["aar"], ctx["tok"], ctx["pt"]
                pa2, pa2r = pb()
                TR([(pa2[:, k4 * 128:(k4 + 1) * 128], aa[:, k4 * 128:(k4 + 1) * 128]) for k4 in range(4)], [aar, "idb"], [pa2r])
                ACT(ART[:, :, tok], pa2.rearrange("p (k n) -> p k n", n=128), AF.Copy, [pa2r], ["art%d" % pt])

            rctx = ret_front(0)
            for i in range(8):
                if i + 1 < 8:
                    nctx = ret_front(i + 1, mid_cb=lambda c=rctx: ret_aa(c))
                else:
                    nctx = None
                    ret_aa(rctx)
                ret_tail(rctx)
                rctx = nctx
            if l == 0 and hf == 0:
                DBG("ART", ART, ["art0", "art1"])

            PjR = Rot("pj", [1026], BF16)
            BjR = Rot("bj", [1024], BF16)
            cxR = Rot("cx", [512], F32)
            DGR = Rot("dg", [3, 128], BF16)
            def sc_front(j):
                W, wr = wblock(l, "sc%d" % j)
                pj, pjr = PjR.get()
                bj, bjr = BjR.get()
                if hf == 0:
                    MEMSET(pj[:, 0:2], 0.0, [pjr + "h"])
                else:
                    CP(pj[:, 0:2], PH[:, j, :], ["ph%d" % j], [pjr + "h"])
                dg, dgr = DGR.get()
                for k in range(3):
                    TS(dg[:, k, :], ident_f, PV[:, PV_SCW + j * 3 + k:PV_SCW + j * 3 + k + 1], None, ALU.mult, None,
                       ["cst", "pv"], [dgr + str(k)])
                for pt in range(2):
                    htr = ["ht%d_%d" % (kc, pt) for kc in range(8)]
                    ps3 = []
                    for b in range(3):
                        p, pr = pf()
                        MM([(p, W[:, kc, b * 128:(b + 1) * 128], HT[:, kc, pt * 512:(pt + 1) * 512], kc == 0, kc == 7)
                            for kc in range(8)], htr + [wr], [pr])
                        ps3.append((p, pr))
                    ACT(bj[:, pt * 512:(pt + 1) * 512], ps3[0][0], AF.Copy, [ps3[0][1]], [bjr + str(pt)])
                    cx, cxr = cxR.get()
                    ACT(cx, ps3[1][0], AF.Copy, [ps3[1][1]], [cxr])
                    TT(pj[:, 2 + pt * 512:2 + (pt + 1) * 512], ps3[2][0], cx, ALU.mult, [ps3[2][1], cxr], [pjr + str(pt)])
                CP(PH[:, j, :], pj[:, 1024:1026], [pjr + "1", pjr + "h"], ["ph%d" % j])
                return (j, pj, pjr, bj, bjr, dg, dgr)

            def sc_tail(ctx):
                j, pj, pjr, bj, bjr, dg, dgr = ctx
                for pt in range(2):
                    p, pr = pf()
                    MM([(p, dg[:, k, :], pj[:, pt * 512 + k:pt * 512 + k + 512], k == 0, k == 2) for k in range(3)],
                       [dgr + "0", dgr + "1", dgr + "2", pjr + "h", pjr + "0", pjr + "1"], [pr])
                    TT(AST[:, j, pt * 512:(pt + 1) * 512], p, bj[:, pt * 512:(pt + 1) * 512], ALU.mult, [pr, bjr + str(pt)], ["ast%d" % pt])

            sctx = sc_front(0)
            for j in range(4):
                nctx = sc_front(j + 1) if j + 1 < 4 else None
                sc_tail(sctx)
                sctx = nctx
            AR.pop()
            if l == 0 and hf == 0:
                DBG("AST", AST, ["ast0", "ast1"])

            AR.push()
            G = AR.alloc([4, 1054], BF16)
            CB = AR.alloc([4, 1024], BF16)
            CSQ = AR.alloc([4, 1024], BF16)
            DG31R = Rot("dg31", [31, 128], BF16)
            sbR = Rot("sb", [512], F32)
            for j in range(4):
                W, wr = wblock(l, "cf%d" % j)
                if hf == 0:
                    MEMSET(G[:, j, 0:30], 0.0, ["g%dh" % j])
                else:
                    CP(G[:, j, 0:30], GH[:, j, :], ["gh%d" % j], ["g%dh" % j])
                for pt in range(2):
                    htr = ["ht%d_%d" % (kc, pt) for kc in range(8)]
                    p2 = []
                    for b in range(2):
                        p, pr = pf()
                        MM([(p, W[:, kc, b * 128:(b + 1) * 128], HT[:, kc, pt * 512:(pt + 1) * 512], kc == 0, kc == 7)
                            for kc in range(8)], htr + [wr], [pr])
                        p2.append((p, pr))
                    sb, sbr = sbR.get()
                    ACT(sb, p2[1][0], AF.Sigmoid, [p2[1][1]], [sbr])
                    TT(G[:, j, 30 + pt * 512:30 + (pt + 1) * 512], p2[0][0], sb, ALU.mult, [p2[0][1], sbr], ["g%d_%d" % (j, pt)])
                CP(GH[:, j, :], G[:, j, 1024:1054], ["g%d_1" % j, "g%dh" % j], ["gh%d" % j])
            def dg31_build(j):
                dg, dgr = DG31R.get()
                for k in range(31):
                    sc_ap = PV[:, PV_CFW + j * 31 + k:PV_CFW + j * 31 + k + 1]
                    TS(dg[:, k, :], ident_f, sc_ap, None, ALU.mult, None, ["cst", "pv"], [dgr + "_%d" % k])
                return dg, dgr

            rowA = AR.alloc([512], F32)
            rowB = AR.alloc([512], F32)
            rowC = AR.alloc([512], F32)
            rowD = AR.alloc([512], F32)
            tR = Rot("lnt", [512], F32)
            t2R_ = Rot("lnt2", [512], F32)
            bc = {}

            def ln_chain(pt):
                sl = slice(pt * 512, (pt + 1) * 512)
                pm, pmr = pf(0)
                MM([(pm[0:1, :], ONEC[:, 0:1], CB[:, j, sl], j == 0, j == 3) for j in range(4)],
                   ["onec"] + ["cb%d_%d" % (j, pt) for j in range(4)], [pmr])
                pe2, pe2r = pf(1)
                MM([(pe2[0:1, :], ONEC[:, 0:1], CSQ[:, j, sl], j == 0, j == 3) for j in range(4)],
                   ["onec"] + ["csq%d_%d" % (j, pt) for j in range(4)], [pe2r])
                ACT(rowA[0:1, :], pm[0:1, :], AF.Copy, [pmr], ["rowA"])
                ACT(rowB[0:1, :], pm[0:1, :], AF.Square, [pmr], ["rowB"])
                STT(rowB[0:1, :], pe2[0:1, :], EPS, rowB[0:1, :], ALU.add, ALU.subtract, [pe2r, "rowB"], ["rowB"])
                POW(rowC[0:1, :], rowB[0:1, :], ["rowB"], ["rowC"])
                STT(rowD[0:1, :], rowA[0:1, :], -1.0, rowC[0:1, :], ALU.mult, ALU.mult, ["rowA", "rowC"], ["rowD"])
                pr_, prr = pf(2 + 2 * pt)
                MM([(pr_, ONER[0:1, :], rowC[0:1, :], True, True)], ["oner", "rowC"], [prr])
                pn_, pnr = pf(3 + 2 * pt)
                MM([(pn_, ONER[0:1, :], rowD[0:1, :], True, True)], ["oner", "rowD"], [pnr])
                bc[pt] = (pr_, prr, pn_, pnr)

            def ln_norm(pt):
                sl = slice(pt * 512, (pt + 1) * 512)
                pr_, prr, pn_, pnr = bc[pt]
                for j in range(4):
                    t, tr = tR.get()
                    TT(t, pr_, CB[:, j, sl], ALU.mult, [prr, "cb%d_%d" % (j, pt)], [tr])
                    t2, t2r = t2R_.get()
                    TT(t2, pn_, t, ALU.add, [pnr, tr], [t2r])
                    ACT(ACFT[:, j, sl], t2, AF.Silu, [t2r, "pv"], ["acft%d" % pt],
                        scale=PV[:, PV_LNG + j:PV_LNG + j + 1], bias=PV[:, PV_LNB + j:PV_LNB + j + 1])

            dgc = dg31_build(0)
            for j in range(4):
                dgn = dg31_build(j + 1) if j + 1 < 4 else None
                dg, dgr = dgc
                for pt in range(2):
                    p, pr = pf(0) if (j == 3 and pt == 1) else pf()
                    MM([(p, dg[:, k, :], G[:, j, pt * 512 + k:pt * 512 + k + 512], k == 0, k == 30) for k in range(31)],
                       [dgr + "_%d" % k for k in range(31)] + ["g%dh" % j, "g%d_0" % j, "g%d_1" % j], [pr])
                    bias = PV[:, PV_CFB + j:PV_CFB + j + 1]
                    ACT(CB[:, j, pt * 512:(pt + 1) * 512], p, AF.Identity, [pr, "pv"], ["cb%d_%d" % (j, pt)], bias=bias)
                    ACT(CSQ[:, j, pt * 512:(pt + 1) * 512], p, AF.Square, [pr, "pv"], ["csq%d_%d" % (j, pt)], bias=bias)
                    if j == 3 and pt == 0:
                        ln_chain(0)
                dgc = dgn
            ln_chain(1)
            ln_norm(0)
            ln_norm(1)
            AR.pop()
            if l == 0 and hf == 0:
                DBG("ACFT", ACFT, ["acft0", "acft1"])

            AR.push()
            MT = AR.alloc([8, 1024], BF16)
            sgR = Rot("sgm", [512], F32, 3)
            mR = Rot("mm", [512], F32, 2)
            tmR = Rot("tm", [512], F32, 2)
            AB = [ART, AST, ACFT]
            ABn = ["art", "ast", "acft"]
            for f in range(8):
                Wgb, wgr_ = wblock(l, "gb%d" % f)
                wbr_ = wgr_
                Wg_ = Wgb[:, 0, 0:3072].rearrange("p (k n) -> p k n", n=384)
                Wb_ = Wgb[:, 0, 3072:4608].rearrange("p (k n) -> p k n", n=384)
                for pt in range(2):
                    sl = slice(pt * 512, (pt + 1) * 512)
                    htr = ["ht%d_%d" % (kc, pt) for kc in range(8)]
                    m, mr = mR.get()
                    for b in range(3):
                        pg_, pgr_ = pf()
                        MM([(pg_, Wg_[:, kc, b * 128:(b + 1) * 128], HT[:, kc, sl], kc == 0, kc == 7) for kc in range(8)],
                           htr + [wgr_], [pgr_])
                        py_, pyr_ = pf()
                        MM([(py_, Wb_[:, k4, b * 128:(b + 1) * 128], AB[b][:, k4, sl], k4 == 0, k4 == 3) for k4 in range(4)],
                           [ABn[b] + str(pt), wbr_], [pyr_])
                        sgm, sgmr = sgR.get()
                        ACT(sgm, pg_, AF.Sigmoid, [pgr_], [sgmr])
                        if b == 0:
                            TT(m, py_, sgm, ALU.mult, [pyr_, sgmr], [mr])
                        else:
                            tm, tmr = tmR.get()
                            TT(tm, py_, sgm, ALU.mult, [pyr_, sgmr], [tmr])
                            if b == 1:
                                TT(m, m, tm, ALU.add, [mr, tmr], [mr])
                            else:
                                TT(MT[:, f, sl], m, tm, ALU.add, [mr, tmr], ["mt%d" % pt])
            if l == 0 and hf == 0:
                DBG("MT", MT, ["mt0", "mt1"])

            S.dma("sp", lambda e, l=l: e.dma_start(out=GBC[:], in_=gbc_d[l, 0]), "gbc", writes=["gbc"])
            Wo0, wo0r = wblock(l, "wo0")
            Wo1, wo1r = wblock(l, "wo1")
            tmpR = {"junk": AR.alloc([512], BF16), "t": Rot("pnt", [512], F32, 2)}
            if hf == 0:
                prenorm_stats([8 + i for i in range(8)], 1)
            else:
                ffn_stats(0)
            for i in range(8):
                tt = hf * 8 + i
                pt = i // 4
                tok = slice(i * 128, (i + 1) * 128)
                Y, Yr = [], []
                for h, (Wo, wor) in enumerate(((Wo0, wo0r), (Wo1, wo1r))):
                    p, pr = pf()
                    MM([(p, MT[:, kc, tok], Wo[:, kc, :], kc == 0, kc == 7) for kc in range(8)], ["mt%d" % pt, wor], [pr])
                    Y.append(p)
                    Yr.append(pr)
                postnorm(tt, Y, Yr, tmpR)
            AR.pop()
            AR.pop()
            if l == 0 and hf == 0:
                DBG("X1", X[:, 0:8, :], ["x%d" % t for t in range(8)])

        AR.push()
        WD = AR.alloc([NFF, 1024], BF16)
        ACTT = AR.alloc([NFF, 512], BF16)
        HN2 = AR.alloc([2, 1024], BF16)
        UGR = Rot("ug", [514], BF16)
        UVR = Rot("uv", [514], BF16)
        DGF = Rot("dgf", [6, 128], BF16)
        slR = Rot("sl", [512], BF16)
        tmpR = {"junk": AR.alloc([512], BF16), "t": Rot("pnt", [512], F32, 2)}
        S.dma("sp", lambda e, l=l: e.dma_start(out=GBC[:], in_=gbc_d[l, 1]), "gbc", writes=["gbc"])
        wd_issued = 0

        def ffn_apply(q):
            sl_ = (q % 2) * 16
            g = q % 2
            for pair in range(2):
                for a2 in range(2):
                    a = pair * 2 + a2
                    tt = q * 4 + a
                    TS(HN2[:, a2, :], X[:, tt, :], PST[:, sl_ + 8 + a:sl_ + 9 + a], None, ALU.mult, None,
                       ["x%d" % tt, "frs%d" % (q % 2)], ["hn2_%d" % a2])
                for kc in range(8):
                    p, pr = pb()
                    TR([(p[:, a2 * 128:(a2 + 1) * 128], HN2[:, a2, kc * 128:(kc + 1) * 128]) for a2 in range(2)],
                       ["hn2_0", "hn2_1", "idb"], [pr])
                    dst = HT[:, kc, g * 512 + pair * 256:g * 512 + (pair + 1) * 256]
                    res = ["ht%d_%d_%d" % (kc, g, pair)]
                    if kc % 2 == 0:
                        ACT(dst, p[:, 0:256], AF.Copy, [pr, "pv"], res, scale=PV[:, PV_FFNPRE + kc:PV_FFNPRE + kc + 1])
                    else:
                        TS(dst, p[:, 0:256], PV[:, PV_FFNPRE + kc:PV_FFNPRE + kc + 1], None, ALU.mult, None, [pr, "pv"], res)

        ffn_apply(0)
        for qt in range(4):
            hg = qt % 2
            HTq = HT[:, :, hg * 512:(hg + 1) * 512]
            htr = ["ht%d_%d_%d" % (kc, hg, pr_) for kc in range(8) for pr_ in range(2)]
            def up_front(j):
                W, wr = wblock(l, "up%d" % j)
                ug, ugr = UGR.get()
                uv, uvr = UVR.get()
                if qt == 0:
                    MEMSET(ug[:, 0:2], 0.0, [ugr + "h"])
                    MEMSET(uv[:, 0:2], 0.0, [uvr + "h"])
                else:
                    CP(ug[:, 0:2], FH[:, j, 0:2], ["fh%d" % j], [ugr + "h"])
                    CP(uv[:, 0:2], FH[:, j, 2:4], ["fh%d" % j], [uvr + "h"])
                dg, dgr = DGF.get()
                for k in range(3):
                    TS(dg[:, k, :], ident_f, PV[:, PV_FGW + j * 3 + k:PV_FGW + j * 3 + k + 1], None, ALU.mult, None, ["cst", "pv"], [dgr + str(k)])
                    TS(dg[:, 3 + k, :], ident_f, PV[:, PV_FVW + j * 3 + k:PV_FVW + j * 3 + k + 1], None, ALU.mult, None, ["cst", "pv"], [dgr + str(3 + k)])
                pg_, pgr_ = pf()
                MM([(pg_, W[:, kc, 0:128], HTq[:, kc, :], kc == 0, kc == 7) for kc in range(8)], htr + [wr], [pgr_])
                pv2, pv2r = pf()
                MM([(pv2, W[:, kc, 128:256], HTq[:, kc, :], kc == 0, kc == 7) for kc in range(8)], htr + [wr], [pv2r])
                ACT(ug[:, 2:514], pg_, AF.Copy, [pgr_], [ugr])
                ACT(uv[:, 2:514], pv2, AF.Copy, [pv2r], [uvr])
                CP(FH[:, j, 0:2], ug[:, 512:514], [ugr, ugr + "h"], ["fh%d" % j])
                CP(FH[:, j, 2:4], uv[:, 512:514], [uvr, uvr + "h"], ["fh%d" % j])
                return (j, ug, ugr, uv, uvr, dg, dgr)

            def up_tail(ctx):
                j, ug, ugr, uv, uvr, dg, dgr = ctx
                pcg, pcgr = pf()
                MM([(pcg, dg[:, k, :], ug[:, k:k + 512], k == 0, k == 2) for k in range(3)],
                   [dgr + "0", dgr + "1", dgr + "2", ugr, ugr + "h"], [pcgr])
                pcv, pcvr = pf()
                MM([(pcv, dg[:, 3 + k, :], uv[:, k:k + 512], k == 0, k == 2) for k in range(3)],
                   [dgr + "3", dgr + "4", dgr + "5", uvr, uvr + "h"], [pcvr])
                sl_, slr = slR.get()
                ACT(sl_, pcg, AF.Silu, [pcgr], [slr])
                TT(ACTT[:, j, :], pcv, sl_, ALU.mult, [pcvr, slr], ["actt%d" % j])

            def wd_maybe(j):
                nonlocal wd_issued
                if qt == 0 and j % 2 == 1 and wd_issued < 11:
                    c = wd_issued
                    o, kc_, n_ = offs["wd%d" % c]
                    S.dma("pool", lambda e, o_=WD[:, 2 * c:2 * c + 2, :].rearrange("p k (a b) -> p (k a) b", b=512),
                          i_=wst_d[l, :, o:o + 2048].rearrange("p (a b) -> p a b", b=512): e.dma_start(out=o_, in_=i_),
                          "wd", writes=["wd"])
                    wd_issued += 1

            uctx = up_front(0)
            for j in range(NFF):
                wd_maybe(j)
                if qt < 3 and j == 6:
                    ffn_stats(qt + 1)
                if qt < 3 and j == 13:
                    ffn_apply(qt + 1)
                nctx = up_front(j + 1) if j + 1 < NFF else None
                up_tail(uctx)
                uctx = nctx
            if l == 0 and qt == 0:
                DBG("ACTT", ACTT, ["actt%d" % j for j in range(NFF)])
            if qt == 3 and l + 1 < n_layers:
                prenorm_stats([i for i in range(8)], 0)
            for a in range(4):
                tt = qt * 4 + a
                tok = slice(a * 128, (a + 1) * 128)
                Y, Yr = [], []
                for h in range(2):
                    p, pr = pf()
                    MM([(p, ACTT[:, kc, tok], WD[:, kc, h * 512:(h + 1) * 512], kc == 0, kc == NFF - 1) for kc in range(NFF)],
                       ["actt%d" % kc for kc in range(NFF)] + ["wd"], [pr])
                    Y.append(p)
                    Yr.append(pr)
                postnorm(tt, Y, Yr, tmpR)
            if l == n_layers - 1:
                S.dma("sp", lambda e, qt=qt: e.dma_start(
                    out=out_d[qt * 512:(qt + 1) * 512, :].rearrange("(a p) d -> p a d", p=128),
                    in_=X[:, qt * 4:(qt + 1) * 4, :]), "out%d" % qt, reads=["x%d" % (qt * 4 + a) for a in range(4)])
        AR.pop()
    S.barrier()
    S.emit()
    return nc, dbg_d, S, AR


_CACHE = {}


def kernel(**inputs):
    x = np.asarray(inputs["x"], np.float32)
    B = x.shape[0]
    wst = np.stack([prep_layer_stream(inputs, l) for l in range(NL)], axis=0)
    pv = np.stack([prep_pv(inputs, l) for l in range(NL)], axis=0)
    gbc = np.stack([np.stack([np.broadcast_to(np.asarray(inputs["norm_mix_post"][l], np.float32)[None, :], (128, D)),
                              np.broadcast_to(np.asarray(inputs["norm_ffn_post"][l], np.float32)[None, :], (128, D))], axis=0)
                    for l in range(NL)], axis=0)
    gbc = np.ascontiguousarray(gbc)
    cst, rope = make_consts()
    nc = build()[0]
    in_maps = [{"x": np.ascontiguousarray(x[b]), "wst": wst, "pv": pv, "gbc": gbc, "cst": cst, "rope": rope} for b in range(B)]
    res = run_bass_kernel_spmd(nc, in_maps, core_ids=list(range(B)))
    return np.stack([np.asarray(r["out"], np.float32) for r in res.results], axis=0)
```

```python
import contextlib
import numpy as np
import concourse.bass as bass
import concourse.mybir as mybir
from concourse.bass_utils import run_bass_kernel_spmd

F32 = mybir.dt.float32
BF16 = mybir.dt.bfloat16
AF = mybir.ActivationFunctionType
ALU = mybir.AluOpType

D = 1024
SEQ = 2048
NL = 2
NFF = 22
EPS = 1e-6
NSLOT = 3
SLOTC = 4608
ENGS = ("pe", "act", "dve", "pool", "sp")

PV_MIXPRE, PV_FFNPRE, PV_SCW, PV_CFW, PV_CFB, PV_LNG, PV_LNB, PV_FGW, PV_FVW = 0, 8, 16, 28, 152, 156, 160, 164, 230
NPV = 296
C_ID, C_DM, C_XI, C_ZS, C_GC = 0, 128, 640, 896, 1152
NCST = 1154


class Sched:
    def __init__(self, nc, self_sync=("act", "dve", "pool")):
        self.nc = nc
        self.prog = {e: [] for e in ENGS}
        self.cnt = {e: 0 for e in ENGS}
        self.waited = {}
        self.lastw = {}
        self.readers = {}
        self.self_sync = set(self_sync)
        self.dma_sems = {}
        self.n_instr = 0

    def _need(self, eng, reads, writes, skip=None):
        need = {}

        def add(dep):
            if dep is None:
                return
            d, v = dep
            if need.get(d, 0) < v:
                need[d] = v

        for r in reads:
            add(self.lastw.get(r))
            if r.startswith("pf") or r.startswith("pb"):
                for d, v in self.readers.get(r, {}).items():
                    if d != eng:
                        add((d, v))
        for w in writes:
            add(self.lastw.get(w))
            for d, v in self.readers.get(w, {}).items():
                add((d, v))
        out = []
        for d, v in need.items():
            if d == skip:
                continue
            if d == eng and eng not in self.self_sync:
                continue
            key = (eng, d)
            if self.waited.get(key, 0) >= v:
                continue
            self.waited[key] = v
            out.append((d, v))
        return out

    def _mark(self, who, idx, reads, writes):
        for r in reads:
            self.readers.setdefault(r, {})[who] = idx
        for w in writes:
            self.lastw[w] = (who, idx)
            self.readers[w] = {}

    def op(self, eng, fn, reads=(), writes=()):
        waits = self._need(eng, reads, writes)
        self.cnt[eng] += 1
        self.prog[eng].append((waits, fn, ("eng", eng, 1)))
        self._mark(eng, self.cnt[eng], reads, writes)
        self.n_instr += 1

    def pe(self, fns, reads=(), writes=()):
        waits = self._need("pe", reads, writes)
        self.cnt["pe"] += 1
        n = len(fns)
        for i, fn in enumerate(fns):
            self.prog["pe"].append((waits if i == 0 else [], fn, ("eng", "pe", 1) if i == n - 1 else None))
        self._mark("pe", self.cnt["pe"], reads, writes)
        self.n_instr += n

    def dma(self, queue, fn, semname, reads=(), writes=()):
        d = "dma:" + semname
        waits = self._need(queue, reads, writes, skip=d)
        c = self.dma_sems.setdefault(semname, [0])
        c[0] += 16
        self.prog[queue].append((waits, fn, ("dma", semname, 16)))
        self._mark(d, c[0], reads, writes)
        self.n_instr += 1

    def barrier(self):
        snap = dict(self.cnt)
        dsnap = {"dma:" + k: v[0] for k, v in self.dma_sems.items()}
        for e in ENGS:
            if e == "pe":
                continue
            waits = []
            for d, v in list(snap.items()) + list(dsnap.items()):
                if d != e and v > 0 and self.waited.get((e, d), 0) < v:
                    self.waited[(e, d)] = v
                    waits.append((d, v))
            if waits:
                self.prog[e].append((waits, None, None))

    def emit(self):
        nc = self.nc
        with nc.cleanup_on_exit():
            sems = {}
            for e in ENGS:
                sems[e] = nc.alloc_semaphore(name="s_" + e)
            for k in self.dma_sems:
                sems["dma:" + k] = nc.alloc_semaphore(name="d_" + k)
            for s_ in sems.values():
                nc.gpsimd.sem_clear(s_)
            nc.all_engine_barrier()
            with nc.Block() as block:

                def run(eng_name):
                    def body(eng):
                        for waits, fn, inc in self.prog[eng_name]:
                            for d, v in waits:
                                eng.wait_ge(sems[d], v)
                            if fn is None:
                                continue
                            ins = fn(eng)
                            if inc is not None:
                                kind, name, n = inc
                                ins.then_inc(sems[name] if kind == "eng" else sems["dma:" + name], n)
                    return body

                block.tensor(run("pe"))
                block.scalar(run("act"))
                block.vector(run("dve"))
                block.gpsimd(run("pool"))
                block.sync(run("sp"))


def _blk(w):
    K, n = w.shape
    return np.ascontiguousarray(w.reshape(K // 128, 128, n).transpose(1, 0, 2)).reshape(128, -1)


def weight_blocks():
    blocks = [("q", 8, 512), ("k", 8, 512), ("v", 8, 512), ("g", 8, 512)]
    blocks += [("sc%d" % j, 8, 384) for j in range(4)]
    blocks += [("cf%d" % j, 8, 256) for j in range(4)]
    blocks += [("gb%d" % f, 1, 4608) for f in range(8)]
    blocks += [("wo0", 8, 512), ("wo1", 8, 512)]
    blocks += [("up%d" % j, 8, 256) for j in range(NFF)]
    blocks += [("wd%d" % c, 2, 1024) for c in range(11)]
    offs = {}
    o = 0
    for name, kc, n in blocks:
        offs[name] = (o, kc, n)
        o += kc * n
    return blocks, offs, o


def prep_layer_stream(inp, l):
    w_in = np.asarray(inp["w_in"][l], dtype=np.float32)
    perm = np.array([h * 64 + (i + 32) % 64 for h in range(4) for i in range(64)])
    q = w_in[:, 0:256]
    k = w_in[:, 256:512]
    parts = {}
    parts["q"] = np.concatenate([q, q[:, perm]], axis=1)
    parts["k"] = np.concatenate([k, k[:, perm]], axis=1)
    parts["v"] = w_in[:, 512:1024]
    parts["g"] = w_in[:, 1024:1536]
    for j in range(4):
        s = slice(j * 128, (j + 1) * 128)
        parts["sc%d" % j] = np.concatenate([w_in[:, 1536:2048][:, s], w_in[:, 2048:2560][:, s], w_in[:, 2560:3072][:, s]], axis=1)
        parts["cf%d" % j] = np.concatenate([w_in[:, 3072:3584][:, s], w_in[:, 3584:4096][:, s]], axis=1)
    wro = np.asarray(inp["w_ret_out"][l], np.float32)
    wso = np.asarray(inp["w_sc_out"][l], np.float32)
    wco = np.asarray(inp["w_cf_out"][l], np.float32)
    for f in range(8):
        s = slice(f * 128, (f + 1) * 128)
        gt = np.concatenate([w_in[:, 4096 + b * 1024: 4096 + (b + 1) * 1024][:, s] for b in range(3)], axis=1)
        bo = np.concatenate([wro[:, s], wso[:, s], wco[:, s]], axis=1)
        parts["gb%d" % f] = np.concatenate([_blk(gt), _blk(bo)], axis=1)
    wo = np.asarray(inp["w_o"][l], np.float32)
    parts["wo0"] = wo[:, 0:512]
    parts["wo1"] = wo[:, 512:1024]
    wup = np.asarray(inp["w_up"][l], np.float32)
    for j in range(NFF):
        s = slice(j * 128, (j + 1) * 128)
        parts["up%d" % j] = np.concatenate([wup[:, :2816][:, s], wup[:, 2816:][:, s]], axis=1)
    wd = np.asarray(inp["w_down"][l], np.float32)
    for c in range(11):
        parts["wd%d" % c] = wd[c * 256:(c + 1) * 256, :]
    blocks, offs, total = weight_blocks()
    out = np.empty((128, total), np.float32)
    for name, kc, n in blocks:
        o = offs[name][0]
        out[:, o:o + kc * n] = parts[name] if name.startswith("gb") else _blk(parts[name])
    return out


def prep_pv(inp, l):
    pv = np.zeros((128, NPV), np.float32)

    def fm(vec, nchunk):
        return np.asarray(vec, np.float32).reshape(nchunk, 128).T

    pv[:, PV_MIXPRE:PV_MIXPRE + 8] = fm(inp["norm_mix_pre"][l], 8)
    pv[:, PV_FFNPRE:PV_FFNPRE + 8] = fm(inp["norm_ffn_pre"][l], 8)
    scw = np.asarray(inp["sc_conv_w"][l], np.float32)
    pv[:, PV_SCW:PV_SCW + 12] = scw.reshape(3, 4, 128).transpose(2, 1, 0).reshape(128, 12)
    cfw = np.asarray(inp["cf_conv_w"][l], np.float32)
    pv[:, PV_CFW:PV_CFW + 124] = cfw.reshape(31, 4, 128).transpose(2, 1, 0).reshape(128, 124)
    pv[:, PV_CFB:PV_CFB + 4] = fm(inp["cf_conv_b"][l], 4)
    pv[:, PV_LNG:PV_LNG + 4] = fm(inp["cf_ln_g"][l], 4)
    pv[:, PV_LNB:PV_LNB + 4] = fm(inp["cf_ln_b"][l], 4)
    fw = np.asarray(inp["ffn_conv_w"][l], np.float32)
    pv[:, PV_FGW:PV_FGW + 66] = fw[:, :2816].reshape(3, NFF, 128).transpose(2, 1, 0).reshape(128, 66)
    pv[:, PV_FVW:PV_FVW + 66] = fw[:, 2816:].reshape(3, NFF, 128).transpose(2, 1, 0).reshape(128, 66)
    return pv


def make_consts():
    cst = np.zeros((128, NCST), np.float64)
    cst[:, C_ID:C_ID + 128] = np.eye(128)
    gam = 1.0 - np.exp2(-5.0 - np.arange(4))
    m = np.arange(128)[:, None]
    c = np.arange(128)[None, :]
    for h in range(4):
        dm = np.where(c >= m, gam[h] ** np.maximum(c - m, 0), 0.0) * 0.125
        sl_ = [0, 2, 1, 3][h]
        cst[:, C_DM + sl_ * 128:C_DM + (sl_ + 1) * 128] = dm
        cst[:, C_ZS + h * 64:C_ZS + (h + 1) * 64] = (0.125 * gam[h] ** (127 - np.arange(128)))[:, None]
    p = np.arange(128)
    for ch in range(2):
        hh = ch * 2 + p // 64
        xi = gam[hh][:, None] ** (np.arange(128)[None, :] + 1.0)
        cst[:, C_XI + ch * 128:C_XI + (ch + 1) * 128] = xi
        cst[:, C_GC + ch] = gam[hh] ** 128
    inv = 10000.0 ** (-np.arange(32) / 32.0)
    t = np.arange(SEQ)[None, :]
    ang = t * inv[p % 32][:, None]
    sign = np.where((p % 64) < 32, -1.0, 1.0)[:, None]
    rope = np.stack([np.cos(ang), sign * np.sin(ang)], axis=0)
    return cst.astype(np.float32), rope.astype(np.float32)


class Arena:
    def __init__(self, nc, S, nbytes):
        self.t = nc.alloc_sbuf_tensor("arena", [128, nbytes // 2], BF16)
        self.top = 0
        self.cap = nbytes
        self.stack = []
        self.S = S
        self.peak = 0

    def push(self):
        self.stack.append(self.top)

    def pop(self):
        self.S.barrier()
        self.top = self.stack.pop()

    def alloc(self, shape, dtype):
        n = int(np.prod(shape))
        esz = 4 if dtype == F32 else 2
        nb = (n * esz + 63) // 64 * 64
        off = self.top
        self.top += nb
        self.peak = max(self.peak, self.top)
        assert self.top <= self.cap, ("arena overflow", self.top, self.cap)
        ap = self.t[:, off // 2: off // 2 + (n * esz) // 2]
        if dtype == F32:
            ap = ap.bitcast(F32)
        if len(shape) == 2:
            names = "a b"
        else:
            names = "a b c"
        if len(shape) == 1:
            return ap
        kw = {"a": shape[0]} if len(shape) == 2 else {"a": shape[0], "b": shape[1]}
        return ap.rearrange("p (%s) -> p %s" % (names, names), **kw)


def build(n_layers=NL, dbg=()):
    nc = bass.Bass("TRN2", target_bir_lowering=False)
    blocks, offs, WCOLS = weight_blocks()
    x_d = nc.dram_tensor("x", [SEQ, D], F32, kind="ExternalInput").ap()
    wst_d = nc.dram_tensor("wst", [n_layers, 128, WCOLS], F32, kind="ExternalInput").ap()
    pv_d = nc.dram_tensor("pv", [n_layers, 128, NPV], F32, kind="ExternalInput").ap()
    gbc_d = nc.dram_tensor("gbc", [n_layers, 2, 128, D], F32, kind="ExternalInput").ap()
    cst_d = nc.dram_tensor("cst", [128, NCST], F32, kind="ExternalInput").ap()
    rope_d = nc.dram_tensor("rope", [2, 128, SEQ], F32, kind="ExternalInput").ap()
    out_d = nc.dram_tensor("out", [SEQ, D], F32, kind="ExternalOutput").ap()
    dbg_d = {}

    S = Sched(nc)

    X = nc.alloc_sbuf_tensor("X", [128, 16, D], F32)
    HT = nc.alloc_sbuf_tensor("HT", [128, 8, 1024], BF16)
    RING = nc.alloc_sbuf_tensor("RING", [128, NSLOT, SLOTC], BF16)
    CST = nc.alloc_sbuf_tensor("CST", [128, NCST], F32)
    PV = nc.alloc_sbuf_tensor("PVt", [128, NPV], F32)
    GBC = nc.alloc_sbuf_tensor("GBC", [128, D], F32)
    IDB = nc.alloc_sbuf_tensor("IDB", [128, 128], BF16)
    ONEC = nc.alloc_sbuf_tensor("ONEC", [128, 2], BF16)
    ONER = nc.alloc_sbuf_tensor("ONER", [1, 128], F32)
    RS = nc.alloc_sbuf_tensor("RS", [128, 2, 128], F32)
    RB = nc.alloc_sbuf_tensor("RB", [128, 2, 128], BF16)
    PH = nc.alloc_sbuf_tensor("PH", [128, 4, 2], BF16)
    GH = nc.alloc_sbuf_tensor("GH", [128, 4, 30], BF16)
    FH = nc.alloc_sbuf_tensor("FH", [128, NFF, 4], BF16)
    SM_ = nc.alloc_sbuf_tensor("SMALL", [128, 64], F32)
    PST = nc.alloc_sbuf_tensor("PST", [128, 32], F32)
    MST = nc.alloc_sbuf_tensor("MST", [128, 48], F32)
    JUNK = nc.alloc_sbuf_tensor("JUNK", [128, 1024], BF16)
    PSF = nc.alloc_psum_tensor("PSF", [128, 6, 512], F32)
    PSB = nc.alloc_psum_tensor("PSB", [128, 2, 1024], BF16)
    AR = Arena(nc, S, 85 * 1024)

    ident_f = CST[:, C_ID:C_ID + 128]
    dmask = CST[:, C_DM:C_DM + 512]
    zsfull = CST[:, C_ZS:C_ZS + 256]

    st = {"pf": 0, "pb": 0, "blk": 0, "issued": 0}
    stream = []
    for l in range(n_layers):
        for hf in range(2):
            for name, kc, n in blocks:
                if not (name.startswith("up") or name.startswith("wd")):
                    stream.append((l, name))
        for qt in range(4):
            for j in range(NFF):
                stream.append((l, "up%d" % j))

    def pf(fixed=None):
        if fixed is not None:
            return PSF[:, fixed, :], "pf%d" % fixed
        b = st["pf"] % 6
        st["pf"] += 1
        return PSF[:, b, :], "pf%d" % b

    def pb():
        b = st["pb"] % 2
        st["pb"] += 1
        return PSB[:, b, 0:512], "pb%d" % b

    def issue_block(i):
        l, name = stream[i]
        o, kc, n = offs[name]
        s = i % NSLOT
        cols = kc * n
        S.dma("pool", lambda e: e.dma_start(
            out=RING[:, s, 0:cols].rearrange("p (a b) -> p a b", b=512),
            in_=wst_d[l, :, o:o + cols].rearrange("p (a b) -> p a b", b=512)),
            "ring%d" % s, writes=["ring%d" % s])

    def wblock(l, name):
        i = st["blk"]
        assert stream[i] == (l, name), (stream[i], l, name)
        st["blk"] += 1
        while st["issued"] <= min(i + 1, len(stream) - 1):
            issue_block(st["issued"])
            st["issued"] += 1
        o, kc, n = offs[name]
        s = i % NSLOT
        return RING[:, s, 0:kc * n].rearrange("p (k n) -> p k n", n=n), "ring%d" % s

    class Rot:
        def __init__(self, name, shape, dtype, n=2):
            self.bufs = [AR.alloc(shape, dtype) for _ in range(n)]
            self.name = name
            self.i = 0

        def get(self):
            k = self.i % len(self.bufs)
            self.i += 1
            return self.bufs[k], "%s#%d" % (self.name, k)

    def ACT(out, in_, func, r, w, **kw):
        S.op("act", lambda e: e.activation(out=out, in_=in_, func=func, **kw), r, w)

    def TT(out, in0, in1, op, r, w):
        S.op("dve", lambda e: e.tensor_tensor(out=out, in0=in0, in1=in1, op=op), r, w)

    def TS(out, in0, s1, s2, op0, op1, r, w):
        if op1 is None:
            S.op("dve", lambda e: e.tensor_scalar(out=out, in0=in0, scalar1=s1, scalar2=None, op0=op0), r, w)
        else:
            S.op("dve", lambda e: e.tensor_scalar(out=out, in0=in0, scalar1=s1, scalar2=s2, op0=op0, op1=op1), r, w)

    def STT(out, in0, sc, in1, op0, op1, r, w):
        S.op("dve", lambda e: e.scalar_tensor_tensor(out=out, in0=in0, scalar=sc, in1=in1, op0=op0, op1=op1), r, w)

    def CP(out, in_, r, w):
        S.op("dve", lambda e: e.tensor_copy(out=out, in_=in_), r, w)

    def MM(specs, r, w):
        S.pe([(lambda e, o=o, a=a, b=b, s0=s0, s1=s1: e.matmul(o, lhsT=a, rhs=b, start=s0, stop=s1))
              for (o, a, b, s0, s1) in specs], r, w)

    def TR(specs, r, w):
        S.pe([(lambda e, o=o, a=a: e.transpose(out=o, in_=a, identity=IDB[:])) for (o, a) in specs], r, w)

    def POW(out, in0, r, w):
        tag = w[0] + "~sq"
        S.op("act", lambda e: e.activation(out=out, in_=in0, func=AF.Sqrt), r, [tag])
        S.op("dve", lambda e: e.reciprocal(out=out, in_=out), [tag], w)

    def MEMSET(ap, val, w):
        S.op("dve", lambda e: e.memset(ap, val), [], w)

    def DBG(name, ap, reads):
        if name not in dbg:
            return
        shape = list(ap.shape)
        d = nc.dram_tensor("dbg_" + name, shape, ap.dtype, kind="ExternalOutput").ap()
        dbg_d[name] = d
        S.dma("sp", lambda e: e.dma_start(out=d, in_=ap), "dbg_" + name, reads=reads)

    S.dma("sp", lambda e: e.dma_start(out=CST[:], in_=cst_d), "cst", writes=["cst"])
    for q4 in range(4):
        S.dma("sp", lambda e, q4=q4: e.dma_start(
            out=X[:, q4 * 4:(q4 + 1) * 4, :],
            in_=x_d[q4 * 512:(q4 + 1) * 512, :].rearrange("(a p) d -> p a d", p=128)),
            "x%d" % q4, writes=["x%d" % (q4 * 4 + a) for a in range(4)])
    CP(IDB[:], ident_f, ["cst"], ["idb"])
    MEMSET(ONEC[:], 1.0 / 512.0, ["onec"])
    MEMSET(ONER[:], 1.0, ["oner"])

    def prenorm_stats(tiles, slot):
        for g4 in range(2):
            so = slot * 24 + g4 * 4
            tg = "%d_%d" % (slot, g4)
            for i in range(4):
                tt = tiles[g4 * 4 + i]
                ACT(JUNK[:], X[:, tt, :], AF.Square, ["x%d" % tt], ["junkp", "mss" + tg], accum_out=MST[:, so + i:so + i + 1])
            TS(MST[:, so + 8:so + 12], MST[:, so:so + 4], 1.0 / D, EPS, ALU.mult, ALU.add, ["mss" + tg], ["mms" + tg])
            POW(MST[:, so + 16:so + 20], MST[:, so + 8:so + 12], ["mms" + tg], ["mrs" + tg])

    def prenorm(l, tiles, gcol, HTv, hn_alloc, hn_res, slot):
        for g4 in range(2):
            so = slot * 24 + g4 * 4
            tg = "%d_%d" % (slot, g4)
            for i in range(g4 * 4, g4 * 4 + 4):
                tt = tiles[i]
                TS(hn_alloc[:, i, :], X[:, tt, :], MST[:, so + 16 + (i - g4 * 4):so + 17 + (i - g4 * 4)], None, ALU.mult, None,
                   ["x%d" % tt, "mrs" + tg], hn_res(i))
            for kc in range(8):
                p, pr = pb()
                TR([(p[:, a * 128:(a + 1) * 128], hn_alloc[:, g4 * 4 + a, kc * 128:(kc + 1) * 128]) for a in range(4)],
                   [x for a in range(4) for x in hn_res(g4 * 4 + a)] + ["idb"], [pr])
                dst = HTv[:, kc, g4 * 512:(g4 + 1) * 512]
                res = ["ht%d_%d" % (kc, g4)]
                if kc % 2 == 0:
                    ACT(dst, p, AF.Copy, [pr, "pv"], res, scale=PV[:, gcol + kc:gcol + kc + 1])
                else:
                    TS(dst, p, PV[:, gcol + kc:gcol + kc + 1], None, ALU.mult, None, [pr, "pv"], res)

    def ffn_stats(q):
        sl_ = (q % 2) * 16
        for a in range(4):
            tt = q * 4 + a
            ACT(JUNK[:], X[:, tt, :], AF.Square, ["x%d" % tt], ["junkp", "fss%d" % (q % 2)],
                accum_out=PST[:, sl_ + a:sl_ + a + 1])
        TS(PST[:, sl_ + 4:sl_ + 8], PST[:, sl_:sl_ + 4], 1.0 / D, EPS, ALU.mult, ALU.add, ["fss%d" % (q % 2)], ["fms%d" % (q % 2)])
        POW(PST[:, sl_ + 8:sl_ + 12], PST[:, sl_ + 4:sl_ + 8], ["fms%d" % (q % 2)], ["frs%d" % (q % 2)])

    def postnorm(tt, Y, Yr, tmpR):
        junk = tmpR["junk"]
        for h in range(2):
            ACT(junk, Y[h], AF.Square, [Yr[h]], ["junk", "ss2_%d" % h], accum_out=SM_[:, 32 + h:33 + h])
        TT(SM_[:, 34:35], SM_[:, 32:33], SM_[:, 33:34], ALU.add, ["ss2_0", "ss2_1"], ["ms2"])
        TS(SM_[:, 35:36], SM_[:, 34:35], 1.0 / D, EPS, ALU.mult, ALU.add, ["ms2"], ["ms2b"])
        POW(SM_[:, 36:37], SM_[:, 35:36], ["ms2b"], ["rstd2"])
        for h in range(2):
            t, tr = tmpR["t"].get()
            STT(t, Y[h], SM_[:, 36:37], GBC[:, h * 512:(h + 1) * 512], ALU.mult, ALU.mult, [Yr[h], "rstd2", "gbc"], [tr])
            TT(X[:, tt, h * 512:(h + 1) * 512], X[:, tt, h * 512:(h + 1) * 512], t, ALU.add, ["x%d" % tt, tr], ["x%d" % tt])

    for l in range(n_layers):
        S.dma("sp", lambda e, l=l: e.dma_start(out=PV[:], in_=pv_d[l]), "pv", writes=["pv"])
        for hf in range(2):
            T0 = hf * 1024
            AR.push()
            ART = AR.alloc([4, 1024], BF16)
            AST = AR.alloc([4, 1024], BF16)
            ACFT = AR.alloc([4, 1024], BF16)

            AR.push()
            HN = AR.alloc([8, 1024], BF16)
            if l == 0 and hf == 0:
                prenorm_stats([i for i in range(8)], 0)
            prenorm(l, [hf * 8 + i for i in range(8)], PV_MIXPRE, HT, HN, lambda i: ["hn%d" % i], hf)
            AR.pop()
            htall = ["ht%d_%d" % (kc, g) for kc in range(8) for g in range(2)]
            if l == 0 and hf == 0:
                DBG("HT", HT[:], htall)

            AR.push()
            QT = AR.alloc([2, 1024], BF16)
            KT = AR.alloc([2, 1024], BF16)
            QXT = AR.alloc([2, 1024], BF16)
            ropeR = Rot("rope", [2, 512], F32)
            t1R = Rot("t1", [512], F32)
            t2R = Rot("t2", [512], F32)
            for which in ("q", "k"):
                W, wr = wblock(l, which)
                dstT = QT if which == "q" else KT
                for pt in range(2):
                    rp, rr = ropeR.get()
                    S.dma("sp", lambda e, rp=rp, src_=rope_d[:, :, T0 + pt * 512:T0 + (pt + 1) * 512].rearrange("t p n -> p t n"): e.dma_start(
                        out=rp, in_=src_),
                        "rope" + rr[-1], writes=[rr])
                    for c in range(2):
                        pa, par = pf()
                        MM([(pa, W[:, kc, c * 128:(c + 1) * 128], HT[:, kc, pt * 512:(pt + 1) * 512], kc == 0, kc == 7)
                            for kc in range(8)], [wr] + ["ht%d_%d" % (kc, pt) for kc in range(8)], [par])
                        pb_, pbr = pf()
                        MM([(pb_, W[:, kc, 256 + c * 128:256 + (c + 1) * 128], HT[:, kc, pt * 512:(pt + 1) * 512], kc == 0, kc == 7)
                            for kc in range(8)], [wr] + ["ht%d_%d" % (kc, pt) for kc in range(8)], [pbr])
                        t1, t1r = t1R.get()
                        t2, t2r = t2R.get()
                        TT(t1, pa, rp[:, 0, :], ALU.mult, [par, rr], [t1r])
                        TT(t2, pb_, rp[:, 1, :], ALU.mult, [pbr, rr], [t2r])
                        dres = "%sT%d_%d" % (which, c, pt)
                        TT(dstT[:, c, pt * 512:(pt + 1) * 512], t1, t2, ALU.add, [t1r, t2r], [dres])
                        if which == "q":
                            for r4 in range(4):
                                cs = slice(pt * 512 + r4 * 128, pt * 512 + (r4 + 1) * 128)
                                TT(QXT[:, c, cs], QT[:, c, cs], CST[:, C_XI + c * 128:C_XI + (c + 1) * 128], ALU.mult,
                                   [dres, "cst"], ["qxT%d_%d" % (c, pt)])
            if l == 0 and hf == 0:
                DBG("QT", QT, ["qT%d_%d" % (c, pt) for c in range(2) for pt in range(2)])
                DBG("KT", KT, ["kT%d_%d" % (c, pt) for c in range(2) for pt in range(2)])

            Wv, wvr = wblock(l, "v")
            Wg, wgr = wblock(l, "g")
            VbR = Rot("vb", [512], BF16)
            SGR = Rot("sg", [512], BF16)
            KZR = Rot("kz", [256], BF16)
            SMR = Rot("sm", [512], BF16)
            ONR = Rot("on", [512], F32)
            AAR = Rot("aa", [512], BF16)
            if hf == 0:
                MEMSET(RS[:], 0.0, ["rs"])
                MEMSET(RB[:], 0.0, ["rb"])
            def ret_front(i, mid_cb=None):
                n = hf * 8 + i
                pt = i // 4
                tok = slice(i * 128, (i + 1) * 128)
                htr = ["ht%d_%d" % (kc, pt) for kc in range(8)]
                pv_, pvr = pf(0)
                MM([(pv_, HT[:, kc, tok], Wv[:, kc, :], kc == 0, kc == 7) for kc in range(8)], htr + [wvr], [pvr])
                vb, vbr = VbR.get()
                CP(vb, pv_, [pvr], [vbr])
                pg, pgr = pf(1)
                MM([(pg, HT[:, kc, tok], Wg[:, kc, :], kc == 0, kc == 7) for kc in range(8)], htr + [wgr], [pgr])
                sg, sgr = SGR.get()
                ACT(sg, pg, AF.Silu, [pgr], [sgr])
                pk, pkr = pb()
                TR([(pk[:, c * 128:(c + 1) * 128], KT[:, c, tok]) for c in range(2)],
                   ["kT%d_%d" % (c, pt) for c in range(2)] + ["idb"], [pkr])
                kz, kzr = KZR.get()
                TT(kz, pk[:, 0:256], zsfull, ALU.mult, [pkr, "cst"], [kzr])
                psA, psAr = pf(2)
                psB, psBr = pf(3)
                HS = [0, 2, 1, 3]
                SL = [0, 2, 1, 3]
                sc_specs = []
                for h in range(4):
                    s_ = SL[h]
                    bank = psA if s_ < 2 else psB
                    sc_specs.append((bank[:, (s_ % 2) * 128:(s_ % 2) * 128 + 128],
                                     KT[(h % 2) * 64:(h % 2) * 64 + 64, h // 2, tok],
                                     QT[(h % 2) * 64:(h % 2) * 64 + 64, h // 2, tok], True, True))
                MM(sc_specs, ["kT%d_%d" % (c, pt) for c in range(2)] + ["qT%d_%d" % (c, pt) for c in range(2)], [psAr, psBr])
                sm, smr = SMR.get()
                TT(sm[:, 0:256], psA[:, 0:256], dmask[:, 0:256], ALU.mult, [psAr, "cst"], [smr + "a"])
                TT(sm[:, 256:512], psB[:, 0:256], dmask[:, 256:512], ALU.mult, [psBr, "cst"], [smr + "b"])
                if mid_cb is not None:
                    mid_cb()
                poA, poAr = pf(4)
                poB, poBr = pf(5)

                def obank(h):
                    s_ = SL[h]
                    return (poA if s_ < 2 else poB)[:, (s_ % 2) * 128:(s_ % 2) * 128 + 128]

                def obr(h):
                    return poAr if SL[h] < 2 else poBr

                specs = []
                for h in range(4):
                    s_ = SL[h]
                    specs.append((obank(h), sm[:, s_ * 128:(s_ + 1) * 128], vb[:, h * 128:(h + 1) * 128], True, n == 0))
                    if n > 0:
                        specs.append((obank(h),
                                      QXT[(h % 2) * 64:(h % 2) * 64 + 64, h // 2, tok],
                                      RB[(h % 2) * 64:(h % 2) * 64 + 64, h // 2, :], False, True))
                MM(specs, [smr + "a", smr + "b", vbr, "rb"] + ["qxT%d_%d" % (c, pt) for c in range(2)], [poAr, poBr])
                if n < 15:
                    pkv, pkvr = pf(2)
                    MM([(pkv[:, p * 256:(p + 1) * 256], kz[:, p * 128:(p + 1) * 128], vb[:, p * 256:(p + 1) * 256], True, True)
                        for p in range(2)], [kzr, vbr], [pkvr])
                    for p in range(2):
                        for j in range(2):
                            rows = slice(j * 64, (j + 1) * 64)
                            STT(RS[rows, p, :], RS[rows, p, :], CST[rows, C_GC + p:C_GC + p + 1],
                                pkv[rows, p * 256 + j * 128:p * 256 + (j + 1) * 128], ALU.mult, ALU.add,
                                ["rs", pkvr, "cst"], ["rs"])
                    CP(RB[:], RS[:], ["rs"], ["rb"])
                for h in range(4):
                    S.op("dve", lambda e, o_=SM_[:, 40 + h * 6:46 + h * 6], i_=obank(h): e.bn_stats(out=o_, in_=i_),
                         [obr(h)], ["bst%d" % h])
                for h in range(4):
                    S.op("dve", lambda e, o_=SM_[:, 20 + h * 2:22 + h * 2], i_=SM_[:, 40 + h * 6:46 + h * 6]: e.bn_aggr(out=o_, in_=i_),
                         ["bst%d" % h], ["mv%d" % h])
                mv3 = SM_[:, 20:28].rearrange("p (h t) -> p h t", t=2)
                TS(SM_[:, 28:32], mv3[:, :, 1], EPS, None, ALU.add, None, ["mv%d" % h for h in range(4)], ["ve"])
                POW(SM_[:, 4:8], SM_[:, 28:32], ["ve"], ["grstd"])
                STT(SM_[:, 12:16], mv3[:, :, 0], -1.0, SM_[:, 4:8], ALU.mult, ALU.mult, ["mv%d" % h for h in range(4)] + ["grstd"], ["gnmr"])
                on, onr = ONR.get()
                for h in range(4):
                    ACT(on[:, h * 128:(h + 1) * 128], obank(h), AF.Identity, [obr(h), "grstd", "gnmr"], [onr + str(h)],
                        scale=SM_[:, 4 + h:5 + h], bias=SM_[:, 12 + h:13 + h])
                return {"on": on, "onr": onr, "sg": sg, "sgr": sgr, "tok": tok, "pt": pt}

            def ret_aa(ctx):
                aa, aar = AAR.get()
                TT(aa, ctx["on"], ctx["sg"], ALU.mult, [ctx["onr"] + str(h) for h in range(4)] + [ctx["sgr"]], [aar])
                ctx["aa"], ctx["aar"] = aa, aar

            def ret_tail(ctx):
                aa, aar, tok, pt = ctx["aa"], ctx["aar"], ctx["tok"], ctx["pt"]
                pa2, pa2r = pb()
                TR([(pa2[:, k4 * 128:(k4 + 1) * 128], aa[:, k4 * 128:(k4 + 1) * 128]) for k4 in range(4)], [aar, "idb"], [pa2r])
                ACT(ART[:, :, tok], pa2.rearrange("p (k n) -> p k n", n=128), AF.Copy, [pa2r], ["art%d" % pt])

            rctx = ret_front(0)
            for i in range(8):
                if i + 1 < 8:
                    nctx = ret_front(i + 1, mid_cb=lambda c=rctx: ret_aa(c))
                else:
                    nctx = None
                    ret_aa(rctx)
                ret_tail(rctx)
                rctx = nctx
            if l == 0 and hf == 0:
                DBG("ART", ART, ["art0", "art1"])

            PjR = Rot("pj", [1026], BF16)
            BjR = Rot("bj", [1024], BF16)
            cxR = Rot("cx", [512], F32)
            DGR = Rot("dg", [3, 128], BF16)
            def sc_front(j):
                W, wr = wblock(l, "sc%d" % j)
                pj, pjr = PjR.get()
                bj, bjr = BjR.get()
                if hf == 0:
                    MEMSET(pj[:, 0:2], 0.0, [pjr + "h"])
                else:
                    CP(pj[:, 0:2], PH[:, j, :], ["ph%d" % j], [pjr + "h"])
                dg, dgr = DGR.get()
                for k in range(3):
                    TS(dg[:, k, :], ident_f, PV[:, PV_SCW + j * 3 + k:PV_SCW + j * 3 + k + 1], None, ALU.mult, None,
                       ["cst", "pv"], [dgr + str(k)])
                for pt in range(2):
                    htr = ["ht%d_%d" % (kc, pt) for kc in range(8)]
                    ps3 = []
                    for b in range(3):
                        p, pr = pf()
                        MM([(p, W[:, kc, b * 128:(b + 1) * 128], HT[:, kc, pt * 512:(pt + 1) * 512], kc == 0, kc == 7)
                            for kc in range(8)], htr + [wr], [pr])
                        ps3.append((p, pr))
                    ACT(bj[:, pt * 512:(pt + 1) * 512], ps3[0][0], AF.Copy, [ps3[0][1]], [bjr + str(pt)])
                    cx, cxr = cxR.get()
                    ACT(cx, ps3[1][0], AF.Copy, [ps3[1][1]], [cxr])
                    TT(pj[:, 2 + pt * 512:2 + (pt + 1) * 512], ps3[2][0], cx, ALU.mult, [ps3[2][1], cxr], [pjr + str(pt)])
                CP(PH[:, j, :], pj[:, 1024:1026], [pjr + "1", pjr + "h"], ["ph%d" % j])
                return (j, pj, pjr, bj, bjr, dg, dgr)

            def sc_tail(ctx):
                j, pj, pjr, bj, bjr, dg, dgr = ctx
                for pt in range(2):
                    p, pr = pf()
                    MM([(p, dg[:, k, :], pj[:, pt * 512 + k:pt * 512 + k + 512], k == 0, k == 2) for k in range(3)],
                       [dgr + "0", dgr + "1", dgr + "2", pjr + "h", pjr + "0", pjr + "1"], [pr])
                    TT(AST[:, j, pt * 512:(pt + 1) * 512], p, bj[:, pt * 512:(pt + 1) * 512], ALU.mult, [pr, bjr + str(pt)], ["ast%d" % pt])

            sctx = sc_front(0)
            for j in range(4):
                nctx = sc_front(j + 1) if j + 1 < 4 else None
                sc_tail(sctx)
                sctx = nctx
            AR.pop()
            if l == 0 and hf == 0:
                DBG("AST", AST, ["ast0", "ast1"])

            AR.push()
            G = AR.alloc([4, 1054], BF16)
            CB = AR.alloc([4, 1024], BF16)
            CSQ = AR.alloc([4, 1024], BF16)
            DG31R = Rot("dg31", [31, 128], BF16)
            sbR = Rot("sb", [512], F32)
            for j in range(4):
                W, wr = wblock(l, "cf%d" % j)
                if hf == 0:
                    MEMSET(G[:, j, 0:30], 0.0, ["g%dh" % j])
                else:
                    CP(G[:, j, 0:30], GH[:, j, :], ["gh%d" % j], ["g%dh" % j])
                for pt in range(2):
                    htr = ["ht%d_%d" % (kc, pt) for kc in range(8)]
                    p2 = []
                    for b in range(2):
                        p, pr = pf()
                        MM([(p, W[:, kc, b * 128:(b + 1) * 128], HT[:, kc, pt * 512:(pt + 1) * 512], kc == 0, kc == 7)
                            for kc in range(8)], htr + [wr], [pr])
                        p2.append((p, pr))
                    sb, sbr = sbR.get()
                    ACT(sb, p2[1][0], AF.Sigmoid, [p2[1][1]], [sbr])
                    TT(G[:, j, 30 + pt * 512:30 + (pt + 1) * 512], p2[0][0], sb, ALU.mult, [p2[0][1], sbr], ["g%d_%d" % (j, pt)])
                CP(GH[:, j, :], G[:, j, 1024:1054], ["g%d_1" % j, "g%dh" % j], ["gh%d" % j])
            def dg31_build(j):
                dg, dgr = DG31R.get()
                for k in range(31):
                    sc_ap = PV[:, PV_CFW + j * 31 + k:PV_CFW + j * 31 + k + 1]
                    TS(dg[:, k, :], ident_f, sc_ap, None, ALU.mult, None, ["cst", "pv"], [dgr + "_%d" % k])
                return dg, dgr

            rowA = AR.alloc([512], F32)
            rowB = AR.alloc([512], F32)
            rowC = AR.alloc([512], F32)
            rowD = AR.alloc([512], F32)
            tR = Rot("lnt", [512], F32)
            t2R_ = Rot("lnt2", [512], F32)
            bc = {}

            def ln_chain(pt):
                sl = slice(pt * 512, (pt + 1) * 512)
                pm, pmr = pf(0)
                MM([(pm[0:1, :], ONEC[:, 0:1], CB[:, j, sl], j == 0, j == 3) for j in range(4)],
                   ["onec"] + ["cb%d_%d" % (j, pt) for j in range(4)], [pmr])
                pe2, pe2r = pf(1)
                MM([(pe2[0:1, :], ONEC[:, 0:1], CSQ[:, j, sl], j == 0, j == 3) for j in range(4)],
                   ["onec"] + ["csq%d_%d" % (j, pt) for j in range(4)], [pe2r])
                ACT(rowA[0:1, :], pm[0:1, :], AF.Copy, [pmr], ["rowA"])
                ACT(rowB[0:1, :], pm[0:1, :], AF.Square, [pmr], ["rowB"])
                STT(rowB[0:1, :], pe2[0:1, :], EPS, rowB[0:1, :], ALU.add, ALU.subtract, [pe2r, "rowB"], ["rowB"])
                POW(rowC[0:1, :], rowB[0:1, :], ["rowB"], ["rowC"])
                STT(rowD[0:1, :], rowA[0:1, :], -1.0, rowC[0:1, :], ALU.mult, ALU.mult, ["rowA", "rowC"], ["rowD"])
                pr_, prr = pf(2 + 2 * pt)
                MM([(pr_, ONER[0:1, :], rowC[0:1, :], True, True)], ["oner", "rowC"], [prr])
                pn_, pnr = pf(3 + 2 * pt)
                MM([(pn_, ONER[0:1, :], rowD[0:1, :], True, True)], ["oner", "rowD"], [pnr])
                bc[pt] = (pr_, prr, pn_, pnr)

            def ln_norm(pt):
                sl = slice(pt * 512, (pt + 1) * 512)
                pr_, prr, pn_, pnr = bc[pt]
                for j in range(4):
                    t, tr = tR.get()
                    TT(t, pr_, CB[:, j, sl], ALU.mult, [prr, "cb%d_%d" % (j, pt)], [tr])
                    t2, t2r = t2R_.get()
                    TT(t2, pn_, t, ALU.add, [pnr, tr], [t2r])
                    ACT(ACFT[:, j, sl], t2, AF.Silu, [t2r, "pv"], ["acft%d" % pt],
                        scale=PV[:, PV_LNG + j:PV_LNG + j + 1], bias=PV[:, PV_LNB + j:PV_LNB + j + 1])

            dgc = dg31_build(0)
            for j in range(4):
                dgn = dg31_build(j + 1) if j + 1 < 4 else None
                dg, dgr = dgc
                for pt in range(2):
                    p, pr = pf(0) if (j == 3 and pt == 1) else pf()
                    MM([(p, dg[:, k, :], G[:, j, pt * 512 + k:pt * 512 + k + 512], k == 0, k == 30) for k in range(31)],
                       [dgr + "_%d" % k for k in range(31)] + ["g%dh" % j, "g%d_0" % j, "g%d_1" % j], [pr])
                    bias = PV[:, PV_CFB + j:PV_CFB + j + 1]
                    ACT(CB[:, j, pt * 512:(pt + 1) * 512], p, AF.Identity, [pr, "pv"], ["cb%d_%d" % (j, pt)], bias=bias)
                    ACT(CSQ[:, j, pt * 512:(pt + 1) * 512], p, AF.Square, [pr, "pv"], ["csq%d_%d" % (j, pt)], bias=bias)
                    if j == 3 and pt == 0:
                        ln_chain(0)
                dgc = dgn
            ln_chain(1)
            ln_norm(0)
            ln_norm(1)
            AR.pop()
            if l == 0 and hf == 0:
                DBG("ACFT", ACFT, ["acft0", "acft1"])

            AR.push()
            MT = AR.alloc([8, 1024], BF16)
            sgR = Rot("sgm", [512], F32, 3)
            mR = Rot("mm", [512], F32, 2)
            tmR = Rot("tm", [512], F32, 2)
            AB = [ART, AST, ACFT]
            ABn = ["art", "ast", "acft"]
            for f in range(8):
                Wgb, wgr_ = wblock(l, "gb%d" % f)
                wbr_ = wgr_
                Wg_ = Wgb[:, 0, 0:3072].rearrange("p (k n) -> p k n", n=384)
                Wb_ = Wgb[:, 0, 3072:4608].rearrange("p (k n) -> p k n", n=384)
                for pt in range(2):
                    sl = slice(pt * 512, (pt + 1) * 512)
                    htr = ["ht%d_%d" % (kc, pt) for kc in range(8)]
                    m, mr = mR.get()
                    for b in range(3):
                        pg_, pgr_ = pf()
                        MM([(pg_, Wg_[:, kc, b * 128:(b + 1) * 128], HT[:, kc, sl], kc == 0, kc == 7) for kc in range(8)],
                           htr + [wgr_], [pgr_])
                        py_, pyr_ = pf()
                        MM([(py_, Wb_[:, k4, b * 128:(b + 1) * 128], AB[b][:, k4, sl], k4 == 0, k4 == 3) for k4 in range(4)],
                           [ABn[b] + str(pt), wbr_], [pyr_])
                        sgm, sgmr = sgR.get()
                        ACT(sgm, pg_, AF.Sigmoid, [pgr_], [sgmr])
                        if b == 0:
                            TT(m, py_, sgm, ALU.mult, [pyr_, sgmr], [mr])
                        else:
                            tm, tmr = tmR.get()
                            TT(tm, py_, sgm, ALU.mult, [pyr_, sgmr], [tmr])
                            if b == 1:
                                TT(m, m, tm, ALU.add, [mr, tmr], [mr])
                            else:
                                TT(MT[:, f, sl], m, tm, ALU.add, [mr, tmr], ["mt%d" % pt])
            if l == 0 and hf == 0:
                DBG("MT", MT, ["mt0", "mt1"])

            S.dma("sp", lambda e, l=l: e.dma_start(out=GBC[:], in_=gbc_d[l, 0]), "gbc", writes=["gbc"])
            Wo0, wo0r = wblock(l, "wo0")
            Wo1, wo1r = wblock(l, "wo1")
            tmpR = {"junk": AR.alloc([512], BF16), "t": Rot("pnt", [512], F32, 2)}
            if hf == 0:
                prenorm_stats([8 + i for i in range(8)], 1)
            else:
                ffn_stats(0)
            for i in range(8):
                tt = hf * 8 + i
                pt = i // 4
                tok = slice(i * 128, (i + 1) * 128)
                Y, Yr = [], []
                for h, (Wo, wor) in enumerate(((Wo0, wo0r), (Wo1, wo1r))):
                    p, pr = pf()
                    MM([(p, MT[:, kc, tok], Wo[:, kc, :], kc == 0, kc == 7) for kc in range(8)], ["mt%d" % pt, wor], [pr])
                    Y.append(p)
                    Yr.append(pr)
                postnorm(tt, Y, Yr, tmpR)
            AR.pop()
            AR.pop()
            if l == 0 and hf == 0:
                DBG("X1", X[:, 0:8, :], ["x%d" % t for t in range(8)])

        AR.push()
        WD = AR.alloc([NFF, 1024], BF16)
        ACTT = AR.alloc([NFF, 512], BF16)
        HN2 = AR.alloc([2, 1024], BF16)
        UGR = Rot("ug", [514], BF16)
        UVR = Rot("uv", [514], BF16)
        DGF = Rot("dgf", [6, 128], BF16)
        slR = Rot("sl", [512], BF16)
        tmpR = {"junk": AR.alloc([512], BF16), "t": Rot("pnt", [512], F32, 2)}
        S.dma("sp", lambda e, l=l: e.dma_start(out=GBC[:], in_=gbc_d[l, 1]), "gbc", writes=["gbc"])
        wd_issued = 0

        def ffn_apply(q):
            sl_ = (q % 2) * 16
            g = q % 2
            for pair in range(2):
                for a2 in range(2):
                    a = pair * 2 + a2
                    tt = q * 4 + a
                    TS(HN2[:, a2, :], X[:, tt, :], PST[:, sl_ + 8 + a:sl_ + 9 + a], None, ALU.mult, None,
                       ["x%d" % tt, "frs%d" % (q % 2)], ["hn2_%d" % a2])
                for kc in range(8):
                    p, pr = pb()
                    TR([(p[:, a2 * 128:(a2 + 1) * 128], HN2[:, a2, kc * 128:(kc + 1) * 128]) for a2 in range(2)],
                       ["hn2_0", "hn2_1", "idb"], [pr])
                    dst = HT[:, kc, g * 512 + pair * 256:g * 512 + (pair + 1) * 256]
                    res = ["ht%d_%d_%d" % (kc, g, pair)]
                    if kc % 2 == 0:
                        ACT(dst, p[:, 0:256], AF.Copy, [pr, "pv"], res, scale=PV[:, PV_FFNPRE + kc:PV_FFNPRE + kc + 1])
                    else:
                        TS(dst, p[:, 0:256], PV[:, PV_FFNPRE + kc:PV_FFNPRE + kc + 1], None, ALU.mult, None, [pr, "pv"], res)

        ffn_apply(0)
        for qt in range(4):
            hg = qt % 2
            HTq = HT[:, :, hg * 512:(hg + 1) * 512]
            htr = ["ht%d_%d_%d" % (kc, hg, pr_) for kc in range(8) for pr_ in range(2)]
            def up_front(j):
                W, wr = wblock(l, "up%d" % j)
                ug, ugr = UGR.get()
                uv, uvr = UVR.get()
                if qt == 0:
                    MEMSET(ug[:, 0:2], 0.0, [ugr + "h"])
                    MEMSET(uv[:, 0:2], 0.0, [uvr + "h"])
                else:
                    CP(ug[:, 0:2], FH[:, j, 0:2], ["fh%d" % j], [ugr + "h"])
                    CP(uv[:, 0:2], FH[:, j, 2:4], ["fh%d" % j], [uvr + "h"])
                dg, dgr = DGF.get()
                for k in range(3):
                    TS(dg[:, k, :], ident_f, PV[:, PV_FGW + j * 3 + k:PV_FGW + j * 3 + k + 1], None, ALU.mult, None, ["cst", "pv"], [dgr + str(k)])
                    TS(dg[:, 3 + k, :], ident_f, PV[:, PV_FVW + j * 3 + k:PV_FVW + j * 3 + k + 1], None, ALU.mult, None, ["cst", "pv"], [dgr + str(3 + k)])
                pg_, pgr_ = pf()
                MM([(pg_, W[:, kc, 0:128], HTq[:, kc, :], kc == 0, kc == 7) for kc in range(8)], htr + [wr], [pgr_])
                pv2, pv2r = pf()
                MM([(pv2, W[:, kc, 128:256], HTq[:, kc, :], kc == 0, kc == 7) for kc in range(8)], htr + [wr], [pv2r])
                ACT(ug[:, 2:514], pg_, AF.Copy, [pgr_], [ugr])
                ACT(uv[:, 2:514], pv2, AF.Copy, [pv2r], [uvr])
                CP(FH[:, j, 0:2], ug[:, 512:514], [ugr, ugr + "h"], ["fh%d" % j])
                CP(FH[:, j, 2:4], uv[:, 512:514], [uvr, uvr + "h"], ["fh%d" % j])
                return (j, ug, ugr, uv, uvr, dg, dgr)

            def up_tail(ctx):
                j, ug, ugr, uv, uvr, dg, dgr = ctx
                pcg, pcgr = pf()
                MM([(pcg, dg[:, k, :], ug[:, k:k + 512], k == 0, k == 2) for k in range(3)],
                   [dgr + "0", dgr + "1", dgr + "2", ugr, ugr + "h"], [pcgr])
                pcv, pcvr = pf()
                MM([(pcv, dg[:, 3 + k, :], uv[:, k:k + 512], k == 0, k == 2) for k in range(3)],
                   [dgr + "3", dgr + "4", dgr + "5", uvr, uvr + "h"], [pcvr])
                sl_, slr = slR.get()
                ACT(sl_, pcg, AF.Silu, [pcgr], [slr])
                TT(ACTT[:, j, :], pcv, sl_, ALU.mult, [pcvr, slr], ["actt%d" % j])

            def wd_maybe(j):
                nonlocal wd_issued
                if qt == 0 and j % 2 == 1 and wd_issued < 11:
                    c = wd_issued
                    o, kc_, n_ = offs["wd%d" % c]
                    S.dma("pool", lambda e, o_=WD[:, 2 * c:2 * c + 2, :].rearrange("p k (a b) -> p (k a) b", b=512),
                          i_=wst_d[l, :, o:o + 2048].rearrange("p (a b) -> p a b", b=512): e.dma_start(out=o_, in_=i_),
                          "wd", writes=["wd"])
                    wd_issued += 1

            uctx = up_front(0)
            for j in range(NFF):
                wd_maybe(j)
                if qt < 3 and j == 6:
                    ffn_stats(qt + 1)
                if qt < 3 and j == 13:
                    ffn_apply(qt + 1)
                nctx = up_front(j + 1) if j + 1 < NFF else None
                up_tail(uctx)
                uctx = nctx
            if l == 0 and qt == 0:
                DBG("ACTT", ACTT, ["actt%d" % j for j in range(NFF)])
            if qt == 3 and l + 1 < n_layers:
                prenorm_stats([i for i in range(8)], 0)
            for a in range(4):
                tt = qt * 4 + a
                tok = slice(a * 128, (a + 1) * 128)
                Y, Yr = [], []
                for h in range(2):
                    p, pr = pf()
                    MM([(p, ACTT[:, kc, tok], WD[:, kc, h * 512:(h + 1) * 512], kc == 0, kc == NFF - 1) for kc in range(NFF)],
                       ["actt%d" % kc for kc in range(NFF)] + ["wd"], [pr])
                    Y.append(p)
                    Yr.append(pr)
                postnorm(tt, Y, Yr, tmpR)
            if l == n_layers - 1:
                S.dma("sp", lambda e, qt=qt: e.dma_start(
                    out=out_d[qt * 512:(qt + 1) * 512, :].rearrange("(a p) d -> p a d", p=128),
                    in_=X[:, qt * 4:(qt + 1) * 4, :]), "out%d" % qt, reads=["x%d" % (qt * 4 + a) for a in range(4)])
        AR.pop()
    S.barrier()
    S.emit()
    return nc, dbg_d, S, AR


_CACHE = {}


def kernel(**inputs):
    x = np.asarray(inputs["x"], np.float32)
    B = x.shape[0]
    wst = np.stack([prep_layer_stream(inputs, l) for l in range(NL)], axis=0)
    pv = np.stack([prep_pv(inputs, l) for l in range(NL)], axis=0)
    gbc = np.stack([np.stack([np.broadcast_to(np.asarray(inputs["norm_mix_post"][l], np.float32)[None, :], (128, D)),
                              np.broadcast_to(np.asarray(inputs["norm_ffn_post"][l], np.float32)[None, :], (128, D))], axis=0)
                    for l in range(NL)], axis=0)
    gbc = np.ascontiguousarray(gbc)
    cst, rope = make_consts()
    nc = build()[0]
    in_maps = [{"x": np.ascontiguousarray(x[b]), "wst": wst, "pv": pv, "gbc": gbc, "cst": cst, "rope": rope} for b in range(B)]
    res = run_bass_kernel_spmd(nc, in_maps, core_ids=list(range(B)))
    return np.stack([np.asarray(r["out"], np.float32) for r in res.results], axis=0)
```

```python
import contextlib
import numpy as np
import concourse.bass as bass
import concourse.mybir as mybir
from concourse.bass_utils import run_bass_kernel_spmd

F32 = mybir.dt.float32
BF16 = mybir.dt.bfloat16
AF = mybir.ActivationFunctionType
ALU = mybir.AluOpType

D = 1024
SEQ = 2048
NL = 2
NFF = 22
EPS = 1e-6
NSLOT = 3
SLOTC = 4608
ENGS = ("pe", "act", "dve", "pool", "sp")

PV_MIXPRE, PV_FFNPRE, PV_SCW, PV_CFW, PV_CFB, PV_LNG, PV_LNB, PV_FGW, PV_FVW = 0, 8, 16, 28, 152, 156, 160, 164, 230
NPV = 296
C_ID, C_DM, C_XI, C_ZS, C_GC = 0, 128, 640, 896, 1152
NCST = 1154


class Sched:
    def __init__(self, nc, self_sync=("act", "dve", "pool")):
        self.nc = nc
        self.prog = {e: [] for e in ENGS}
        self.cnt = {e: 0 for e in ENGS}
        self.waited = {}
        self.lastw = {}
        self.readers = {}
        self.self_sync = set(self_sync)
        self.dma_sems = {}
        self.n_instr = 0

    def _need(self, eng, reads, writes, skip=None):
        need = {}

        def add(dep):
            if dep is None:
                return
            d, v = dep
            if need.get(d, 0) < v:
                need[d] = v

        for r in reads:
            add(self.lastw.get(r))
            if r.startswith("pf") or r.startswith("pb"):
                for d, v in self.readers.get(r, {}).items():
                    if d != eng:
                        add((d, v))
        for w in writes:
            add(self.lastw.get(w))
            for d, v in self.readers.get(w, {}).items():
                add((d, v))
        out = []
        for d, v in need.items():
            if d == skip:
                continue
            if d == eng and eng not in self.self_sync:
                continue
            key = (eng, d)
            if self.waited.get(key, 0) >= v:
                continue
            self.waited[key] = v
            out.append((d, v))
        return out

    def _mark(self, who, idx, reads, writes):
        for r in reads:
            self.readers.setdefault(r, {})[who] = idx
        for w in writes:
            self.lastw[w] = (who, idx)
            self.readers[w] = {}

    def op(self, eng, fn, reads=(), writes=()):
        waits = self._need(eng, reads, writes)
        self.cnt[eng] += 1
        self.prog[eng].append((waits, fn, ("eng", eng, 1)))
        self._mark(eng, self.cnt[eng], reads, writes)
        self.n_instr += 1

    def pe(self, fns, reads=(), writes=()):
        waits = self._need("pe", reads, writes)
        self.cnt["pe"] += 1
        n = len(fns)
        for i, fn in enumerate(fns):
            self.prog["pe"].append((waits if i == 0 else [], fn, ("eng", "pe", 1) if i == n - 1 else None))
        self._mark("pe", self.cnt["pe"], reads, writes)
        self.n_instr += n

    def dma(self, queue, fn, semname, reads=(), writes=()):
        d = "dma:" + semname
        waits = self._need(queue, reads, writes, skip=d)
        c = self.dma_sems.setdefault(semname, [0])
        c[0] += 16
        self.prog[queue].append((waits, fn, ("dma", semname, 16)))
        self._mark(d, c[0], reads, writes)
        self.n_instr += 1

    def barrier(self, pool_wait=True):
        snap = dict(self.cnt)
        dsnap = {"dma:" + k: v[0] for k, v in self.dma_sems.items()}
        for e in ENGS:
            if e == "pe":
                continue
            if e == "pool" and not pool_wait:
                continue
            waits = []
            for d, v in list(snap.items()) + list(dsnap.items()):
                if d != e and v > 0 and self.waited.get((e, d), 0) < v:
                    self.waited[(e, d)] = v
                    waits.append((d, v))
            if waits:
                self.prog[e].append((waits, None, None))

    def emit(self):
        nc = self.nc
        with nc.cleanup_on_exit():
            sems = {}
            for e in ENGS:
                sems[e] = nc.alloc_semaphore(name="s_" + e)
            for k in self.dma_sems:
                sems["dma:" + k] = nc.alloc_semaphore(name="d_" + k)
            for s_ in sems.values():
                nc.gpsimd.sem_clear(s_)
            nc.all_engine_barrier()
            with nc.Block() as block:

                def run(eng_name):
                    def body(eng):
                        for waits, fn, inc in self.prog[eng_name]:
                            for d, v in waits:
                                eng.wait_ge(sems[d], v)
                            if fn is None:
                                continue
                            ins = fn(eng)
                            if inc is not None:
                                kind, name, n = inc
                                ins.then_inc(sems[name] if kind == "eng" else sems["dma:" + name], n)
                    return body

                block.tensor(run("pe"))
                block.scalar(run("act"))
                block.vector(run("dve"))
                block.gpsimd(run("pool"))
                block.sync(run("sp"))


def _blk(w):
    K, n = w.shape
    return np.ascontiguousarray(w.reshape(K // 128, 128, n).transpose(1, 0, 2)).reshape(128, -1)


def weight_blocks():
    blocks = [("q", 8, 512), ("k", 8, 512), ("v", 8, 512), ("g", 8, 512)]
    blocks += [("sc%d" % j, 8, 384) for j in range(4)]
    blocks += [("cf%d" % j, 8, 256) for j in range(4)]
    blocks += [("gb%d" % f, 1, 4608) for f in range(8)]
    blocks += [("wo0", 8, 512), ("wo1", 8, 512)]
    blocks += [("up%d" % j, 8, 256) for j in range(NFF)]
    blocks += [("wd%d" % c, 2, 1024) for c in range(11)]
    offs = {}
    o = 0
    for name, kc, n in blocks:
        offs[name] = (o, kc, n)
        o += kc * n
    return blocks, offs, o


def prep_layer_stream(inp, l):
    w_in = np.asarray(inp["w_in"][l], dtype=np.float32)
    perm = np.array([h * 64 + (i + 32) % 64 for h in range(4) for i in range(64)])
    q = w_in[:, 0:256]
    k = w_in[:, 256:512]
    parts = {}
    parts["q"] = np.concatenate([q, q[:, perm]], axis=1)
    parts["k"] = np.concatenate([k, k[:, perm]], axis=1)
    parts["v"] = w_in[:, 512:1024]
    parts["g"] = w_in[:, 1024:1536]
    for j in range(4):
        s = slice(j * 128, (j + 1) * 128)
        parts["sc%d" % j] = np.concatenate([w_in[:, 1536:2048][:, s], w_in[:, 2048:2560][:, s], w_in[:, 2560:3072][:, s]], axis=1)
        parts["cf%d" % j] = np.concatenate([w_in[:, 3072:3584][:, s], w_in[:, 3584:4096][:, s]], axis=1)
    wro = np.asarray(inp["w_ret_out"][l], np.float32)
    wso = np.asarray(inp["w_sc_out"][l], np.float32)
    wco = np.asarray(inp["w_cf_out"][l], np.float32)
    for f in range(8):
        s = slice(f * 128, (f + 1) * 128)
        gt = np.concatenate([w_in[:, 4096 + b * 1024: 4096 + (b + 1) * 1024][:, s] for b in range(3)], axis=1)
        bo = np.concatenate([wro[:, s], wso[:, s], wco[:, s]], axis=1)
        parts["gb%d" % f] = np.concatenate([_blk(gt), _blk(bo)], axis=1)
    wo = np.asarray(inp["w_o"][l], np.float32)
    parts["wo0"] = wo[:, 0:512]
    parts["wo1"] = wo[:, 512:1024]
    wup = np.asarray(inp["w_up"][l], np.float32)
    for j in range(NFF):
        s = slice(j * 128, (j + 1) * 128)
        parts["up%d" % j] = np.concatenate([wup[:, :2816][:, s], wup[:, 2816:][:, s]], axis=1)
    wd = np.asarray(inp["w_down"][l], np.float32)
    for c in range(11):
        parts["wd%d" % c] = wd[c * 256:(c + 1) * 256, :]
    blocks, offs, total = weight_blocks()
    out = np.empty((128, total), np.float32)
    for name, kc, n in blocks:
        o = offs[name][0]
        out[:, o:o + kc * n] = parts[name] if name.startswith("gb") else _blk(parts[name])
    return out


def prep_pv(inp, l):
    pv = np.zeros((128, NPV), np.float32)

    def fm(vec, nchunk):
        return np.asarray(vec, np.float32).reshape(nchunk, 128).T

    pv[:, PV_MIXPRE:PV_MIXPRE + 8] = fm(inp["norm_mix_pre"][l], 8)
    pv[:, PV_FFNPRE:PV_FFNPRE + 8] = fm(inp["norm_ffn_pre"][l], 8)
    scw = np.asarray(inp["sc_conv_w"][l], np.float32)
    pv[:, PV_SCW:PV_SCW + 12] = scw.reshape(3, 4, 128).transpose(2, 1, 0).reshape(128, 12)
    cfw = np.asarray(inp["cf_conv_w"][l], np.float32)
    pv[:, PV_CFW:PV_CFW + 124] = cfw.reshape(31, 4, 128).transpose(2, 1, 0).reshape(128, 124)
    pv[:, PV_CFB:PV_CFB + 4] = fm(inp["cf_conv_b"][l], 4)
    pv[:, PV_LNG:PV_LNG + 4] = fm(inp["cf_ln_g"][l], 4)
    pv[:, PV_LNB:PV_LNB + 4] = fm(inp["cf_ln_b"][l], 4)
    fw = np.asarray(inp["ffn_conv_w"][l], np.float32)
    pv[:, PV_FGW:PV_FGW + 66] = fw[:, :2816].reshape(3, NFF, 128).transpose(2, 1, 0).reshape(128, 66)
    pv[:, PV_FVW:PV_FVW + 66] = fw[:, 2816:].reshape(3, NFF, 128).transpose(2, 1, 0).reshape(128, 66)
    return pv


def make_consts():
    cst = np.zeros((128, NCST), np.float64)
    cst[:, C_ID:C_ID + 128] = np.eye(128)
    gam = 1.0 - np.exp2(-5.0 - np.arange(4))
    m = np.arange(128)[:, None]
    c = np.arange(128)[None, :]
    for h in range(4):
        dm = np.where(c >= m, gam[h] ** np.maximum(c - m, 0), 0.0) * 0.125
        sl_ = [0, 2, 1, 3][h]
        cst[:, C_DM + sl_ * 128:C_DM + (sl_ + 1) * 128] = dm
        cst[:, C_ZS + h * 64:C_ZS + (h + 1) * 64] = (0.125 * gam[h] ** (127 - np.arange(128)))[:, None]
    p = np.arange(128)
    for ch in range(2):
        hh = ch * 2 + p // 64
        xi = gam[hh][:, None] ** (np.arange(128)[None, :] + 1.0)
        cst[:, C_XI + ch * 128:C_XI + (ch + 1) * 128] = xi
        cst[:, C_GC + ch] = gam[hh] ** 128
    inv = 10000.0 ** (-np.arange(32) / 32.0)
    t = np.arange(SEQ)[None, :]
    ang = t * inv[p % 32][:, None]
    sign = np.where((p % 64) < 32, -1.0, 1.0)[:, None]
    rope = np.stack([np.cos(ang), sign * np.sin(ang)], axis=0)
    return cst.astype(np.float32), rope.astype(np.float32)


class Arena:
    def __init__(self, nc, S, nbytes):
        self.t = nc.alloc_sbuf_tensor("arena", [128, nbytes // 2], BF16)
        self.top = 0
        self.cap = nbytes
        self.stack = []
        self.S = S
        self.peak = 0

    def push(self):
        self.stack.append(self.top)

    def pop(self, pool_wait=True):
        self.S.barrier(pool_wait=pool_wait)
        self.top = self.stack.pop()

    def alloc(self, shape, dtype):
        n = int(np.prod(shape))
        esz = 4 if dtype == F32 else 2
        nb = (n * esz + 63) // 64 * 64
        off = self.top
        self.top += nb
        self.peak = max(self.peak, self.top)
        assert self.top <= self.cap, ("arena overflow", self.top, self.cap)
        ap = self.t[:, off // 2: off // 2 + (n * esz) // 2]
        if dtype == F32:
            ap = ap.bitcast(F32)
        if len(shape) == 2:
            names = "a b"
        else:
            names = "a b c"
        if len(shape) == 1:
            return ap
        kw = {"a": shape[0]} if len(shape) == 2 else {"a": shape[0], "b": shape[1]}
        return ap.rearrange("p (%s) -> p %s" % (names, names), **kw)


def build(n_layers=NL, dbg=()):
    nc = bass.Bass("TRN2", target_bir_lowering=False)
    blocks, offs, WCOLS = weight_blocks()
    x_d = nc.dram_tensor("x", [SEQ, D], F32, kind="ExternalInput").ap()
    wst_d = nc.dram_tensor("wst", [n_layers, 128, WCOLS], F32, kind="ExternalInput").ap()
    pv_d = nc.dram_tensor("pv", [n_layers, 128, NPV], F32, kind="ExternalInput").ap()
    gbc_d = nc.dram_tensor("gbc", [n_layers, 2, 128, D], F32, kind="ExternalInput").ap()
    cst_d = nc.dram_tensor("cst", [128, NCST], F32, kind="ExternalInput").ap()
    rope_d = nc.dram_tensor("rope", [2, 128, SEQ], F32, kind="ExternalInput").ap()
    out_d = nc.dram_tensor("out", [SEQ, D], F32, kind="ExternalOutput").ap()
    dbg_d = {}

    S = Sched(nc)

    X = nc.alloc_sbuf_tensor("X", [128, 16, D], F32)
    HT = nc.alloc_sbuf_tensor("HT", [128, 8, 1024], BF16)
    RING = nc.alloc_sbuf_tensor("RING", [128, NSLOT, SLOTC], BF16)
    CST = nc.alloc_sbuf_tensor("CST", [128, NCST], F32)
    PV = nc.alloc_sbuf_tensor("PVt", [128, NPV], F32)
    GBC = nc.alloc_sbuf_tensor("GBC", [128, D], F32)
    IDB = nc.alloc_sbuf_tensor("IDB", [128, 128], BF16)
    ONEC = nc.alloc_sbuf_tensor("ONEC", [128, 2], BF16)
    ONER = nc.alloc_sbuf_tensor("ONER", [1, 128], F32)
    RS = nc.alloc_sbuf_tensor("RS", [128, 2, 128], F32)
    RB = nc.alloc_sbuf_tensor("RB", [128, 2, 128], BF16)
    PH = nc.alloc_sbuf_tensor("PH", [128, 4, 2], BF16)
    GH = nc.alloc_sbuf_tensor("GH", [128, 4, 30], BF16)
    FH = nc.alloc_sbuf_tensor("FH", [128, NFF, 4], BF16)
    SM_ = nc.alloc_sbuf_tensor("SMALL", [128, 64], F32)
    PST = nc.alloc_sbuf_tensor("PST", [128, 32], F32)
    MST = nc.alloc_sbuf_tensor("MST", [128, 48], F32)
    JUNK = nc.alloc_sbuf_tensor("JUNK", [128, 1024], BF16)
    PSF = nc.alloc_psum_tensor("PSF", [128, 6, 512], F32)
    PSB = nc.alloc_psum_tensor("PSB", [128, 2, 1024], BF16)
    AR = Arena(nc, S, 85 * 1024)

    ident_f = CST[:, C_ID:C_ID + 128]
    dmask = CST[:, C_DM:C_DM + 512]
    zsfull = CST[:, C_ZS:C_ZS + 256]

    st = {"pf": 0, "pb": 0, "blk": 0, "issued": 0}
    stream = []
    for l in range(n_layers):
        for hf in range(2):
            for name, kc, n in blocks:
                if not (name.startswith("up") or name.startswith("wd")):
                    stream.append((l, name))
        for qt in range(4):
            for j in range(NFF):
                stream.append((l, "up%d" % j))

    def pf(fixed=None):
        if fixed is not None:
            return PSF[:, fixed, :], "pf%d" % fixed
        b = st["pf"] % 6
        st["pf"] += 1
        return PSF[:, b, :], "pf%d" % b

    def pb():
        b = st["pb"] % 2
        st["pb"] += 1
        return PSB[:, b, 0:512], "pb%d" % b

    def issue_block(i):
        l, name = stream[i]
        o, kc, n = offs[name]
        s = i % NSLOT
        cols = kc * n
        S.dma("pool", lambda e: e.dma_start(
            out=RING[:, s, 0:cols].rearrange("p (a b) -> p a b", b=512),
            in_=wst_d[l, :, o:o + cols].rearrange("p (a b) -> p a b", b=512)),
            "ring%d" % s, writes=["ring%d" % s])

    def wblock(l, name):
        i = st["blk"]
        assert stream[i] == (l, name), (stream[i], l, name)
        st["blk"] += 1
        while st["issued"] <= min(i + 1, len(stream) - 1):
            issue_block(st["issued"])
            st["issued"] += 1
        o, kc, n = offs[name]
        s = i % NSLOT
        return RING[:, s, 0:kc * n].rearrange("p (k n) -> p k n", n=n), "ring%d" % s

    class Rot:
        def __init__(self, name, shape, dtype, n=2):
            self.bufs = [AR.alloc(shape, dtype) for _ in range(n)]
            self.name = name
            self.i = 0

        def get(self):
            k = self.i % len(self.bufs)
            self.i += 1
            return self.bufs[k], "%s#%d" % (self.name, k)

    def ACT(out, in_, func, r, w, **kw):
        S.op("act", lambda e: e.activation(out=out, in_=in_, func=func, **kw), r, w)

    def TT(out, in0, in1, op, r, w):
        S.op("dve", lambda e: e.tensor_tensor(out=out, in0=in0, in1=in1, op=op), r, w)

    def TS(out, in0, s1, s2, op0, op1, r, w):
        if op1 is None:
            S.op("dve", lambda e: e.tensor_scalar(out=out, in0=in0, scalar1=s1, scalar2=None, op0=op0), r, w)
        else:
            S.op("dve", lambda e: e.tensor_scalar(out=out, in0=in0, scalar1=s1, scalar2=s2, op0=op0, op1=op1), r, w)

    def STT(out, in0, sc, in1, op0, op1, r, w):
        S.op("dve", lambda e: e.scalar_tensor_tensor(out=out, in0=in0, scalar=sc, in1=in1, op0=op0, op1=op1), r, w)

    def CP(out, in_, r, w):
        S.op("dve", lambda e: e.tensor_copy(out=out, in_=in_), r, w)

    def MM(specs, r, w):
        S.pe([(lambda e, o=o, a=a, b=b, s0=s0, s1=s1: e.matmul(o, lhsT=a, rhs=b, start=s0, stop=s1))
              for (o, a, b, s0, s1) in specs], r, w)

    def TR(specs, r, w):
        S.pe([(lambda e, o=o, a=a: e.transpose(out=o, in_=a, identity=IDB[:])) for (o, a) in specs], r, w)

    def POW(out, in0, r, w):
        tag = w[0] + "~sq"
        S.op("act", lambda e: e.activation(out=out, in_=in0, func=AF.Sqrt), r, [tag])
        S.op("dve", lambda e: e.reciprocal(out=out, in_=out), [tag], w)

    def MEMSET(ap, val, w):
        S.op("dve", lambda e: e.memset(ap, val), [], w)

    def DBG(name, ap, reads):
        if name not in dbg:
            return
        shape = list(ap.shape)
        d = nc.dram_tensor("dbg_" + name, shape, ap.dtype, kind="ExternalOutput").ap()
        dbg_d[name] = d
        S.dma("sp", lambda e: e.dma_start(out=d, in_=ap), "dbg_" + name, reads=reads)

    S.dma("sp", lambda e: e.dma_start(out=CST[:], in_=cst_d), "cst", writes=["cst"])
    for q4 in range(4):
        S.dma("sp", lambda e, q4=q4: e.dma_start(
            out=X[:, q4 * 4:(q4 + 1) * 4, :],
            in_=x_d[q4 * 512:(q4 + 1) * 512, :].rearrange("(a p) d -> p a d", p=128)),
            "x%d" % q4, writes=["x%d" % (q4 * 4 + a) for a in range(4)])
    CP(IDB[:], ident_f, ["cst"], ["idb"])
    MEMSET(ONEC[:], 1.0 / 512.0, ["onec"])
    MEMSET(ONER[:], 1.0, ["oner"])

    def prenorm_stats(tiles, slot):
        for g4 in range(2):
            so = slot * 24 + g4 * 4
            tg = "%d_%d" % (slot, g4)
            for i in range(4):
                tt = tiles[g4 * 4 + i]
                ACT(JUNK[:], X[:, tt, :], AF.Square, ["x%d" % tt], ["junkp", "mss" + tg], accum_out=MST[:, so + i:so + i + 1])
            TS(MST[:, so + 8:so + 12], MST[:, so:so + 4], 1.0 / D, EPS, ALU.mult, ALU.add, ["mss" + tg], ["mms" + tg])
            POW(MST[:, so + 16:so + 20], MST[:, so + 8:so + 12], ["mms" + tg], ["mrs" + tg])

    def prenorm(l, tiles, gcol, HTv, hn_alloc, hn_res, slot):
        for g4 in range(2):
            so = slot * 24 + g4 * 4
            tg = "%d_%d" % (slot, g4)
            for i in range(g4 * 4, g4 * 4 + 4):
                tt = tiles[i]
                TS(hn_alloc[:, i, :], X[:, tt, :], MST[:, so + 16 + (i - g4 * 4):so + 17 + (i - g4 * 4)], None, ALU.mult, None,
                   ["x%d" % tt, "mrs" + tg], hn_res(i))
            for kc in range(8):
                p, pr = pb()
                TR([(p[:, a * 128:(a + 1) * 128], hn_alloc[:, g4 * 4 + a, kc * 128:(kc + 1) * 128]) for a in range(4)],
                   [x for a in range(4) for x in hn_res(g4 * 4 + a)] + ["idb"], [pr])
                dst = HTv[:, kc, g4 * 512:(g4 + 1) * 512]
                res = ["ht%d_%d" % (kc, g4)]
                if kc % 2 == 0:
                    ACT(dst, p, AF.Copy, [pr, "pv"], res, scale=PV[:, gcol + kc:gcol + kc + 1])
                else:
                    TS(dst, p, PV[:, gcol + kc:gcol + kc + 1], None, ALU.mult, None, [pr, "pv"], res)

    def ffn_stats(q):
        sl_ = (q % 2) * 16
        for a in range(4):
            tt = q * 4 + a
            ACT(JUNK[:], X[:, tt, :], AF.Square, ["x%d" % tt], ["junkp", "fss%d" % (q % 2)],
                accum_out=PST[:, sl_ + a:sl_ + a + 1])
        TS(PST[:, sl_ + 4:sl_ + 8], PST[:, sl_:sl_ + 4], 1.0 / D, EPS, ALU.mult, ALU.add, ["fss%d" % (q % 2)], ["fms%d" % (q % 2)])
        POW(PST[:, sl_ + 8:sl_ + 12], PST[:, sl_ + 4:sl_ + 8], ["fms%d" % (q % 2)], ["frs%d" % (q % 2)])

    def postnorm(tt, Y, Yr, tmpR):
        junk = tmpR["junk"]
        for h in range(2):
            ACT(junk, Y[h], AF.Square, [Yr[h]], ["junk", "ss2_%d" % h], accum_out=SM_[:, 32 + h:33 + h])
        TT(SM_[:, 34:35], SM_[:, 32:33], SM_[:, 33:34], ALU.add, ["ss2_0", "ss2_1"], ["ms2"])
        TS(SM_[:, 35:36], SM_[:, 34:35], 1.0 / D, EPS, ALU.mult, ALU.add, ["ms2"], ["ms2b"])
        POW(SM_[:, 36:37], SM_[:, 35:36], ["ms2b"], ["rstd2"])
        for h in range(2):
            t, tr = tmpR["t"].get()
            STT(t, Y[h], SM_[:, 36:37], GBC[:, h * 512:(h + 1) * 512], ALU.mult, ALU.mult, [Yr[h], "rstd2", "gbc"], [tr])
            TT(X[:, tt, h * 512:(h + 1) * 512], X[:, tt, h * 512:(h + 1) * 512], t, ALU.add, ["x%d" % tt, tr], ["x%d" % tt])

    for l in range(n_layers):
        S.dma("sp", lambda e, l=l: e.dma_start(out=PV[:], in_=pv_d[l]), "pv", writes=["pv"])
        for hf in range(2):
            T0 = hf * 1024
            AR.push()
            ART = AR.alloc([4, 1024], BF16)
            AST = AR.alloc([4, 1024], BF16)
            ACFT = AR.alloc([4, 1024], BF16)

            AR.push()
            HN = AR.alloc([8, 1024], BF16)
            if l == 0 and hf == 0:
                prenorm_stats([i for i in range(8)], 0)
            prenorm(l, [hf * 8 + i for i in range(8)], PV_MIXPRE, HT, HN, lambda i: ["hn%d" % i], hf)
            AR.pop(pool_wait=False)
            htall = ["ht%d_%d" % (kc, g) for kc in range(8) for g in range(2)]
            if l == 0 and hf == 0:
                DBG("HT", HT[:], htall)

            AR.push()
            QT = AR.alloc([2, 1024], BF16)
            KT = AR.alloc([2, 1024], BF16)
            QXT = AR.alloc([2, 1024], BF16)
            ropeR = Rot("rope", [2, 512], F32)
            t1R = Rot("t1", [512], F32)
            t2R = Rot("t2", [512], F32)
            for which in ("q", "k"):
                W, wr = wblock(l, which)
                dstT = QT if which == "q" else KT
                for pt in range(2):
                    rp, rr = ropeR.get()
                    S.dma("sp", lambda e, rp=rp, src_=rope_d[:, :, T0 + pt * 512:T0 + (pt + 1) * 512].rearrange("t p n -> p t n"): e.dma_start(
                        out=rp, in_=src_),
                        "rope" + rr[-1], writes=[rr])
                    for c in range(2):
                        pa, par = pf()
                        MM([(pa, W[:, kc, c * 128:(c + 1) * 128], HT[:, kc, pt * 512:(pt + 1) * 512], kc == 0, kc == 7)
                            for kc in range(8)], [wr] + ["ht%d_%d" % (kc, pt) for kc in range(8)], [par])
                        pb_, pbr = pf()
                        MM([(pb_, W[:, kc, 256 + c * 128:256 + (c + 1) * 128], HT[:, kc, pt * 512:(pt + 1) * 512], kc == 0, kc == 7)
                            for kc in range(8)], [wr] + ["ht%d_%d" % (kc, pt) for kc in range(8)], [pbr])
                        t1, t1r = t1R.get()
                        t2, t2r = t2R.get()
                        TT(t1, pa, rp[:, 0, :], ALU.mult, [par, rr], [t1r])
                        TT(t2, pb_, rp[:, 1, :], ALU.mult, [pbr, rr], [t2r])
                        dres = "%sT%d_%d" % (which, c, pt)
                        TT(dstT[:, c, pt * 512:(pt + 1) * 512], t1, t2, ALU.add, [t1r, t2r], [dres])
                        if which == "q":
                            for r4 in range(4):
                                cs = slice(pt * 512 + r4 * 128, pt * 512 + (r4 + 1) * 128)
                                TT(QXT[:, c, cs], QT[:, c, cs], CST[:, C_XI + c * 128:C_XI + (c + 1) * 128], ALU.mult,
                                   [dres, "cst"], ["qxT%d_%d" % (c, pt)])
            if l == 0 and hf == 0:
                DBG("QT", QT, ["qT%d_%d" % (c, pt) for c in range(2) for pt in range(2)])
                DBG("KT", KT, ["kT%d_%d" % (c, pt) for c in range(2) for pt in range(2)])

            Wv, wvr = wblock(l, "v")
            Wg, wgr = wblock(l, "g")
            VbR = Rot("vb", [512], BF16)
            SGR = Rot("sg", [512], BF16)
            KZR = Rot("kz", [256], BF16)
            SMR = Rot("sm", [512], BF16)
            ONR = Rot("on", [512], F32)
            AAR = Rot("aa", [512], BF16)
            if hf == 0:
                MEMSET(RS[:], 0.0, ["rs"])
                MEMSET(RB[:], 0.0, ["rb"])
            def ret_front(i, mid_cb=None):
                n = hf * 8 + i
                pt = i // 4
                tok = slice(i * 128, (i + 1) * 128)
                htr = ["ht%d_%d" % (kc, pt) for kc in range(8)]
                pv_, pvr = pf(0)
                MM([(pv_, HT[:, kc, tok], Wv[:, kc, :], kc == 0, kc == 7) for kc in range(8)], htr + [wvr], [pvr])
                vb, vbr = VbR.get()
                CP(vb, pv_, [pvr], [vbr])
                pg, pgr = pf(1)
                MM([(pg, HT[:, kc, tok], Wg[:, kc, :], kc == 0, kc == 7) for kc in range(8)], htr + [wgr], [pgr])
                sg, sgr = SGR.get()
                ACT(sg, pg, AF.Silu, [pgr], [sgr])
                pk, pkr = pb()
                TR([(pk[:, c * 128:(c + 1) * 128], KT[:, c, tok]) for c in range(2)],
                   ["kT%d_%d" % (c, pt) for c in range(2)] + ["idb"], [pkr])
                kz, kzr = KZR.get()
                TT(kz, pk[:, 0:256], zsfull, ALU.mult, [pkr, "cst"], [kzr])
                psA, psAr = pf(2)
                psB, psBr = pf(3)
                HS = [0, 2, 1, 3]
                SL = [0, 2, 1, 3]
                sc_specs = []
                for h in range(4):
                    s_ = SL[h]
                    bank = psA if s_ < 2 else psB
                    sc_specs.append((bank[:, (s_ % 2) * 128:(s_ % 2) * 128 + 128],
                                     KT[(h % 2) * 64:(h % 2) * 64 + 64, h // 2, tok],
                                     QT[(h % 2) * 64:(h % 2) * 64 + 64, h // 2, tok], True, True))
                MM(sc_specs, ["kT%d_%d" % (c, pt) for c in range(2)] + ["qT%d_%d" % (c, pt) for c in range(2)], [psAr, psBr])
                sm, smr = SMR.get()
                TT(sm[:, 0:256], psA[:, 0:256], dmask[:, 0:256], ALU.mult, [psAr, "cst"], [smr + "a"])
                TT(sm[:, 256:512], psB[:, 0:256], dmask[:, 256:512], ALU.mult, [psBr, "cst"], [smr + "b"])
                if mid_cb is not None:
                    mid_cb()
                poA, poAr = pf(4)
                poB, poBr = pf(5)

                def obank(h):
                    s_ = SL[h]
                    return (poA if s_ < 2 else poB)[:, (s_ % 2) * 128:(s_ % 2) * 128 + 128]

                def obr(h):
                    return poAr if SL[h] < 2 else poBr

                specs = []
                for h in range(4):
                    s_ = SL[h]
                    specs.append((obank(h), sm[:, s_ * 128:(s_ + 1) * 128], vb[:, h * 128:(h + 1) * 128], True, n == 0))
                    if n > 0:
                        specs.append((obank(h),
                                      QXT[(h % 2) * 64:(h % 2) * 64 + 64, h // 2, tok],
                                      RB[(h % 2) * 64:(h % 2) * 64 + 64, h // 2, :], False, True))
                MM(specs, [smr + "a", smr + "b", vbr, "rb"] + ["qxT%d_%d" % (c, pt) for c in range(2)], [poAr, poBr])
                if n < 15:
                    pkv, pkvr = pf(2)
                    MM([(pkv[:, p * 256:(p + 1) * 256], kz[:, p * 128:(p + 1) * 128], vb[:, p * 256:(p + 1) * 256], True, True)
                        for p in range(2)], [kzr, vbr], [pkvr])
                    for p in range(2):
                        for j in range(2):
                            rows = slice(j * 64, (j + 1) * 64)
                            STT(RS[rows, p, :], RS[rows, p, :], CST[rows, C_GC + p:C_GC + p + 1],
                                pkv[rows, p * 256 + j * 128:p * 256 + (j + 1) * 128], ALU.mult, ALU.add,
                                ["rs", pkvr, "cst"], ["rs"])
                    CP(RB[:], RS[:], ["rs"], ["rb"])
                for h in range(4):
                    S.op("dve", lambda e, o_=SM_[:, 40 + h * 6:46 + h * 6], i_=obank(h): e.bn_stats(out=o_, in_=i_),
                         [obr(h)], ["bst%d" % h])
                for h in range(4):
                    S.op("dve", lambda e, o_=SM_[:, 20 + h * 2:22 + h * 2], i_=SM_[:, 40 + h * 6:46 + h * 6]: e.bn_aggr(out=o_, in_=i_),
                         ["bst%d" % h], ["mv%d" % h])
                mv3 = SM_[:, 20:28].rearrange("p (h t) -> p h t", t=2)
                TS(SM_[:, 28:32], mv3[:, :, 1], EPS, None, ALU.add, None, ["mv%d" % h for h in range(4)], ["ve"])
                POW(SM_[:, 4:8], SM_[:, 28:32], ["ve"], ["grstd"])
                STT(SM_[:, 12:16], mv3[:, :, 0], -1.0, SM_[:, 4:8], ALU.mult, ALU.mult, ["mv%d" % h for h in range(4)] + ["grstd"], ["gnmr"])
                on, onr = ONR.get()
                for h in range(4):
                    ACT(on[:, h * 128:(h + 1) * 128], obank(h), AF.Identity, [obr(h), "grstd", "gnmr"], [onr + str(h)],
                        scale=SM_[:, 4 + h:5 + h], bias=SM_[:, 12 + h:13 + h])
                return {"on": on, "onr": onr, "sg": sg, "sgr": sgr, "tok": tok, "pt": pt}

            def ret_aa(ctx):
                aa, aar = AAR.get()
                TT(aa, ctx["on"], ctx["sg"], ALU.mult, [ctx["onr"] + str(h) for h in range(4)] + [ctx["sgr"]], [aar])
                ctx["aa"], ctx["aar"] = aa, aar

            def ret_tail(ctx):
                aa, aar, tok, pt = ctx["aa"], ctx["aar"], ctx["tok"], ctx["pt"]
                pa2, pa2r = pb()
                TR([(pa2[:, k4 * 128:(k4 + 1) * 128], aa[:, k4 * 128:(k4 + 1) * 128]) for k4 in range(4)], [aar, "idb"], [pa2r])
                ACT(ART[:, :, tok], pa2.rearrange("p (k n) -> p k n", n=128), AF.Copy, [pa2r], ["art%d" % pt])

            rctx = ret_front(0)
            for i in range(8):
                if i + 1 < 8:
                    nctx = ret_front(i + 1, mid_cb=lambda c=rctx: ret_aa(c))
                else:
                    nctx = None
                    ret_aa(rctx)
                ret_tail(rctx)
                rctx = nctx
            if l == 0 and hf == 0:
                DBG("ART", ART, ["art0", "art1"])

            PjR = Rot("pj", [1026], BF16)
            BjR = Rot("bj", [1024], BF16)
            cxR = Rot("cx", [512], F32)
            DGR = Rot("dg", [3, 128], BF16)
            def sc_front(j):
                W, wr = wblock(l, "sc%d" % j)
                pj, pjr = PjR.get()
                bj, bjr = BjR.get()
                if hf == 0:
                    MEMSET(pj[:, 0:2], 0.0, [pjr + "h"])
                else:
                    CP(pj[:, 0:2], PH[:, j, :], ["ph%d" % j], [pjr + "h"])
                dg, dgr = DGR.get()
                for k in range(3):
                    TS(dg[:, k, :], ident_f, PV[:, PV_SCW + j * 3 + k:PV_SCW + j * 3 + k + 1], None, ALU.mult, None,
                       ["cst", "pv"], [dgr + str(k)])
                for pt in range(2):
                    htr = ["ht%d_%d" % (kc, pt) for kc in range(8)]
                    ps3 = []
                    for b in range(3):
                        p, pr = pf()
                        MM([(p, W[:, kc, b * 128:(b + 1) * 128], HT[:, kc, pt * 512:(pt + 1) * 512], kc == 0, kc == 7)
                            for kc in range(8)], htr + [wr], [pr])
                        ps3.append((p, pr))
                    ACT(bj[:, pt * 512:(pt + 1) * 512], ps3[0][0], AF.Copy, [ps3[0][1]], [bjr + str(pt)])
                    cx, cxr = cxR.get()
                    ACT(cx, ps3[1][0], AF.Copy, [ps3[1][1]], [cxr])
                    TT(pj[:, 2 + pt * 512:2 + (pt + 1) * 512], ps3[2][0], cx, ALU.mult, [ps3[2][1], cxr], [pjr + str(pt)])
                CP(PH[:, j, :], pj[:, 1024:1026], [pjr + "1", pjr + "h"], ["ph%d" % j])
                return (j, pj, pjr, bj, bjr, dg, dgr)

            def sc_tail(ctx):
                j, pj, pjr, bj, bjr, dg, dgr = ctx
                for pt in range(2):
                    p, pr = pf()
                    MM([(p, dg[:, k, :], pj[:, pt * 512 + k:pt * 512 + k + 512], k == 0, k == 2) for k in range(3)],
                       [dgr + "0", dgr + "1", dgr + "2", pjr + "h", pjr + "0", pjr + "1"], [pr])
                    TT(AST[:, j, pt * 512:(pt + 1) * 512], p, bj[:, pt * 512:(pt + 1) * 512], ALU.mult, [pr, bjr + str(pt)], ["ast%d" % pt])

            sctx = sc_front(0)
            for j in range(4):
                nctx = sc_front(j + 1) if j + 1 < 4 else None
                sc_tail(sctx)
                sctx = nctx
            AR.pop(pool_wait=False)
            if l == 0 and hf == 0:
                DBG("AST", AST, ["ast0", "ast1"])

            AR.push()
            G = AR.alloc([4, 1054], BF16)
            CB = AR.alloc([4, 1024], BF16)
            CSQ = AR.alloc([4, 1024], BF16)
            DG31R = Rot("dg31", [31, 128], BF16)
            sbR = Rot("sb", [512], F32)
            for j in range(4):
                W, wr = wblock(l, "cf%d" % j)
                if hf == 0:
                    MEMSET(G[:, j, 0:30], 0.0, ["g%dh" % j])
                else:
                    CP(G[:, j, 0:30], GH[:, j, :], ["gh%d" % j], ["g%dh" % j])
                for pt in range(2):
                    htr = ["ht%d_%d" % (kc, pt) for kc in range(8)]
                    p2 = []
                    for b in range(2):
                        p, pr = pf()
                        MM([(p, W[:, kc, b * 128:(b + 1) * 128], HT[:, kc, pt * 512:(pt + 1) * 512], kc == 0, kc == 7)
                            for kc in range(8)], htr + [wr], [pr])
                        p2.append((p, pr))
                    sb, sbr = sbR.get()
                    ACT(sb, p2[1][0], AF.Sigmoid, [p2[1][1]], [sbr])
                    TT(G[:, j, 30 + pt * 512:30 + (pt + 1) * 512], p2[0][0], sb, ALU.mult, [p2[0][1], sbr], ["g%d_%d" % (j, pt)])
                CP(GH[:, j, :], G[:, j, 1024:1054], ["g%d_1" % j, "g%dh" % j], ["gh%d" % j])
            def dg31_build(j):
                dg, dgr = DG31R.get()
                for k in range(31):
                    sc_ap = PV[:, PV_CFW + j * 31 + k:PV_CFW + j * 31 + k + 1]
                    TS(dg[:, k, :], ident_f, sc_ap, None, ALU.mult, None, ["cst", "pv"], [dgr + "_%d" % k])
                return dg, dgr

            rowA = AR.alloc([512], F32)
            rowB = AR.alloc([512], F32)
            rowC = AR.alloc([512], F32)
            rowD = AR.alloc([512], F32)
            tR = Rot("lnt", [512], F32)
            t2R_ = Rot("lnt2", [512], F32)
            bc = {}

            def ln_chain(pt):
                sl = slice(pt * 512, (pt + 1) * 512)
                pm, pmr = pf(0)
                MM([(pm[0:1, :], ONEC[:, 0:1], CB[:, j, sl], j == 0, j == 3) for j in range(4)],
                   ["onec"] + ["cb%d_%d" % (j, pt) for j in range(4)], [pmr])
                pe2, pe2r = pf(1)
                MM([(pe2[0:1, :], ONEC[:, 0:1], CSQ[:, j, sl], j == 0, j == 3) for j in range(4)],
                   ["onec"] + ["csq%d_%d" % (j, pt) for j in range(4)], [pe2r])
                ACT(rowA[0:1, :], pm[0:1, :], AF.Copy, [pmr], ["rowA"])
                ACT(rowB[0:1, :], pm[0:1, :], AF.Square, [pmr], ["rowB"])
                STT(rowB[0:1, :], pe2[0:1, :], EPS, rowB[0:1, :], ALU.add, ALU.subtract, [pe2r, "rowB"], ["rowB"])
                POW(rowC[0:1, :], rowB[0:1, :], ["rowB"], ["rowC"])
                STT(rowD[0:1, :], rowA[0:1, :], -1.0, rowC[0:1, :], ALU.mult, ALU.mult, ["rowA", "rowC"], ["rowD"])
                pr_, prr = pf(2 + 2 * pt)
                MM([(pr_, ONER[0:1, :], rowC[0:1, :], True, True)], ["oner", "rowC"], [prr])
                pn_, pnr = pf(3 + 2 * pt)
                MM([(pn_, ONER[0:1, :], rowD[0:1, :], True, True)], ["oner", "rowD"], [pnr])
                bc[pt] = (pr_, prr, pn_, pnr)

            def ln_norm(pt):
                sl = slice(pt * 512, (pt + 1) * 512)
                pr_, prr, pn_, pnr = bc[pt]
                for j in range(4):
                    t, tr = tR.get()
                    TT(t, pr_, CB[:, j, sl], ALU.mult, [prr, "cb%d_%d" % (j, pt)], [tr])
                    t2, t2r = t2R_.get()
                    TT(t2, pn_, t, ALU.add, [pnr, tr], [t2r])
                    ACT(ACFT[:, j, sl], t2, AF.Silu, [t2r, "pv"], ["acft%d" % pt],
                        scale=PV[:, PV_LNG + j:PV_LNG + j + 1], bias=PV[:, PV_LNB + j:PV_LNB + j + 1])

            dgc = dg31_build(0)
            for j in range(4):
                dgn = dg31_build(j + 1) if j + 1 < 4 else None
                dg, dgr = dgc
                for pt in range(2):
                    p, pr = pf(0) if (j == 3 and pt == 1) else pf()
                    MM([(p, dg[:, k, :], G[:, j, pt * 512 + k:pt * 512 + k + 512], k == 0, k == 30) for k in range(31)],
                       [dgr + "_%d" % k for k in range(31)] + ["g%dh" % j, "g%d_0" % j, "g%d_1" % j], [pr])
                    bias = PV[:, PV_CFB + j:PV_CFB + j + 1]
                    ACT(CB[:, j, pt * 512:(pt + 1) * 512], p, AF.Identity, [pr, "pv"], ["cb%d_%d" % (j, pt)], bias=bias)
                    ACT(CSQ[:, j, pt * 512:(pt + 1) * 512], p, AF.Square, [pr, "pv"], ["csq%d_%d" % (j, pt)], bias=bias)
                    if j == 3 and pt == 0:
                        ln_chain(0)
                dgc = dgn
            ln_chain(1)
            ln_norm(0)
            ln_norm(1)
            AR.pop(pool_wait=False)
            if l == 0 and hf == 0:
                DBG("ACFT", ACFT, ["acft0", "acft1"])

            AR.push()
            MT = AR.alloc([8, 1024], BF16)
            sgR = Rot("sgm", [512], F32, 3)
            mR = Rot("mm", [512], F32, 2)
            tmR = Rot("tm", [512], F32, 2)
            AB = [ART, AST, ACFT]
            st["pf"] = 0
            ABn = ["art", "ast", "acft"]
            for f in range(8):
                Wgb, wgr_ = wblock(l, "gb%d" % f)
                wbr_ = wgr_
                Wg_ = Wgb[:, 0, 0:3072].rearrange("p (k n) -> p k n", n=384)
                Wb_ = Wgb[:, 0, 3072:4608].rearrange("p (k n) -> p k n", n=384)
                for pt in range(2):
                    sl = slice(pt * 512, (pt + 1) * 512)
                    htr = ["ht%d_%d" % (kc, pt) for kc in range(8)]
                    m, mr = mR.get()
                    for b in range(3):
                        pg_, pgr_ = pf()
                        MM([(pg_, Wg_[:, kc, b * 128:(b + 1) * 128], HT[:, kc, sl], kc == 0, kc == 7) for kc in range(8)],
                           htr + [wgr_], [pgr_])
                        py_, pyr_ = pf()
                        MM([(py_, Wb_[:, k4, b * 128:(b + 1) * 128], AB[b][:, k4, sl], k4 == 0, k4 == 3) for k4 in range(4)],
                           [ABn[b] + str(pt), wbr_], [pyr_])
                        sgm, sgmr = sgR.get()
                        ACT(sgm, pg_, AF.Sigmoid, [pgr_], [sgmr])
                        if b == 0:
                            TT(m, py_, sgm, ALU.mult, [pyr_, sgmr], [mr])
                        else:
                            tm, tmr = tmR.get()
                            TT(tm, py_, sgm, ALU.mult, [pyr_, sgmr], [tmr])
                            if b == 1:
                                TT(m, m, tm, ALU.add, [mr, tmr], [mr])
                            else:
                                TT(MT[:, f, sl], m, tm, ALU.add, [mr, tmr], ["mt%d" % pt])
            if l == 0 and hf == 0:
                DBG("MT", MT, ["mt0", "mt1"])

            S.dma("sp", lambda e, l=l: e.dma_start(out=GBC[:], in_=gbc_d[l, 0]), "gbc", writes=["gbc"])
            Wo0, wo0r = wblock(l, "wo0")
            Wo1, wo1r = wblock(l, "wo1")
            tmpR = {"junk": AR.alloc([512], BF16), "t": Rot("pnt", [512], F32, 2)}
            if hf == 0:
                prenorm_stats([8 + i for i in range(8)], 1)
            else:
                ffn_stats(0)
            for i in range(8):
                tt = hf * 8 + i
                pt = i // 4
                tok = slice(i * 128, (i + 1) * 128)
                Y, Yr = [], []
                for h, (Wo, wor) in enumerate(((Wo0, wo0r), (Wo1, wo1r))):
                    p, pr = pf()
                    MM([(p, MT[:, kc, tok], Wo[:, kc, :], kc == 0, kc == 7) for kc in range(8)], ["mt%d" % pt, wor], [pr])
                    Y.append(p)
                    Yr.append(pr)
                postnorm(tt, Y, Yr, tmpR)
            AR.pop()
            AR.pop()
            if l == 0 and hf == 0:
                DBG("X1", X[:, 0:8, :], ["x%d" % t for t in range(8)])

        AR.push()
        WD = AR.alloc([NFF, 1024], BF16)
        ACTT = AR.alloc([NFF, 512], BF16)
        HN2 = AR.alloc([2, 1024], BF16)
        UGR = Rot("ug", [514], BF16)
        UVR = Rot("uv", [514], BF16)
        DGF = Rot("dgf", [6, 128], BF16)
        slR = Rot("sl", [512], BF16)
        tmpR = {"junk": AR.alloc([512], BF16), "t": Rot("pnt", [512], F32, 2)}
        S.dma("sp", lambda e, l=l: e.dma_start(out=GBC[:], in_=gbc_d[l, 1]), "gbc", writes=["gbc"])
        wd_issued = 0

        def ffn_apply(q):
            sl_ = (q % 2) * 16
            g = q % 2
            for pair in range(2):
                for a2 in range(2):
                    a = pair * 2 + a2
                    tt = q * 4 + a
                    TS(HN2[:, a2, :], X[:, tt, :], PST[:, sl_ + 8 + a:sl_ + 9 + a], None, ALU.mult, None,
                       ["x%d" % tt, "frs%d" % (q % 2)], ["hn2_%d" % a2])
                for kc in range(8):
                    p, pr = pb()
                    TR([(p[:, a2 * 128:(a2 + 1) * 128], HN2[:, a2, kc * 128:(kc + 1) * 128]) for a2 in range(2)],
                       ["hn2_0", "hn2_1", "idb"], [pr])
                    dst = HT[:, kc, g * 512 + pair * 256:g * 512 + (pair + 1) * 256]
                    res = ["ht%d_%d_%d" % (kc, g, pair)]
                    if kc % 2 == 0:
                        ACT(dst, p[:, 0:256], AF.Copy, [pr, "pv"], res, scale=PV[:, PV_FFNPRE + kc:PV_FFNPRE + kc + 1])
                    else:
                        TS(dst, p[:, 0:256], PV[:, PV_FFNPRE + kc:PV_FFNPRE + kc + 1], None, ALU.mult, None, [pr, "pv"], res)

        ffn_apply(0)
        for qt in range(4):
            hg = qt % 2
            HTq = HT[:, :, hg * 512:(hg + 1) * 512]
            htr = ["ht%d_%d_%d" % (kc, hg, pr_) for kc in range(8) for pr_ in range(2)]
            def up_front(j):
                W, wr = wblock(l, "up%d" % j)
                ug, ugr = UGR.get()
                uv, uvr = UVR.get()
                if qt == 0:
                    MEMSET(ug[:, 0:2], 0.0, [ugr + "h"])
                    MEMSET(uv[:, 0:2], 0.0, [uvr + "h"])
                else:
                    CP(ug[:, 0:2], FH[:, j, 0:2], ["fh%d" % j], [ugr + "h"])
                    CP(uv[:, 0:2], FH[:, j, 2:4], ["fh%d" % j], [uvr + "h"])
                dg, dgr = DGF.get()
                for k in range(3):
                    TS(dg[:, k, :], ident_f, PV[:, PV_FGW + j * 3 + k:PV_FGW + j * 3 + k + 1], None, ALU.mult, None, ["cst", "pv"], [dgr + str(k)])
                    TS(dg[:, 3 + k, :], ident_f, PV[:, PV_FVW + j * 3 + k:PV_FVW + j * 3 + k + 1], None, ALU.mult, None, ["cst", "pv"], [dgr + str(3 + k)])
                pg_, pgr_ = pf()
                MM([(pg_, W[:, kc, 0:128], HTq[:, kc, :], kc == 0, kc == 7) for kc in range(8)], htr + [wr], [pgr_])
                pv2, pv2r = pf()
                MM([(pv2, W[:, kc, 128:256], HTq[:, kc, :], kc == 0, kc == 7) for kc in range(8)], htr + [wr], [pv2r])
                ACT(ug[:, 2:514], pg_, AF.Copy, [pgr_], [ugr])
                ACT(uv[:, 2:514], pv2, AF.Copy, [pv2r], [uvr])
                CP(FH[:, j, 0:2], ug[:, 512:514], [ugr, ugr + "h"], ["fh%d" % j])
                CP(FH[:, j, 2:4], uv[:, 512:514], [uvr, uvr + "h"], ["fh%d" % j])
                return (j, ug, ugr, uv, uvr, dg, dgr)

            def up_tail(ctx):
                j, ug, ugr, uv, uvr, dg, dgr = ctx
                pcg, pcgr = pf()
                MM([(pcg, dg[:, k, :], ug[:, k:k + 512], k == 0, k == 2) for k in range(3)],
                   [dgr + "0", dgr + "1", dgr + "2", ugr, ugr + "h"], [pcgr])
                pcv, pcvr = pf()
                MM([(pcv, dg[:, 3 + k, :], uv[:, k:k + 512], k == 0, k == 2) for k in range(3)],
                   [dgr + "3", dgr + "4", dgr + "5", uvr, uvr + "h"], [pcvr])
                sl_, slr = slR.get()
                ACT(sl_, pcg, AF.Silu, [pcgr], [slr])
                TT(ACTT[:, j, :], pcv, sl_, ALU.mult, [pcvr, slr], ["actt%d" % j])

            def wd_maybe(j):
                nonlocal wd_issued
                if qt == 0 and j % 2 == 1 and wd_issued < 11:
                    c = wd_issued
                    o, kc_, n_ = offs["wd%d" % c]
                    S.dma("pool", lambda e, o_=WD[:, 2 * c:2 * c + 2, :].rearrange("p k (a b) -> p (k a) b", b=512),
                          i_=wst_d[l, :, o:o + 2048].rearrange("p (a b) -> p a b", b=512): e.dma_start(out=o_, in_=i_),
                          "wd", writes=["wd"])
                    wd_issued += 1

            uctx = up_front(0)
            for j in range(NFF):
                wd_maybe(j)
                if qt < 3 and j == 6:
                    ffn_stats(qt + 1)
                if qt < 3 and j == 13:
                    ffn_apply(qt + 1)
                nctx = up_front(j + 1) if j + 1 < NFF else None
                up_tail(uctx)
                uctx = nctx
            if l == 0 and qt == 0:
                DBG("ACTT", ACTT, ["actt%d" % j for j in range(NFF)])
            if qt == 3 and l + 1 < n_layers:
                prenorm_stats([i for i in range(8)], 0)
            for a in range(4):
                tt = qt * 4 + a
                tok = slice(a * 128, (a + 1) * 128)
                Y, Yr = [], []
                for h in range(2):
                    p, pr = pf()
                    MM([(p, ACTT[:, kc, tok], WD[:, kc, h * 512:(h + 1) * 512], kc == 0, kc == NFF - 1) for kc in range(NFF)],
                       ["actt%d" % kc for kc in range(NFF)] + ["wd"], [pr])
                    Y.append(p)
                    Yr.append(pr)
                postnorm(tt, Y, Yr, tmpR)
            if l == n_layers - 1:
                S.dma("sp", lambda e, qt=qt: e.dma_start(
                    out=out_d[qt * 512:(qt + 1) * 512, :].rearrange("(a p) d -> p a d", p=128),
                    in_=X[:, qt * 4:(qt + 1) * 4, :]), "out%d" % qt, reads=["x%d" % (qt * 4 + a) for a in range(4)])
        AR.pop()
    S.barrier()
    S.emit()
    return nc, dbg_d, S, AR


_CACHE = {}


def kernel(**inputs):
    x = np.asarray(inputs["x"], np.float32)
    B = x.shape[0]
    wst = np.stack([prep_layer_stream(inputs, l) for l in range(NL)], axis=0)
    pv = np.stack([prep_pv(inputs, l) for l in range(NL)], axis=0)
    gbc = np.stack([np.stack([np.broadcast_to(np.asarray(inputs["norm_mix_post"][l], np.float32)[None, :], (128, D)),
                              np.broadcast_to(np.asarray(inputs["norm_ffn_post"][l], np.float32)[None, :], (128, D))], axis=0)
                    for l in range(NL)], axis=0)
    gbc = np.ascontiguousarray(gbc)
    cst, rope = make_consts()
    nc = build()[0]
    in_maps = [{"x": np.ascontiguousarray(x[b]), "wst": wst, "pv": pv, "gbc": gbc, "cst": cst, "rope": rope} for b in range(B)]
    res = run_bass_kernel_spmd(nc, in_maps, core_ids=list(range(B)))
    return np.stack([np.asarray(r["out"], np.float32) for r in res.results], axis=0)
```

```python
import contextlib
import numpy as np
import concourse.bass as bass
import concourse.mybir as mybir
from concourse.bass_utils import run_bass_kernel_spmd

F32 = mybir.dt.float32
BF16 = mybir.dt.bfloat16
AF = mybir.ActivationFunctionType
ALU = mybir.AluOpType

D = 1024
SEQ = 2048
NL = 2
NFF = 22
EPS = 1e-6
NSLOT = 3
SLOTC = 4608
ENGS = ("pe", "act", "dve", "pool", "sp")

PV_MIXPRE, PV_FFNPRE, PV_SCW, PV_CFW, PV_CFB, PV_LNG, PV_LNB, PV_FGW, PV_FVW = 0, 8, 16, 28, 152, 156, 160, 164, 230
NPV = 296
C_ID, C_DM, C_XI, C_ZS, C_GC = 0, 128, 640, 896, 1152
NCST = 1154


class Sched:
    def __init__(self, nc, self_sync=("act", "dve", "pool")):
        self.nc = nc
        self.prog = {e: [] for e in ENGS}
        self.cnt = {e: 0 for e in ENGS}
        self.waited = {}
        self.lastw = {}
        self.readers = {}
        self.self_sync = set(self_sync)
        self.dma_sems = {}
        self.n_instr = 0

    def _need(self, eng, reads, writes, skip=None):
        need = {}

        def add(dep):
            if dep is None:
                return
            d, v = dep
            if need.get(d, 0) < v:
                need[d] = v

        for r in reads:
            add(self.lastw.get(r))
            if r.startswith("pf") or r.startswith("pb"):
                for d, v in self.readers.get(r, {}).items():
                    if d != eng:
                        add((d, v))
        for w in writes:
            add(self.lastw.get(w))
            for d, v in self.readers.get(w, {}).items():
                add((d, v))
        out = []
        for d, v in need.items():
            if d == skip:
                continue
            if d == eng and eng not in self.self_sync:
                continue
            key = (eng, d)
            if self.waited.get(key, 0) >= v:
                continue
            self.waited[key] = v
            out.append((d, v))
        return out

    def _mark(self, who, idx, reads, writes):
        for r in reads:
            self.readers.setdefault(r, {})[who] = idx
        for w in writes:
            self.lastw[w] = (who, idx)
            self.readers[w] = {}

    def op(self, eng, fn, reads=(), writes=()):
        waits = self._need(eng, reads, writes)
        self.cnt[eng] += 1
        self.prog[eng].append((waits, fn, ("eng", eng, 1)))
        self._mark(eng, self.cnt[eng], reads, writes)
        self.n_instr += 1

    def pe(self, fns, reads=(), writes=()):
        waits = self._need("pe", reads, writes)
        self.cnt["pe"] += 1
        n = len(fns)
        for i, fn in enumerate(fns):
            self.prog["pe"].append((waits if i == 0 else [], fn, ("eng", "pe", 1) if i == n - 1 else None))
        self._mark("pe", self.cnt["pe"], reads, writes)
        self.n_instr += n

    def dma(self, queue, fn, semname, reads=(), writes=()):
        d = "dma:" + semname
        waits = self._need(queue, reads, writes, skip=d)
        c = self.dma_sems.setdefault(semname, [0])
        c[0] += 16
        self.prog[queue].append((waits, fn, ("dma", semname, 16)))
        self._mark(d, c[0], reads, writes)
        self.n_instr += 1

    def barrier(self, pool_wait=True):
        snap = dict(self.cnt)
        dsnap = {"dma:" + k: v[0] for k, v in self.dma_sems.items()}
        for e in ENGS:
            if e == "pe":
                continue
            if e == "pool" and not pool_wait:
                continue
            waits = []
            for d, v in list(snap.items()) + list(dsnap.items()):
                if d != e and v > 0 and self.waited.get((e, d), 0) < v:
                    self.waited[(e, d)] = v
                    waits.append((d, v))
            if waits:
                self.prog[e].append((waits, None, None))

    def emit(self):
        nc = self.nc
        with nc.cleanup_on_exit():
            sems = {}
            for e in ENGS:
                sems[e] = nc.alloc_semaphore(name="s_" + e)
            for k in self.dma_sems:
                sems["dma:" + k] = nc.alloc_semaphore(name="d_" + k)
            for s_ in sems.values():
                nc.gpsimd.sem_clear(s_)
            nc.all_engine_barrier()
            with nc.Block() as block:

                def run(eng_name):
                    def body(eng):
                        for waits, fn, inc in self.prog[eng_name]:
                            for d, v in waits:
                                eng.wait_ge(sems[d], v)
                            if fn is None:
                                continue
                            ins = fn(eng)
                            if inc is not None:
                                kind, name, n = inc
                                ins.then_inc(sems[name] if kind == "eng" else sems["dma:" + name], n)
                    return body

                block.tensor(run("pe"))
                block.scalar(run("act"))
                block.vector(run("dve"))
                block.gpsimd(run("pool"))
                block.sync(run("sp"))


def _blk(w):
    K, n = w.shape
    return np.ascontiguousarray(w.reshape(K // 128, 128, n).transpose(1, 0, 2)).reshape(128, -1)


def weight_blocks():
    blocks = [("q", 8, 512), ("k", 8, 512), ("v", 8, 512), ("g", 8, 512)]
    blocks += [("sc%d" % j, 8, 384) for j in range(4)]
    blocks += [("cf%d" % j, 8, 256) for j in range(4)]
    blocks += [("gb%d" % f, 1, 4608) for f in range(8)]
    blocks += [("wo0", 8, 512), ("wo1", 8, 512)]
    blocks += [("up%d" % j, 8, 256) for j in range(NFF)]
    blocks += [("wd%d" % c, 2, 1024) for c in range(11)]
    offs = {}
    o = 0
    for name, kc, n in blocks:
        offs[name] = (o, kc, n)
        o += kc * n
    return blocks, offs, o


def prep_layer_stream(inp, l):
    w_in = np.asarray(inp["w_in"][l], dtype=np.float32)
    perm = np.array([h * 64 + (i + 32) % 64 for h in range(4) for i in range(64)])
    q = w_in[:, 0:256]
    k = w_in[:, 256:512]
    parts = {}
    parts["q"] = np.concatenate([q, q[:, perm]], axis=1)
    parts["k"] = np.concatenate([k, k[:, perm]], axis=1)
    parts["v"] = w_in[:, 512:1024]
    parts["g"] = w_in[:, 1024:1536]
    for j in range(4):
        s = slice(j * 128, (j + 1) * 128)
        parts["sc%d" % j] = np.concatenate([w_in[:, 1536:2048][:, s], w_in[:, 2048:2560][:, s], w_in[:, 2560:3072][:, s]], axis=1)
        parts["cf%d" % j] = np.concatenate([w_in[:, 3072:3584][:, s], w_in[:, 3584:4096][:, s]], axis=1)
    wro = np.asarray(inp["w_ret_out"][l], np.float32)
    wso = np.asarray(inp["w_sc_out"][l], np.float32)
    wco = np.asarray(inp["w_cf_out"][l], np.float32)
    for f in range(8):
        s = slice(f * 128, (f + 1) * 128)
        gt = np.concatenate([w_in[:, 4096 + b * 1024: 4096 + (b + 1) * 1024][:, s] for b in range(3)], axis=1)
        bo = np.concatenate([wro[:, s], wso[:, s], wco[:, s]], axis=1)
        parts["gb%d" % f] = np.concatenate([_blk(gt), _blk(bo)], axis=1)
    wo = np.asarray(inp["w_o"][l], np.float32)
    parts["wo0"] = wo[:, 0:512]
    parts["wo1"] = wo[:, 512:1024]
    wup = np.asarray(inp["w_up"][l], np.float32)
    for j in range(NFF):
        s = slice(j * 128, (j + 1) * 128)
        parts["up%d" % j] = np.concatenate([wup[:, :2816][:, s], wup[:, 2816:][:, s]], axis=1)
    wd = np.asarray(inp["w_down"][l], np.float32)
    for c in range(11):
        parts["wd%d" % c] = wd[c * 256:(c + 1) * 256, :]
    blocks, offs, total = weight_blocks()
    out = np.empty((128, total), np.float32)
    for name, kc, n in blocks:
        o = offs[name][0]
        out[:, o:o + kc * n] = parts[name] if name.startswith("gb") else _blk(parts[name])
    return out


def prep_pv(inp, l):
    pv = np.zeros((128, NPV), np.float32)

    def fm(vec, nchunk):
        return np.asarray(vec, np.float32).reshape(nchunk, 128).T

    pv[:, PV_MIXPRE:PV_MIXPRE + 8] = fm(inp["norm_mix_pre"][l], 8)
    pv[:, PV_FFNPRE:PV_FFNPRE + 8] = fm(inp["norm_ffn_pre"][l], 8)
    scw = np.asarray(inp["sc_conv_w"][l], np.float32)
    pv[:, PV_SCW:PV_SCW + 12] = scw.reshape(3, 4, 128).transpose(2, 1, 0).reshape(128, 12)
    cfw = np.asarray(inp["cf_conv_w"][l], np.float32)
    pv[:, PV_CFW:PV_CFW + 124] = cfw.reshape(31, 4, 128).transpose(2, 1, 0).reshape(128, 124)
    pv[:, PV_CFB:PV_CFB + 4] = fm(inp["cf_conv_b"][l], 4)
    pv[:, PV_LNG:PV_LNG + 4] = fm(inp["cf_ln_g"][l], 4)
    pv[:, PV_LNB:PV_LNB + 4] = fm(inp["cf_ln_b"][l], 4)
    fw = np.asarray(inp["ffn_conv_w"][l], np.float32)
    pv[:, PV_FGW:PV_FGW + 66] = fw[:, :2816].reshape(3, NFF, 128).transpose(2, 1, 0).reshape(128, 66)
    pv[:, PV_FVW:PV_FVW + 66] = fw[:, 2816:].reshape(3, NFF, 128).transpose(2, 1, 0).reshape(128, 66)
    return pv


def make_consts():
    cst = np.zeros((128, NCST), np.float64)
    cst[:, C_ID:C_ID + 128] = np.eye(128)
    gam = 1.0 - np.exp2(-5.0 - np.arange(4))
    m = np.arange(128)[:, None]
    c = np.arange(128)[None, :]
    for h in range(4):
        dm = np.where(c >= m, gam[h] ** np.maximum(c - m, 0), 0.0) * 0.125
        sl_ = [0, 2, 1, 3][h]
        cst[:, C_DM + sl_ * 128:C_DM + (sl_ + 1) * 128] = dm
        cst[:, C_ZS + h * 64:C_ZS + (h + 1) * 64] = (0.125 * gam[h] ** (127 - np.arange(128)))[:, None]
    p = np.arange(128)
    for ch in range(2):
        hh = ch * 2 + p // 64
        xi = gam[hh][:, None] ** (np.arange(128)[None, :] + 1.0)
        cst[:, C_XI + ch * 128:C_XI + (ch + 1) * 128] = xi
        cst[:, C_GC + ch] = gam[hh] ** 128
    inv = 10000.0 ** (-np.arange(32) / 32.0)
    t = np.arange(SEQ)[None, :]
    ang = t * inv[p % 32][:, None]
    sign = np.where((p % 64) < 32, -1.0, 1.0)[:, None]
    rope = np.stack([np.cos(ang), sign * np.sin(ang)], axis=0)
    return cst.astype(np.float32), rope.astype(np.float32)


class Arena:
    def __init__(self, nc, S, nbytes):
        self.t = nc.alloc_sbuf_tensor("arena", [128, nbytes // 2], BF16)
        self.top = 0
        self.cap = nbytes
        self.stack = []
        self.S = S
        self.peak = 0

    def push(self):
        self.stack.append(self.top)

    def pop(self, pool_wait=True):
        self.S.barrier(pool_wait=pool_wait)
        self.top = self.stack.pop()

    def alloc(self, shape, dtype):
        n = int(np.prod(shape))
        esz = 4 if dtype == F32 else 2
        nb = (n * esz + 63) // 64 * 64
        off = self.top
        self.top += nb
        self.peak = max(self.peak, self.top)
        assert self.top <= self.cap, ("arena overflow", self.top, self.cap)
        ap = self.t[:, off // 2: off // 2 + (n * esz) // 2]
        if dtype == F32:
            ap = ap.bitcast(F32)
        if len(shape) == 2:
            names = "a b"
        else:
            names = "a b c"
        if len(shape) == 1:
            return ap
        kw = {"a": shape[0]} if len(shape) == 2 else {"a": shape[0], "b": shape[1]}
        return ap.rearrange("p (%s) -> p %s" % (names, names), **kw)


def build(n_layers=NL, dbg=()):
    nc = bass.Bass("TRN2", target_bir_lowering=False)
    blocks, offs, WCOLS = weight_blocks()
    x_d = nc.dram_tensor("x", [SEQ, D], F32, kind="ExternalInput").ap()
    wst_d = nc.dram_tensor("wst", [n_layers, 128, WCOLS], F32, kind="ExternalInput").ap()
    pv_d = nc.dram_tensor("pv", [n_layers, 128, NPV], F32, kind="ExternalInput").ap()
    gbc_d = nc.dram_tensor("gbc", [n_layers, 2, 128, D], F32, kind="ExternalInput").ap()
    cst_d = nc.dram_tensor("cst", [128, NCST], F32, kind="ExternalInput").ap()
    rope_d = nc.dram_tensor("rope", [2, 128, SEQ], F32, kind="ExternalInput").ap()
    out_d = nc.dram_tensor("out", [SEQ, D], F32, kind="ExternalOutput").ap()
    dbg_d = {}

    S = Sched(nc)

    X = nc.alloc_sbuf_tensor("X", [128, 16, D], F32)
    HT = nc.alloc_sbuf_tensor("HT", [128, 8, 1024], BF16)
    RING = nc.alloc_sbuf_tensor("RING", [128, NSLOT, SLOTC], BF16)
    CST = nc.alloc_sbuf_tensor("CST", [128, NCST], F32)
    PV = nc.alloc_sbuf_tensor("PVt", [128, NPV], F32)
    GBC = nc.alloc_sbuf_tensor("GBC", [128, D], F32)
    IDB = nc.alloc_sbuf_tensor("IDB", [128, 128], BF16)
    ONEC = nc.alloc_sbuf_tensor("ONEC", [128, 2], BF16)
    ONER = nc.alloc_sbuf_tensor("ONER", [1, 128], F32)
    RS = nc.alloc_sbuf_tensor("RS", [128, 2, 128], F32)
    RB = nc.alloc_sbuf_tensor("RB", [128, 2, 128], BF16)
    PH = nc.alloc_sbuf_tensor("PH", [128, 4, 2], BF16)
    GH = nc.alloc_sbuf_tensor("GH", [128, 4, 30], BF16)
    FH = nc.alloc_sbuf_tensor("FH", [128, NFF, 4], BF16)
    SM_ = nc.alloc_sbuf_tensor("SMALL", [128, 64], F32)
    PST = nc.alloc_sbuf_tensor("PST", [128, 32], F32)
    MST = nc.alloc_sbuf_tensor("MST", [128, 48], F32)
    JUNK = nc.alloc_sbuf_tensor("JUNK", [128, 1024], BF16)
    PSF = nc.alloc_psum_tensor("PSF", [128, 6, 512], F32)
    PSB = nc.alloc_psum_tensor("PSB", [128, 2, 1024], BF16)
    AR = Arena(nc, S, 85 * 1024)

    ident_f = CST[:, C_ID:C_ID + 128]
    dmask = CST[:, C_DM:C_DM + 512]
    zsfull = CST[:, C_ZS:C_ZS + 256]

    st = {"pf": 0, "pb": 0, "blk": 0, "issued": 0}
    stream = []
    for l in range(n_layers):
        for hf in range(2):
            for name, kc, n in blocks:
                if not (name.startswith("up") or name.startswith("wd")):
                    stream.append((l, name))
        for qt in range(4):
            for j in range(NFF):
                stream.append((l, "up%d" % j))

    def pf(fixed=None):
        if fixed is not None:
            return PSF[:, fixed, :], "pf%d" % fixed
        b = st["pf"] % 6
        st["pf"] += 1
        return PSF[:, b, :], "pf%d" % b

    def pb():
        b = st["pb"] % 2
        st["pb"] += 1
        return PSB[:, b, 0:512], "pb%d" % b

    def issue_block(i):
        l, name = stream[i]
        o, kc, n = offs[name]
        s = i % NSLOT
        cols = kc * n
        S.dma("pool", lambda e: e.dma_start(
            out=RING[:, s, 0:cols].rearrange("p (a b) -> p a b", b=512),
            in_=wst_d[l, :, o:o + cols].rearrange("p (a b) -> p a b", b=512)),
            "ring%d" % s, writes=["ring%d" % s])

    def wblock(l, name):
        i = st["blk"]
        assert stream[i] == (l, name), (stream[i], l, name)
        st["blk"] += 1
        while st["issued"] <= min(i + 1, len(stream) - 1):
            issue_block(st["issued"])
            st["issued"] += 1
        o, kc, n = offs[name]
        s = i % NSLOT
        return RING[:, s, 0:kc * n].rearrange("p (k n) -> p k n", n=n), "ring%d" % s

    class Rot:
        def __init__(self, name, shape, dtype, n=2):
            self.bufs = [AR.alloc(shape, dtype) for _ in range(n)]
            self.name = name
            self.i = 0

        def get(self):
            k = self.i % len(self.bufs)
            self.i += 1
            return self.bufs[k], "%s#%d" % (self.name, k)

    def ACT(out, in_, func, r, w, **kw):
        S.op("act", lambda e: e.activation(out=out, in_=in_, func=func, **kw), r, w)

    def TT(out, in0, in1, op, r, w):
        S.op("dve", lambda e: e.tensor_tensor(out=out, in0=in0, in1=in1, op=op), r, w)

    def TS(out, in0, s1, s2, op0, op1, r, w):
        if op1 is None:
            S.op("dve", lambda e: e.tensor_scalar(out=out, in0=in0, scalar1=s1, scalar2=None, op0=op0), r, w)
        else:
            S.op("dve", lambda e: e.tensor_scalar(out=out, in0=in0, scalar1=s1, scalar2=s2, op0=op0, op1=op1), r, w)

    def STT(out, in0, sc, in1, op0, op1, r, w):
        S.op("dve", lambda e: e.scalar_tensor_tensor(out=out, in0=in0, scalar=sc, in1=in1, op0=op0, op1=op1), r, w)

    def CP(out, in_, r, w):
        S.op("dve", lambda e: e.tensor_copy(out=out, in_=in_), r, w)

    def MM(specs, r, w):
        S.pe([(lambda e, o=o, a=a, b=b, s0=s0, s1=s1: e.matmul(o, lhsT=a, rhs=b, start=s0, stop=s1))
              for (o, a, b, s0, s1) in specs], r, w)

    def TR(specs, r, w):
        S.pe([(lambda e, o=o, a=a: e.transpose(out=o, in_=a, identity=IDB[:])) for (o, a) in specs], r, w)

    def POW(out, in0, r, w):
        tag = w[0] + "~sq"
        S.op("act", lambda e: e.activation(out=out, in_=in0, func=AF.Sqrt), r, [tag])
        S.op("dve", lambda e: e.reciprocal(out=out, in_=out), [tag], w)

    def MEMSET(ap, val, w):
        S.op("dve", lambda e: e.memset(ap, val), [], w)

    def DBG(name, ap, reads):
        if name not in dbg:
            return
        shape = list(ap.shape)
        d = nc.dram_tensor("dbg_" + name, shape, ap.dtype, kind="ExternalOutput").ap()
        dbg_d[name] = d
        S.dma("sp", lambda e: e.dma_start(out=d, in_=ap), "dbg_" + name, reads=reads)

    S.dma("sp", lambda e: e.dma_start(out=CST[:], in_=cst_d), "cst", writes=["cst"])
    for q4 in range(4):
        S.dma("sp", lambda e, q4=q4: e.dma_start(
            out=X[:, q4 * 4:(q4 + 1) * 4, :],
            in_=x_d[q4 * 512:(q4 + 1) * 512, :].rearrange("(a p) d -> p a d", p=128)),
            "x%d" % q4, writes=["x%d" % (q4 * 4 + a) for a in range(4)])
    CP(IDB[:], ident_f, ["cst"], ["idb"])
    MEMSET(ONEC[:], 1.0 / 512.0, ["onec"])
    MEMSET(ONER[:], 1.0, ["oner"])

    def prenorm_stats(tiles, slot):
        for g4 in range(2):
            so = slot * 24 + g4 * 4
            tg = "%d_%d" % (slot, g4)
            for i in range(4):
                tt = tiles[g4 * 4 + i]
                ACT(JUNK[:], X[:, tt, :], AF.Square, ["x%d" % tt], ["junkp", "mss" + tg], accum_out=MST[:, so + i:so + i + 1])
            TS(MST[:, so + 8:so + 12], MST[:, so:so + 4], 1.0 / D, EPS, ALU.mult, ALU.add, ["mss" + tg], ["mms" + tg])
            POW(MST[:, so + 16:so + 20], MST[:, so + 8:so + 12], ["mms" + tg], ["mrs" + tg])

    def prenorm(l, tiles, gcol, HTv, hn_alloc, hn_res, slot):
        for g4 in range(2):
            so = slot * 24 + g4 * 4
            tg = "%d_%d" % (slot, g4)
            for i in range(g4 * 4, g4 * 4 + 4):
                tt = tiles[i]
                TS(hn_alloc[:, i, :], X[:, tt, :], MST[:, so + 16 + (i - g4 * 4):so + 17 + (i - g4 * 4)], None, ALU.mult, None,
                   ["x%d" % tt, "mrs" + tg], hn_res(i))
            for kc in range(8):
                p, pr = pb()
                TR([(p[:, a * 128:(a + 1) * 128], hn_alloc[:, g4 * 4 + a, kc * 128:(kc + 1) * 128]) for a in range(4)],
                   [x for a in range(4) for x in hn_res(g4 * 4 + a)] + ["idb"], [pr])
                dst = HTv[:, kc, g4 * 512:(g4 + 1) * 512]
                res = ["ht%d_%d" % (kc, g4)]
                if kc % 2 == 0:
                    ACT(dst, p, AF.Copy, [pr, "pv"], res, scale=PV[:, gcol + kc:gcol + kc + 1])
                else:
                    TS(dst, p, PV[:, gcol + kc:gcol + kc + 1], None, ALU.mult, None, [pr, "pv"], res)

    def ffn_stats(q):
        sl_ = (q % 2) * 16
        for a in range(4):
            tt = q * 4 + a
            ACT(JUNK[:], X[:, tt, :], AF.Square, ["x%d" % tt], ["junkp", "fss%d" % (q % 2)],
                accum_out=PST[:, sl_ + a:sl_ + a + 1])
        TS(PST[:, sl_ + 4:sl_ + 8], PST[:, sl_:sl_ + 4], 1.0 / D, EPS, ALU.mult, ALU.add, ["fss%d" % (q % 2)], ["fms%d" % (q % 2)])
        POW(PST[:, sl_ + 8:sl_ + 12], PST[:, sl_ + 4:sl_ + 8], ["fms%d" % (q % 2)], ["frs%d" % (q % 2)])

    def postnorm(tt, Y, Yr, tmpR):
        junk = tmpR["junk"]
        for h in range(2):
            ACT(junk, Y[h], AF.Square, [Yr[h]], ["junk", "ss2_%d" % h], accum_out=SM_[:, 32 + h:33 + h])
        TT(SM_[:, 34:35], SM_[:, 32:33], SM_[:, 33:34], ALU.add, ["ss2_0", "ss2_1"], ["ms2"])
        TS(SM_[:, 35:36], SM_[:, 34:35], 1.0 / D, EPS, ALU.mult, ALU.add, ["ms2"], ["ms2b"])
        POW(SM_[:, 36:37], SM_[:, 35:36], ["ms2b"], ["rstd2"])
        for h in range(2):
            t, tr = tmpR["t"].get()
            STT(t, Y[h], SM_[:, 36:37], GBC[:, h * 512:(h + 1) * 512], ALU.mult, ALU.mult, [Yr[h], "rstd2", "gbc"], [tr])
            TT(X[:, tt, h * 512:(h + 1) * 512], X[:, tt, h * 512:(h + 1) * 512], t, ALU.add, ["x%d" % tt, tr], ["x%d" % tt])

    for l in range(n_layers):
        S.dma("sp", lambda e, l=l: e.dma_start(out=PV[:], in_=pv_d[l]), "pv", writes=["pv"])
        for hf in range(2):
            T0 = hf * 1024
            AR.push()
            ART = AR.alloc([4, 1024], BF16)
            AST = AR.alloc([4, 1024], BF16)
            ACFT = AR.alloc([4, 1024], BF16)

            AR.push()
            HN = AR.alloc([8, 1024], BF16)
            if l == 0 and hf == 0:
                prenorm_stats([i for i in range(8)], 0)
            prenorm(l, [hf * 8 + i for i in range(8)], PV_MIXPRE, HT, HN, lambda i: ["hn%d" % i], hf)
            AR.pop(pool_wait=False)
            htall = ["ht%d_%d" % (kc, g) for kc in range(8) for g in range(2)]
            if l == 0 and hf == 0:
                DBG("HT", HT[:], htall)

            AR.push()
            QT = AR.alloc([2, 1024], BF16)
            KT = AR.alloc([2, 1024], BF16)
            QXT = AR.alloc([2, 1024], BF16)
            ropeR = Rot("rope", [2, 512], F32)
            t1R = Rot("t1", [512], F32)
            t2R = Rot("t2", [512], F32)
            for which in ("q", "k"):
                W, wr = wblock(l, which)
                dstT = QT if which == "q" else KT
                for pt in range(2):
                    rp, rr = ropeR.get()
                    S.dma("sp", lambda e, rp=rp, src_=rope_d[:, :, T0 + pt * 512:T0 + (pt + 1) * 512].rearrange("t p n -> p t n"): e.dma_start(
                        out=rp, in_=src_),
                        "rope" + rr[-1], writes=[rr])
                    for c in range(2):
                        pa, par = pf()
                        MM([(pa, W[:, kc, c * 128:(c + 1) * 128], HT[:, kc, pt * 512:(pt + 1) * 512], kc == 0, kc == 7)
                            for kc in range(8)], [wr] + ["ht%d_%d" % (kc, pt) for kc in range(8)], [par])
                        pb_, pbr = pf()
                        MM([(pb_, W[:, kc, 256 + c * 128:256 + (c + 1) * 128], HT[:, kc, pt * 512:(pt + 1) * 512], kc == 0, kc == 7)
                            for kc in range(8)], [wr] + ["ht%d_%d" % (kc, pt) for kc in range(8)], [pbr])
                        t1, t1r = t1R.get()
                        t2, t2r = t2R.get()
                        TT(t1, pa, rp[:, 0, :], ALU.mult, [par, rr], [t1r])
                        TT(t2, pb_, rp[:, 1, :], ALU.mult, [pbr, rr], [t2r])
                        dres = "%sT%d_%d" % (which, c, pt)
                        TT(dstT[:, c, pt * 512:(pt + 1) * 512], t1, t2, ALU.add, [t1r, t2r], [dres])
                        if which == "q":
                            for r4 in range(4):
                                cs = slice(pt * 512 + r4 * 128, pt * 512 + (r4 + 1) * 128)
                                TT(QXT[:, c, cs], QT[:, c, cs], CST[:, C_XI + c * 128:C_XI + (c + 1) * 128], ALU.mult,
                                   [dres, "cst"], ["qxT%d_%d" % (c, pt)])
            if l == 0 and hf == 0:
                DBG("QT", QT, ["qT%d_%d" % (c, pt) for c in range(2) for pt in range(2)])
                DBG("KT", KT, ["kT%d_%d" % (c, pt) for c in range(2) for pt in range(2)])

            Wv, wvr = wblock(l, "v")
            Wg, wgr = wblock(l, "g")
            VbR = Rot("vb", [512], BF16)
            SGR = Rot("sg", [512], BF16)
            KZR = Rot("kz", [256], BF16)
            SMR = Rot("sm", [512], BF16)
            ONR = Rot("on", [512], F32)
            AAR = Rot("aa", [512], BF16)
            if hf == 0:
                MEMSET(RS[:], 0.0, ["rs"])
                MEMSET(RB[:], 0.0, ["rb"])
            def ret_front(i, mid_cb=None):
                n = hf * 8 + i
                pt = i // 4
                tok = slice(i * 128, (i + 1) * 128)
                htr = ["ht%d_%d" % (kc, pt) for kc in range(8)]
                pv_, pvr = pf(0)
                MM([(pv_, HT[:, kc, tok], Wv[:, kc, :], kc == 0, kc == 7) for kc in range(8)], htr + [wvr], [pvr])
                vb, vbr = VbR.get()
                CP(vb, pv_, [pvr], [vbr])
                pg, pgr = pf(1)
                MM([(pg, HT[:, kc, tok], Wg[:, kc, :], kc == 0, kc == 7) for kc in range(8)], htr + [wgr], [pgr])
                sg, sgr = SGR.get()
                ACT(sg, pg, AF.Silu, [pgr], [sgr])
                pk, pkr = pb()
                TR([(pk[:, c * 128:(c + 1) * 128], KT[:, c, tok]) for c in range(2)],
                   ["kT%d_%d" % (c, pt) for c in range(2)] + ["idb"], [pkr])
                kz, kzr = KZR.get()
                TT(kz, pk[:, 0:256], zsfull, ALU.mult, [pkr, "cst"], [kzr])
                psA, psAr = pf(2)
                psB, psBr = pf(3)
                HS = [0, 2, 1, 3]
                SL = [0, 2, 1, 3]
                sc_specs = []
                for h in range(4):
                    s_ = SL[h]
                    bank = psA if s_ < 2 else psB
                    sc_specs.append((bank[:, (s_ % 2) * 128:(s_ % 2) * 128 + 128],
                                     KT[(h % 2) * 64:(h % 2) * 64 + 64, h // 2, tok],
                                     QT[(h % 2) * 64:(h % 2) * 64 + 64, h // 2, tok], True, True))
                MM(sc_specs, ["kT%d_%d" % (c, pt) for c in range(2)] + ["qT%d_%d" % (c, pt) for c in range(2)], [psAr, psBr])
                sm, smr = SMR.get()
                TT(sm[:, 0:256], psA[:, 0:256], dmask[:, 0:256], ALU.mult, [psAr, "cst"], [smr + "a"])
                TT(sm[:, 256:512], psB[:, 0:256], dmask[:, 256:512], ALU.mult, [psBr, "cst"], [smr + "b"])
                if mid_cb is not None:
                    mid_cb()
                poA, poAr = pf(4)
                poB, poBr = pf(5)

                def obank(h):
                    s_ = SL[h]
                    return (poA if s_ < 2 else poB)[:, (s_ % 2) * 128:(s_ % 2) * 128 + 128]

                def obr(h):
                    return poAr if SL[h] < 2 else poBr

                specs = []
                for h in range(4):
                    s_ = SL[h]
                    specs.append((obank(h), sm[:, s_ * 128:(s_ + 1) * 128], vb[:, h * 128:(h + 1) * 128], True, n == 0))
                    if n > 0:
                        specs.append((obank(h),
                                      QXT[(h % 2) * 64:(h % 2) * 64 + 64, h // 2, tok],
                                      RB[(h % 2) * 64:(h % 2) * 64 + 64, h // 2, :], False, True))
                MM(specs, [smr + "a", smr + "b", vbr, "rb"] + ["qxT%d_%d" % (c, pt) for c in range(2)], [poAr, poBr])
                if n < 15:
                    pkv, pkvr = pf(2)
                    MM([(pkv[:, p * 256:(p + 1) * 256], kz[:, p * 128:(p + 1) * 128], vb[:, p * 256:(p + 1) * 256], True, True)
                        for p in range(2)], [kzr, vbr], [pkvr])
                    for p in range(2):
                        for j in range(2):
                            rows = slice(j * 64, (j + 1) * 64)
                            STT(RS[rows, p, :], RS[rows, p, :], CST[rows, C_GC + p:C_GC + p + 1],
                                pkv[rows, p * 256 + j * 128:p * 256 + (j + 1) * 128], ALU.mult, ALU.add,
                                ["rs", pkvr, "cst"], ["rs"])
                    CP(RB[:], RS[:], ["rs"], ["rb"])
                for h in range(4):
                    S.op("dve", lambda e, o_=SM_[:, 40 + h * 6:46 + h * 6], i_=obank(h): e.bn_stats(out=o_, in_=i_),
                         [obr(h)], ["bst%d" % h])
                for h in range(4):
                    S.op("dve", lambda e, o_=SM_[:, 20 + h * 2:22 + h * 2], i_=SM_[:, 40 + h * 6:46 + h * 6]: e.bn_aggr(out=o_, in_=i_),
                         ["bst%d" % h], ["mv%d" % h])
                mv3 = SM_[:, 20:28].rearrange("p (h t) -> p h t", t=2)
                TS(SM_[:, 28:32], mv3[:, :, 1], EPS, None, ALU.add, None, ["mv%d" % h for h in range(4)], ["ve"])
                POW(SM_[:, 4:8], SM_[:, 28:32], ["ve"], ["grstd"])
                STT(SM_[:, 12:16], mv3[:, :, 0], -1.0, SM_[:, 4:8], ALU.mult, ALU.mult, ["mv%d" % h for h in range(4)] + ["grstd"], ["gnmr"])
                on, onr = ONR.get()
                for h in range(4):
                    ACT(on[:, h * 128:(h + 1) * 128], obank(h), AF.Identity, [obr(h), "grstd", "gnmr"], [onr + str(h)],
                        scale=SM_[:, 4 + h:5 + h], bias=SM_[:, 12 + h:13 + h])
                return {"on": on, "onr": onr, "sg": sg, "sgr": sgr, "tok": tok, "pt": pt}

            def ret_aa(ctx):
                aa, aar = AAR.get()
                TT(aa, ctx["on"], ctx["sg"], ALU.mult, [ctx["onr"] + str(h) for h in range(4)] + [ctx["sgr"]], [aar])
                ctx["aa"], ctx["aar"] = aa, aar

            def ret_tail(ctx):
                aa, aar, tok, pt = ctx["aa"], ctx["aar"], ctx["tok"], ctx["pt"]
                pa2, pa2r = pb()
                TR([(pa2[:, k4 * 128:(k4 + 1) * 128], aa[:, k4 * 128:(k4 + 1) * 128]) for k4 in range(4)], [aar, "idb"], [pa2r])
                ACT(ART[:, :, tok], pa2.rearrange("p (k n) -> p k n", n=128), AF.Copy, [pa2r], ["art%d" % pt])

            rctx = ret_front(0)
            for i in range(8):
                if i + 1 < 8:
                    nctx = ret_front(i + 1, mid_cb=lambda c=rctx: ret_aa(c))
                    ret_tail(rctx)
                    rctx = nctx
            last_ret = rctx

            PjR = Rot("pj", [1026], BF16)
            BjR = Rot("bj", [1024], BF16)
            cxR = Rot("cx", [512], F32)
            DGR = Rot("dg", [3, 128], BF16)
            def sc_front(j):
                W, wr = wblock(l, "sc%d" % j)
                pj, pjr = PjR.get()
                bj, bjr = BjR.get()
                if hf == 0:
                    MEMSET(pj[:, 0:2], 0.0, [pjr + "h"])
                else:
                    CP(pj[:, 0:2], PH[:, j, :], ["ph%d" % j], [pjr + "h"])
                dg, dgr = DGR.get()
                for k in range(3):
                    TS(dg[:, k, :], ident_f, PV[:, PV_SCW + j * 3 + k:PV_SCW + j * 3 + k + 1], None, ALU.mult, None,
                       ["cst", "pv"], [dgr + str(k)])
                for pt in range(2):
                    htr = ["ht%d_%d" % (kc, pt) for kc in range(8)]
                    ps3 = []
                    for b in range(3):
                        p, pr = pf()
                        MM([(p, W[:, kc, b * 128:(b + 1) * 128], HT[:, kc, pt * 512:(pt + 1) * 512], kc == 0, kc == 7)
                            for kc in range(8)], htr + [wr], [pr])
                        ps3.append((p, pr))
                    ACT(bj[:, pt * 512:(pt + 1) * 512], ps3[0][0], AF.Copy, [ps3[0][1]], [bjr + str(pt)])
                    cx, cxr = cxR.get()
                    ACT(cx, ps3[1][0], AF.Copy, [ps3[1][1]], [cxr])
                    TT(pj[:, 2 + pt * 512:2 + (pt + 1) * 512], ps3[2][0], cx, ALU.mult, [ps3[2][1], cxr], [pjr + str(pt)])
                CP(PH[:, j, :], pj[:, 1024:1026], [pjr + "1", pjr + "h"], ["ph%d" % j])
                return (j, pj, pjr, bj, bjr, dg, dgr)

            def sc_tail(ctx):
                j, pj, pjr, bj, bjr, dg, dgr = ctx
                for pt in range(2):
                    p, pr = pf()
                    MM([(p, dg[:, k, :], pj[:, pt * 512 + k:pt * 512 + k + 512], k == 0, k == 2) for k in range(3)],
                       [dgr + "0", dgr + "1", dgr + "2", pjr + "h", pjr + "0", pjr + "1"], [pr])
                    TT(AST[:, j, pt * 512:(pt + 1) * 512], p, bj[:, pt * 512:(pt + 1) * 512], ALU.mult, [pr, bjr + str(pt)], ["ast%d" % pt])

            st["pf"] = 0
            sctx = sc_front(0)
            ret_aa(last_ret)
            ret_tail(last_ret)
            if l == 0 and hf == 0:
                DBG("ART", ART, ["art0", "art1"])
            for j in range(4):
                nctx = sc_front(j + 1) if j + 1 < 4 else None
                sc_tail(sctx)
                sctx = nctx
            AR.pop(pool_wait=False)
            if l == 0 and hf == 0:
                DBG("AST", AST, ["ast0", "ast1"])

            AR.push()
            G = AR.alloc([4, 1054], BF16)
            CB = AR.alloc([4, 1024], BF16)
            CSQ = AR.alloc([4, 1024], BF16)
            DG31R = Rot("dg31", [31, 128], BF16)
            sbR = Rot("sb", [512], F32)
            def dg31_build(j):
                dg, dgr = DG31R.get()
                for k in range(31):
                    sc_ap = PV[:, PV_CFW + j * 31 + k:PV_CFW + j * 31 + k + 1]
                    TS(dg[:, k, :], ident_f, sc_ap, None, ALU.mult, None, ["cst", "pv"], [dgr + "_%d" % k])
                return dg, dgr

            rowA = AR.alloc([512], F32)
            rowB = AR.alloc([512], F32)
            rowC = AR.alloc([512], F32)
            rowD = AR.alloc([512], F32)
            tR = Rot("lnt", [512], F32)
            t2R_ = Rot("lnt2", [512], F32)
            bc = {}

            def ln_chain(pt):
                sl = slice(pt * 512, (pt + 1) * 512)
                pm, pmr = pf(0)
                MM([(pm[0:1, :], ONEC[:, 0:1], CB[:, j, sl], j == 0, j == 3) for j in range(4)],
                   ["onec"] + ["cb%d_%d" % (j, pt) for j in range(4)], [pmr])
                pe2, pe2r = pf(1)
                MM([(pe2[0:1, :], ONEC[:, 0:1], CSQ[:, j, sl], j == 0, j == 3) for j in range(4)],
                   ["onec"] + ["csq%d_%d" % (j, pt) for j in range(4)], [pe2r])
                ACT(rowA[0:1, :], pm[0:1, :], AF.Copy, [pmr], ["rowA"])
                ACT(rowB[0:1, :], pm[0:1, :], AF.Square, [pmr], ["rowB"])
                STT(rowB[0:1, :], pe2[0:1, :], EPS, rowB[0:1, :], ALU.add, ALU.subtract, [pe2r, "rowB"], ["rowB"])
                POW(rowC[0:1, :], rowB[0:1, :], ["rowB"], ["rowC"])
                STT(rowD[0:1, :], rowA[0:1, :], -1.0, rowC[0:1, :], ALU.mult, ALU.mult, ["rowA", "rowC"], ["rowD"])
                pr_, prr = pf(2 + 2 * pt)
                MM([(pr_, ONER[0:1, :], rowC[0:1, :], True, True)], ["oner", "rowC"], [prr])
                pn_, pnr = pf(3 + 2 * pt)
                MM([(pn_, ONER[0:1, :], rowD[0:1, :], True, True)], ["oner", "rowD"], [pnr])
                bc[pt] = (pr_, prr, pn_, pnr)

            def ln_norm(pt):
                sl = slice(pt * 512, (pt + 1) * 512)
                pr_, prr, pn_, pnr = bc[pt]
                for j in range(4):
                    t, tr = tR.get()
                    TT(t, pr_, CB[:, j, sl], ALU.mult, [prr, "cb%d_%d" % (j, pt)], [tr])
                    t2, t2r = t2R_.get()
                    TT(t2, pn_, t, ALU.add, [pnr, tr], [t2r])
                    ACT(ACFT[:, j, sl], t2, AF.Silu, [t2r, "pv"], ["acft%d" % pt],
                        scale=PV[:, PV_LNG + j:PV_LNG + j + 1], bias=PV[:, PV_LNB + j:PV_LNB + j + 1])

            dgc = dg31_build(0)
            for j in range(4):
                W, wr = wblock(l, "cf%d" % j)
                if hf == 0:
                    MEMSET(G[:, j, 0:30], 0.0, ["g%dh" % j])
                else:
                    CP(G[:, j, 0:30], GH[:, j, :], ["gh%d" % j], ["g%dh" % j])
                for pt in range(2):
                    htr = ["ht%d_%d" % (kc, pt) for kc in range(8)]
                    p2 = []
                    for b in range(2):
                        p, pr = pf()
                        MM([(p, W[:, kc, b * 128:(b + 1) * 128], HT[:, kc, pt * 512:(pt + 1) * 512], kc == 0, kc == 7)
                            for kc in range(8)], htr + [wr], [pr])
                        p2.append((p, pr))
                    sb, sbr = sbR.get()
                    ACT(sb, p2[1][0], AF.Sigmoid, [p2[1][1]], [sbr])
                    TT(G[:, j, 30 + pt * 512:30 + (pt + 1) * 512], p2[0][0], sb, ALU.mult, [p2[0][1], sbr], ["g%d_%d" % (j, pt)])
                CP(GH[:, j, :], G[:, j, 1024:1054], ["g%d_1" % j, "g%dh" % j], ["gh%d" % j])
            for j in range(4):
                dgn = dg31_build(j + 1) if j + 1 < 4 else None
                dg, dgr = dgc
                for pt in range(2):
                    p, pr = pf(0) if (j == 3 and pt == 1) else pf()
                    MM([(p, dg[:, k, :], G[:, j, pt * 512 + k:pt * 512 + k + 512], k == 0, k == 30) for k in range(31)],
                       [dgr + "_%d" % k for k in range(31)] + ["g%dh" % j, "g%d_0" % j, "g%d_1" % j], [pr])
                    bias = PV[:, PV_CFB + j:PV_CFB + j + 1]
                    ACT(CB[:, j, pt * 512:(pt + 1) * 512], p, AF.Identity, [pr, "pv"], ["cb%d_%d" % (j, pt)], bias=bias)
                    ACT(CSQ[:, j, pt * 512:(pt + 1) * 512], p, AF.Square, [pr, "pv"], ["csq%d_%d" % (j, pt)], bias=bias)
                    if j == 3 and pt == 0:
                        ln_chain(0)
                dgc = dgn
            ln_chain(1)
            ln_norm(0)
            ln_norm(1)
            AR.pop(pool_wait=False)
            if l == 0 and hf == 0:
                DBG("ACFT", ACFT, ["acft0", "acft1"])

            AR.push()
            MT = AR.alloc([8, 1024], BF16)
            sgR = Rot("sgm", [512], F32, 3)
            mR = Rot("mm", [512], F32, 2)
            tmR = Rot("tm", [512], F32, 2)
            AB = [ART, AST, ACFT]
            st["pf"] = 0
            ABn = ["art", "ast", "acft"]
            for f in range(8):
                Wgb, wgr_ = wblock(l, "gb%d" % f)
                wbr_ = wgr_
                Wg_ = Wgb[:, 0, 0:3072].rearrange("p (k n) -> p k n", n=384)
                Wb_ = Wgb[:, 0, 3072:4608].rearrange("p (k n) -> p k n", n=384)
                for pt in range(2):
                    sl = slice(pt * 512, (pt + 1) * 512)
                    htr = ["ht%d_%d" % (kc, pt) for kc in range(8)]
                    m, mr = mR.get()
                    for b in range(3):
                        pg_, pgr_ = pf()
                        MM([(pg_, Wg_[:, kc, b * 128:(b + 1) * 128], HT[:, kc, sl], kc == 0, kc == 7) for kc in range(8)],
                           htr + [wgr_], [pgr_])
                        py_, pyr_ = pf()
                        MM([(py_, Wb_[:, k4, b * 128:(b + 1) * 128], AB[b][:, k4, sl], k4 == 0, k4 == 3) for k4 in range(4)],
                           [ABn[b] + str(pt), wbr_], [pyr_])
                        sgm, sgmr = sgR.get()
                        ACT(sgm, pg_, AF.Sigmoid, [pgr_], [sgmr])
                        if b == 0:
                            TT(m, py_, sgm, ALU.mult, [pyr_, sgmr], [mr])
                        else:
                            tm, tmr = tmR.get()
                            TT(tm, py_, sgm, ALU.mult, [pyr_, sgmr], [tmr])
                            if b == 1:
                                TT(m, m, tm, ALU.add, [mr, tmr], [mr])
                            else:
                                TT(MT[:, f, sl], m, tm, ALU.add, [mr, tmr], ["mt%d" % pt])
            if l == 0 and hf == 0:
                DBG("MT", MT, ["mt0", "mt1"])

            S.dma("sp", lambda e, l=l: e.dma_start(out=GBC[:], in_=gbc_d[l, 0]), "gbc", writes=["gbc"])
            Wo0, wo0r = wblock(l, "wo0")
            Wo1, wo1r = wblock(l, "wo1")
            tmpR = {"junk": AR.alloc([512], BF16), "t": Rot("pnt", [512], F32, 2)}
            if hf == 0:
                prenorm_stats([8 + i for i in range(8)], 1)
            else:
                ffn_stats(0)
            for i in range(8):
                tt = hf * 8 + i
                pt = i // 4
                tok = slice(i * 128, (i + 1) * 128)
                Y, Yr = [], []
                for h, (Wo, wor) in enumerate(((Wo0, wo0r), (Wo1, wo1r))):
                    p, pr = pf()
                    MM([(p, MT[:, kc, tok], Wo[:, kc, :], kc == 0, kc == 7) for kc in range(8)], ["mt%d" % pt, wor], [pr])
                    Y.append(p)
                    Yr.append(pr)
                postnorm(tt, Y, Yr, tmpR)
            AR.pop()
            AR.pop()
            if l == 0 and hf == 0:
                DBG("X1", X[:, 0:8, :], ["x%d" % t for t in range(8)])

        AR.push()
        WD = AR.alloc([NFF, 1024], BF16)
        ACTT = AR.alloc([NFF, 512], BF16)
        HN2 = AR.alloc([2, 1024], BF16)
        UGR = Rot("ug", [514], BF16)
        UVR = Rot("uv", [514], BF16)
        DGF = Rot("dgf", [6, 128], BF16)
        slR = Rot("sl", [512], BF16)
        tmpR = {"junk": AR.alloc([512], BF16), "t": Rot("pnt", [512], F32, 2)}
        S.dma("sp", lambda e, l=l: e.dma_start(out=GBC[:], in_=gbc_d[l, 1]), "gbc", writes=["gbc"])
        wd_issued = 0

        def ffn_apply(q):
            sl_ = (q % 2) * 16
            g = q % 2
            for pair in range(2):
                for a2 in range(2):
                    a = pair * 2 + a2
                    tt = q * 4 + a
                    TS(HN2[:, a2, :], X[:, tt, :], PST[:, sl_ + 8 + a:sl_ + 9 + a], None, ALU.mult, None,
                       ["x%d" % tt, "frs%d" % (q % 2)], ["hn2_%d" % a2])
                for kc in range(8):
                    p, pr = pb()
                    TR([(p[:, a2 * 128:(a2 + 1) * 128], HN2[:, a2, kc * 128:(kc + 1) * 128]) for a2 in range(2)],
                       ["hn2_0", "hn2_1", "idb"], [pr])
                    dst = HT[:, kc, g * 512 + pair * 256:g * 512 + (pair + 1) * 256]
                    res = ["ht%d_%d_%d" % (kc, g, pair)]
                    if kc % 2 == 0:
                        ACT(dst, p[:, 0:256], AF.Copy, [pr, "pv"], res, scale=PV[:, PV_FFNPRE + kc:PV_FFNPRE + kc + 1])
                    else:
                        TS(dst, p[:, 0:256], PV[:, PV_FFNPRE + kc:PV_FFNPRE + kc + 1], None, ALU.mult, None, [pr, "pv"], res)

        ffn_apply(0)
        for qt in range(4):
            hg = qt % 2
            HTq = HT[:, :, hg * 512:(hg + 1) * 512]
            htr = ["ht%d_%d_%d" % (kc, hg, pr_) for kc in range(8) for pr_ in range(2)]
            def up_front(j):
                W, wr = wblock(l, "up%d" % j)
                ug, ugr = UGR.get()
                uv, uvr = UVR.get()
                if qt == 0:
                    MEMSET(ug[:, 0:2], 0.0, [ugr + "h"])
                    MEMSET(uv[:, 0:2], 0.0, [uvr + "h"])
                else:
                    CP(ug[:, 0:2], FH[:, j, 0:2], ["fh%d" % j], [ugr + "h"])
                    CP(uv[:, 0:2], FH[:, j, 2:4], ["fh%d" % j], [uvr + "h"])
                dg, dgr = DGF.get()
                for k in range(3):
                    TS(dg[:, k, :], ident_f, PV[:, PV_FGW + j * 3 + k:PV_FGW + j * 3 + k + 1], None, ALU.mult, None, ["cst", "pv"], [dgr + str(k)])
                    TS(dg[:, 3 + k, :], ident_f, PV[:, PV_FVW + j * 3 + k:PV_FVW + j * 3 + k + 1], None, ALU.mult, None, ["cst", "pv"], [dgr + str(3 + k)])
                pg_, pgr_ = pf()
                MM([(pg_, W[:, kc, 0:128], HTq[:, kc, :], kc == 0, kc == 7) for kc in range(8)], htr + [wr], [pgr_])
                pv2, pv2r = pf()
                MM([(pv2, W[:, kc, 128:256], HTq[:, kc, :], kc == 0, kc == 7) for kc in range(8)], htr + [wr], [pv2r])
                ACT(ug[:, 2:514], pg_, AF.Copy, [pgr_], [ugr])
                ACT(uv[:, 2:514], pv2, AF.Copy, [pv2r], [uvr])
                CP(FH[:, j, 0:2], ug[:, 512:514], [ugr, ugr + "h"], ["fh%d" % j])
                CP(FH[:, j, 2:4], uv[:, 512:514], [uvr, uvr + "h"], ["fh%d" % j])
                return (j, ug, ugr, uv, uvr, dg, dgr)

            def up_tail(ctx):
                j, ug, ugr, uv, uvr, dg, dgr = ctx
                pcg, pcgr = pf()
                MM([(pcg, dg[:, k, :], ug[:, k:k + 512], k == 0, k == 2) for k in range(3)],
                   [dgr + "0", dgr + "1", dgr + "2", ugr, ugr + "h"], [pcgr])
                pcv, pcvr = pf()
                MM([(pcv, dg[:, 3 + k, :], uv[:, k:k + 512], k == 0, k == 2) for k in range(3)],
                   [dgr + "3", dgr + "4", dgr + "5", uvr, uvr + "h"], [pcvr])
                sl_, slr = slR.get()
                ACT(sl_, pcg, AF.Silu, [pcgr], [slr])
                TT(ACTT[:, j, :], pcv, sl_, ALU.mult, [pcvr, slr], ["actt%d" % j])

            def wd_maybe(j):
                nonlocal wd_issued
                if qt == 0 and j % 2 == 1 and wd_issued < 11:
                    c = wd_issued
                    o, kc_, n_ = offs["wd%d" % c]
                    S.dma("pool", lambda e, o_=WD[:, 2 * c:2 * c + 2, :].rearrange("p k (a b) -> p (k a) b", b=512),
                          i_=wst_d[l, :, o:o + 2048].rearrange("p (a b) -> p a b", b=512): e.dma_start(out=o_, in_=i_),
                          "wd", writes=["wd"])
                    wd_issued += 1

            uctx = up_front(0)
            for j in range(NFF):
                wd_maybe(j)
                if qt < 3 and j == 6:
                    ffn_stats(qt + 1)
                if qt < 3 and j == 13:
                    ffn_apply(qt + 1)
                nctx = up_front(j + 1) if j + 1 < NFF else None
                up_tail(uctx)
                uctx = nctx
            if l == 0 and qt == 0:
                DBG("ACTT", ACTT, ["actt%d" % j for j in range(NFF)])
            if qt == 3 and l + 1 < n_layers:
                prenorm_stats([i for i in range(8)], 0)
            for a in range(4):
                tt = qt * 4 + a
                tok = slice(a * 128, (a + 1) * 128)
                Y, Yr = [], []
                for h in range(2):
                    p, pr = pf()
                    MM([(p, ACTT[:, kc, tok], WD[:, kc, h * 512:(h + 1) * 512], kc == 0, kc == NFF - 1) for kc in range(NFF)],
                       ["actt%d" % kc for kc in range(NFF)] + ["wd"], [pr])
                    Y.append(p)
                    Yr.append(pr)
                postnorm(tt, Y, Yr, tmpR)
            if l == n_layers - 1:
                S.dma("sp", lambda e, qt=qt: e.dma_start(
                    out=out_d[qt * 512:(qt + 1) * 512, :].rearrange("(a p) d -> p a d", p=128),
                    in_=X[:, qt * 4:(qt + 1) * 4, :]), "out%d" % qt, reads=["x%d" % (qt * 4 + a) for a in range(4)])
        AR.pop()
    S.barrier()
    S.emit()
    return nc, dbg_d, S, AR


_CACHE = {}


def kernel(**inputs):
    x = np.asarray(inputs["x"], np.float32)
    B = x.shape[0]
    wst = np.stack([prep_layer_stream(inputs, l) for l in range(NL)], axis=0)
    pv = np.stack([prep_pv(inputs, l) for l in range(NL)], axis=0)
    gbc = np.stack([np.stack([np.broadcast_to(np.asarray(inputs["norm_mix_post"][l], np.float32)[None, :], (128, D)),
                              np.broadcast_to(np.asarray(inputs["norm_ffn_post"][l], np.float32)[None, :], (128, D))], axis=0)
                    for l in range(NL)], axis=0)
    gbc = np.ascontiguousarray(gbc)
    cst, rope = make_consts()
    nc = build()[0]
    in_maps = [{"x": np.ascontiguousarray(x[b]), "wst": wst, "pv": pv, "gbc": gbc, "cst": cst, "rope": rope} for b in range(B)]
    res = run_bass_kernel_spmd(nc, in_maps, core_ids=list(range(B)))
    return np.stack([np.asarray(r["out"], np.float32) for r in res.results], axis=0)
```
